# Optimizing a Trainium2 kernel written in Bass

```python
import jax
import jax.numpy as jnp
from jax import lax
import numpy as np

D_MODEL = 1024
BATCH = 2
SEQ = 8192
DEPTH = 1

GRID_W = 64
CTX_LEN = 256
NA_HEAD_DIM = 64
N_NA_HEADS = (D_MODEL // 2) // NA_HEAD_DIM
NA_WIDTH = N_NA_HEADS * NA_HEAD_DIM
NA_WIN_H = 8
NA_WIN_W = 16
NA_QBLOCK_W = 16
NA_KSPAN_W = NA_QBLOCK_W + NA_WIN_W
ROPE_BASE = 10000.0
HG_DK = 128
HG_DV = 128
N_HG_HEADS = (D_MODEL // 2) // HG_DV
HG_KEY_WIDTH = N_HG_HEADS * HG_DK
HG_WIDTH = N_HG_HEADS * HG_DV
HG_CHUNK = 32
MIX_WIDTH = NA_WIDTH + HG_WIDTH
IN_COLS = 3 * NA_WIDTH + 3 * HG_KEY_WIDTH + 2 * HG_WIDTH
N_EXPERTS = 16
EC_CAPACITY_FACTOR = 2
D_EXPERT = ((8 * D_MODEL // 3 + 63) // 64) * 64
RMS_EPS = 1e-6

kernel_name = 'hybrid_na_hgrn2_ec_diffusion_block'


def rms_norm(x, g):
    xf = x.astype(jnp.float32)
    y = xf * lax.rsqrt(jnp.mean(xf * xf, axis=-1, keepdims=True) + RMS_EPS)
    return (y * g.astype(jnp.float32)).astype(x.dtype)


def modulate(h, shift, scale):
    return h * (1.0 + scale) + shift


def to_heads(x, n_heads):
    b, t, w = x.shape
    return x.reshape(b, t, n_heads, w // n_heads).transpose(0, 2, 1, 3)


def from_heads(x):
    b, h, t, d = x.shape
    return x.transpose(0, 2, 1, 3).reshape(b, t, h * d)


def split_cols(p):
    sizes = (NA_WIDTH, NA_WIDTH, NA_WIDTH, HG_KEY_WIDTH, HG_KEY_WIDTH, HG_KEY_WIDTH, HG_WIDTH, HG_WIDTH)
    bounds = []
    acc = 0
    for s in sizes[:-1]:
        acc += s
        bounds.append(acc)
    return jnp.split(p, bounds, axis=-1)


def rope_1d(x, pos):
    d = x.shape[-1]
    inv_freq = ROPE_BASE ** (-jnp.arange(0, d, 2, dtype=jnp.float32) / d)
    ang = pos[:, None] * inv_freq[None, :]
    cos = jnp.cos(ang).astype(x.dtype)
    sin = jnp.sin(ang).astype(x.dtype)
    x1, x2 = x[..., : d // 2], x[..., d // 2:]
    return jnp.concatenate([x1 * cos - x2 * sin, x1 * sin + x2 * cos], axis=-1)


def axial_rope(x, row_pos, col_pos):
    half = x.shape[-1] // 2
    return jnp.concatenate([rope_1d(x[..., :half], row_pos), rope_1d(x[..., half:], col_pos)], axis=-1)


def neighbourhood_attention(q, k, v, k_ctx, v_ctx, rpb):
    bsz, nh, seq, dh = q.shape
    rows = seq // GRID_W
    kh = min(NA_WIN_H, rows)
    ncb = GRID_W // NA_QBLOCK_W
    scale = dh ** -0.5
    t = jnp.arange(seq)
    row_pos = (t // GRID_W).astype(jnp.float32)
    col_pos = (t % GRID_W).astype(jnp.float32)
    q_rot = axial_rope(q, row_pos, col_pos)
    k_rot = axial_rope(k, row_pos, col_pos)
    r = jnp.arange(rows)
    r0 = jnp.clip(r - kh // 2, 0, rows - kh)
    key_rows = r0[:, None] + jnp.arange(kh)[None, :]
    j = jnp.arange(ncb)
    c0 = jnp.clip(j * NA_QBLOCK_W - NA_WIN_W // 2, 0, GRID_W - NA_KSPAN_W)
    key_cols = c0[:, None] + jnp.arange(NA_KSPAN_W)[None, :]
    tok = key_rows[:, None, :, None] * GRID_W + key_cols[None, :, None, :]
    k_blk = k_rot[:, :, tok]
    v_blk = v[:, :, tok]
    q_cols = j[:, None] * NA_QBLOCK_W + jnp.arange(NA_QBLOCK_W)[None, :]
    win_c0 = jnp.clip(q_cols - NA_WIN_W // 2, 0, GRID_W - NA_WIN_W)
    kc = key_cols[:, None, :]
    in_win = (kc >= win_c0[:, :, None]) & (kc < win_c0[:, :, None] + NA_WIN_W)
    dr_idx = key_rows - r[:, None] + NA_WIN_H - 1
    dc_idx = jnp.clip(kc - q_cols[:, :, None] + NA_WIN_W - 1, 0, 2 * NA_WIN_W - 2)
    bias = rpb[:, dr_idx[:, None, None, :, None], dc_idx[None, :, :, None, :]]
    q_blk = q_rot.reshape(bsz, nh, rows, ncb, NA_QBLOCK_W, dh)
    s_win = jnp.einsum('bhrjqd,bhrjakd->bhrjqak', q_blk, k_blk).astype(jnp.float32) * scale
    s_win = jnp.where(in_win[:, :, None, :], s_win + bias.astype(jnp.float32), -jnp.inf)
    q_plain = q.reshape(bsz, nh, rows, ncb, NA_QBLOCK_W, dh)
    s_ctx = jnp.einsum('bhrjqd,bhld->bhrjql', q_plain, k_ctx).astype(jnp.float32) * scale
    n_win = kh * NA_KSPAN_W
    s_all = jnp.concatenate([s_win.reshape(bsz, nh, rows, ncb, NA_QBLOCK_W, n_win), s_ctx], axis=-1)
    p = jax.nn.softmax(s_all, axis=-1).astype(v.dtype)
    p_win = p[..., :n_win].reshape(bsz, nh, rows, ncb, NA_QBLOCK_W, kh, NA_KSPAN_W)
    o = (jnp.einsum('bhrjqak,bhrjakd->bhrjqd', p_win, v_blk)
         + jnp.einsum('bhrjql,bhld->bhrjqd', p[..., n_win:], v_ctx))
    return from_heads(o.reshape(bsz, nh, seq, dh))


def context_attention(q, k, v):
    scale = q.shape[-1] ** -0.5
    s = jnp.einsum('bhld,bhmd->bhlm', q, k).astype(jnp.float32) * scale
    p = jax.nn.softmax(s, axis=-1).astype(v.dtype)
    return jnp.einsum('bhlm,bhmd->bhld', p, v)


def log_forget(z, lb):
    lb_h = lb.reshape(N_HG_HEADS, 1, HG_DK)
    return jnp.log(lb_h + (1.0 - lb_h) * jax.nn.sigmoid(z.astype(jnp.float32)))


def gla_chunked(q, k, v, log_f, s0):
    bsz, nh, seq, dk = q.shape
    dv = v.shape[-1]
    n = seq // HG_CHUNK
    qc = q.astype(jnp.float32).reshape(bsz, nh, n, HG_CHUNK, dk)
    kc = k.astype(jnp.float32).reshape(bsz, nh, n, HG_CHUNK, dk)
    vc = v.astype(jnp.float32).reshape(bsz, nh, n, HG_CHUNK, dv)
    b = jnp.cumsum(log_f.reshape(bsz, nh, n, HG_CHUNK, dk), axis=3)
    b_end = b[:, :, :, -1:, :]
    q_dec = qc * jnp.exp(b)
    k_dec = kc * jnp.exp(-b)
    idx = jnp.arange(HG_CHUNK)
    lower = idx[:, None] >= idx[None, :]
    a = jnp.where(lower, jnp.einsum('bhnck,bhnsk->bhncs', q_dec, k_dec), 0.0)
    o_intra = jnp.einsum('bhncs,bhnsv->bhncv', a, vc)
    kv = jnp.einsum('bhnck,bhncv->bhnkv', kc * jnp.exp(b_end - b), vc)
    decay = jnp.exp(b_end[:, :, :, 0, :])

    def step(s, inp):
        dec, upd = inp
        return dec[..., None] * s + upd, s

    s_fin, s_prev = lax.scan(step, s0.astype(jnp.float32),
                             (jnp.moveaxis(decay, 2, 0), jnp.moveaxis(kv, 2, 0)))
    s_prev = jnp.moveaxis(s_prev, 0, 2)
    o_inter = jnp.einsum('bhnck,bhnkv->bhncv', q_dec, s_prev)
    return (o_intra + o_inter).reshape(bsz, nh, seq, dv), s_fin


def hgrn2_bidir(q, z_fwd, z_bwd, i, lb, s0_fwd, s0_bwd):
    lf_f = log_forget(z_fwd, lb)
    lf_b = log_forget(z_bwd, lb)
    o_f, s_f = gla_chunked(q, -jnp.expm1(lf_f), i, lf_f, s0_fwd)
    flip = lambda t: jnp.flip(t, axis=2)
    o_b, s_b = gla_chunked(flip(q), flip(-jnp.expm1(lf_b)), flip(i), flip(lf_b), s0_bwd)
    return o_f + flip(o_b), s_f, s_b


def hgrn2_readout(o, g, norm_g):
    return from_heads(rms_norm(o.astype(g.dtype), norm_g) * jax.nn.sigmoid(g))


def ec_moe(h, w_router, w_gate, w_up, w_down):
    bsz, n_tok, d = h.shape
    cap = EC_CAPACITY_FACTOR * n_tok // N_EXPERTS
    aff = jax.nn.softmax(jnp.einsum('btd,de->bte', h, w_router).astype(jnp.float32), axis=-1)
    gate, idx = lax.top_k(jnp.swapaxes(aff, 1, 2), cap)
    xs = jax.vmap(lambda hb, ib: hb[ib])(h, idx)
    hid = (jax.nn.silu(jnp.einsum('becd,edf->becf', xs, w_gate))
           * jnp.einsum('becd,edf->becf', xs, w_up))
    y = jnp.einsum('becf,efd->becd', hid, w_down) * gate[..., None].astype(h.dtype)
    return jax.vmap(lambda yb, ib: jnp.zeros((n_tok, d), y.dtype).at[ib.reshape(-1)].add(yb.reshape(-1, d)))(y, idx)


def setup_inputs(seed: int = 0) -> dict:
    key = jax.random.key(seed)
    ks = jax.random.split(key, 19)
    nrm = jax.random.normal
    d = D_MODEL
    f32 = jnp.float32
    return {
        'x': nrm(ks[0], (BATCH, SEQ, d), f32),
        'c': nrm(ks[1], (BATCH, d), f32),
        'ctx': nrm(ks[2], (BATCH, CTX_LEN, d), f32),
        'c_ctx': nrm(ks[3], (d,), f32),
        'w_mod': 0.5 * d ** -0.5 * nrm(ks[4], (DEPTH, d, 6 * d), f32),
        'b_mod': 0.01 * nrm(ks[5], (DEPTH, 6 * d), f32),
        'g_pre1': 1.0 + 0.05 * nrm(ks[6], (DEPTH, d), f32),
        'g_post1': 1.0 + 0.05 * nrm(ks[7], (DEPTH, d), f32),
        'g_pre2': 1.0 + 0.05 * nrm(ks[8], (DEPTH, d), f32),
        'g_post2': 1.0 + 0.05 * nrm(ks[9], (DEPTH, d), f32),
        'w_in': d ** -0.5 * nrm(ks[10], (DEPTH, d, IN_COLS), f32),
        'w_out': MIX_WIDTH ** -0.5 * nrm(ks[11], (DEPTH, MIX_WIDTH, d), f32),
        'na_rpb': 0.1 * nrm(ks[12], (DEPTH, N_NA_HEADS, 2 * NA_WIN_H - 1, 2 * NA_WIN_W - 1), f32),
        'hg_lb_logits': 0.5 * nrm(ks[13], (DEPTH + 1, HG_KEY_WIDTH), f32),
        'hg_norm': 1.0 + 0.05 * nrm(ks[14], (DEPTH, HG_DV), f32),
        'w_router': d ** -0.5 * nrm(ks[15], (DEPTH, d, N_EXPERTS), f32),
        'w_gate': d ** -0.5 * nrm(ks[16], (DEPTH, N_EXPERTS, d, D_EXPERT), f32),
        'w_up': d ** -0.5 * nrm(ks[17], (DEPTH, N_EXPERTS, d, D_EXPERT), f32),
        'w_down': D_EXPERT ** -0.5 * nrm(ks[18], (DEPTH, N_EXPERTS, D_EXPERT, d), f32),
    }


def reference(x, c, ctx, c_ctx, w_mod, b_mod, g_pre1, g_post1, g_pre2, g_post2, w_in, w_out,
              na_rpb, hg_lb_logits, hg_norm, w_router, w_gate, w_up, w_down):
    bsz = x.shape[0]
    silu_c = jax.nn.silu(c)
    silu_cc = jax.nn.silu(c_ctx)
    lb_all = jnp.cumsum(jax.nn.softmax(hg_lb_logits.astype(jnp.float32), axis=0), axis=0)
    for l in range(DEPTH):
        mod = silu_c @ w_mod[l] + b_mod[l]
        mod_c = silu_cc @ w_mod[l] + b_mod[l]
        sh1, sc1, gt1, sh2, sc2, gt2 = jnp.split(mod[:, None, :], 6, axis=-1)
        sh1c, sc1c, gt1c, sh2c, sc2c, gt2c = jnp.split(mod_c, 6)
        lb = lb_all[l]

        h = modulate(rms_norm(x, g_pre1[l]), sh1, sc1)
        hc = modulate(rms_norm(ctx, g_pre1[l]), sh1c, sc1c)
        qn, kn, vn, qh, zf, zb, ih, gh = split_cols(h @ w_in[l])
        qn_c, kn_c, vn_c, qh_c, zf_c, zb_c, ih_c, gh_c = split_cols(hc @ w_in[l])
        kn_ch = to_heads(kn_c, N_NA_HEADS)
        vn_ch = to_heads(vn_c, N_NA_HEADS)

        na_o = neighbourhood_attention(to_heads(qn, N_NA_HEADS), to_heads(kn, N_NA_HEADS),
                                       to_heads(vn, N_NA_HEADS), kn_ch, vn_ch, na_rpb[l])

        s0 = jnp.zeros((bsz, N_HG_HEADS, HG_DK, HG_DV), jnp.float32)
        o_hc, s_cf, s_cb = hgrn2_bidir(to_heads(qh_c, N_HG_HEADS), to_heads(zf_c, N_HG_HEADS),
                                       to_heads(zb_c, N_HG_HEADS), to_heads(ih_c, N_HG_HEADS), lb, s0, s0)
        o_hl, _, _ = hgrn2_bidir(to_heads(qh, N_HG_HEADS), to_heads(zf, N_HG_HEADS),
                                 to_heads(zb, N_HG_HEADS), to_heads(ih, N_HG_HEADS), lb, s_cf, s_cb)
        hg_o = hgrn2_readout(o_hl, to_heads(gh, N_HG_HEADS), hg_norm[l])

        mix = jnp.concatenate([na_o, hg_o], axis=-1) @ w_out[l]
        x = x + gt1 * rms_norm(mix, g_post1[l])

        h2 = modulate(rms_norm(x, g_pre2[l]), sh2, sc2)
        x = x + gt2 * rms_norm(ec_moe(h2, w_router[l], w_gate[l], w_up[l], w_down[l]), g_post2[l])

        if l < DEPTH - 1:
            o_cna = context_attention(to_heads(qn_c, N_NA_HEADS), kn_ch, vn_ch)
            hg_oc = hgrn2_readout(o_hc, to_heads(gh_c, N_HG_HEADS), hg_norm[l])
            mix_c = jnp.concatenate([from_heads(o_cna), hg_oc], axis=-1) @ w_out[l]
            ctx = ctx + gt1c * rms_norm(mix_c, g_post1[l])
            hc2 = modulate(rms_norm(ctx, g_pre2[l]), sh2c, sc2c)
            ctx = ctx + gt2c * rms_norm(ec_moe(hc2, w_router[l], w_gate[l], w_up[l], w_down[l]), g_post2[l])
    return x
```

```python
import numpy as np
from contextlib import ExitStack
import concourse.bass as bass
import concourse.mybir as mybir
from concourse.bass_utils import run_bass_kernel_spmd

F32 = mybir.dt.float32
BF16 = mybir.dt.bfloat16
I32 = mybir.dt.int32
ALU = mybir.AluOpType
AF = mybir.ActivationFunctionType
AX = mybir.AxisListType

D = 1024
KC = 8
NTO = 16
NTH = 20
OWN0 = 2
TOK = 2048
HTOK = 2560
CTX = 256
DE = 2752
NFC = 22
EPS = 1e-6
GROUPS = [[0, 1, 2, 3], [4, 5, 6, 7]]
NDS = 24
NEG = -30000.0


class _Stop(Exception):
    pass


class V:
    def __init__(s, ap, key):
        s.ap = ap
        s.key = key

    def __getitem__(s, idx):
        return V(s.ap[idx], s.key)

    def sub(s, k):
        return V(s.ap, s.key + "/" + str(k))

    def re(s, pat, **kw):
        return V(s.ap.rearrange(pat, **kw), s.key)

    def bc(s, shape):
        return V(s.ap.to_broadcast(shape), s.key)


def _ap(x):
    return x.ap if isinstance(x, V) else x


def _keys(*xs):
    return [x.key for x in xs if isinstance(x, V)]


def _ovl(a, b):
    return a == b or a.startswith(b + "/") or b.startswith(a + "/")


class Prog:
    def __init__(s, nc, st):
        s.nc = nc
        s.E = {"pe": nc.tensor, "act": nc.scalar, "dve": nc.vector, "pool": nc.gpsimd, "sp": nc.sync}
        s.sems = []
        s.esem = {}
        s.ecnt = {}
        for e in ["pe", "act", "dve", "pool"]:
            s.esem[e] = s._new(st, "s_" + e)
            s.ecnt[e] = 0
        s.dq = {}
        for q in ["sp", "pool"]:
            s.dq[q] = {"sems": [s._new(st, "d_%s%d" % (q, i)) for i in range(NDS)], "use": [0] * NDS, "nxt": 0}
        s.ccsem = s._new(st, "ccs")
        s.cccnt = 0
        s.waited = {e: {} for e in s.E}
        s.bufs = {}
        s.alltok = {}
        s.halt = False

    def _new(s, st, name):
        s.sems.append(st.enter_context(s.nc.semaphore(name)))
        return len(s.sems) - 1

    def _collect(s, reads, writes):
        t = {}

        def add(d):
            for k, v in d.items():
                if t.get(k, 0) < v:
                    t[k] = v
        for k in reads:
            for k2, stt in s.bufs.get(k.split("/")[0], {}).items():
                if _ovl(k, k2):
                    add(stt["w"])
        for k in writes:
            for k2, stt in s.bufs.get(k.split("/")[0], {}).items():
                if _ovl(k, k2):
                    add(stt["w"])
                    add(stt["r"])
        return t

    def _update(s, reads, writes, tok):
        for k in reads:
            stt = s.bufs.setdefault(k.split("/")[0], {}).setdefault(k, {"w": {}, "r": {}})
            if stt["r"].get(tok[0], 0) < tok[1]:
                stt["r"][tok[0]] = tok[1]
        for k in writes:
            d = s.bufs.setdefault(k.split("/")[0], {})
            for k2 in list(d):
                if k2 != k and k2.startswith(k + "/"):
                    del d[k2]
            d[k] = {"w": {tok[0]: tok[1]}, "r": {}}
        if s.alltok.get(tok[0], 0) < tok[1]:
            s.alltok[tok[0]] = tok[1]

    def _wait(s, eng, toks, skip=None):
        e = s.E[eng]
        for sm, v in toks.items():
            if sm == skip:
                continue
            if s.waited[eng].get(sm, 0) < v:
                e.wait_ge(s.sems[sm], v)
                s.waited[eng][sm] = v

    def op(s, eng, fn, reads=(), writes=()):
        if s.halt:
            return
        if eng != "pe":
            pr = [k for k in reads if k.startswith("PS")]
            if pr:
                writes = list(writes) + pr
        toks = s._collect(reads, writes)
        s._wait(eng, toks, skip=s.esem[eng] if eng == "pe" else None)
        ins = fn()
        s.ecnt[eng] += 1
        ins.then_inc(s.sems[s.esem[eng]], 1)
        s._update(reads, writes, (s.esem[eng], s.ecnt[eng]))

    def dma(s, q, fn, reads=(), writes=()):
        if s.halt:
            return
        dq = s.dq[q]
        i = dq["nxt"]
        dq["nxt"] = (i + 1) % NDS
        sm = dq["sems"][i]
        toks = s._collect(reads, writes)
        if dq["use"][i] > 0 and toks.get(sm, 0) < 16 * dq["use"][i]:
            toks[sm] = 16 * dq["use"][i]
        s._wait(q, toks)
        ins = fn()
        dq["use"][i] += 1
        ins.then_inc(s.sems[sm], 16)
        s._update(reads, writes, (sm, 16 * dq["use"][i]))

    def cc(s, kind, op, src, dst):
        if s.halt:
            return
        import os
        if os.environ.get("KNOCC", "") == "1" or (os.environ.get("KNOCC", "") == kind):
            n = min(src.ap.shape[0], dst.ap.shape[0])
            s.ld(dst[0:n, :], src[0:n, :])
            return
        toks = s._collect([src.key], [dst.key])
        s._wait("pool", toks)
        ins = s.nc.gpsimd.collective_compute(kind, op, replica_groups=GROUPS, ins=[src.ap.opt()], outs=[dst.ap.opt()])
        s.cccnt += 1
        ins.then_inc(s.sems[s.ccsem])
        s._update([src.key], [dst.key], (s.ccsem, s.cccnt))

    def barrier(s):
        if s.halt:
            return
        for eng in s.E:
            s._wait(eng, dict(s.alltok), skip=None)

    def mm(s, out, lhsT, rhs, start=True, stop=True):
        s.op("pe", lambda: s.nc.tensor.matmul(_ap(out), _ap(lhsT), _ap(rhs), start=start, stop=stop),
             _keys(lhsT, rhs), _keys(out))

    def tr(s, out, in_, ident):
        s.op("pe", lambda: s.nc.tensor.transpose(_ap(out), _ap(in_), _ap(ident)), _keys(in_, ident), _keys(out))

    def act(s, out, in_, func, bias=None, scale=1.0, accum=None):
        kw = {}
        if bias is not None:
            kw["bias"] = _ap(bias)
        if accum is not None:
            kw["accum_out"] = _ap(accum)
        s.op("act", lambda: s.nc.scalar.activation(out=_ap(out), in_=_ap(in_), func=func, scale=_ap(scale), **kw),
             _keys(in_, bias, scale), _keys(out, accum))

    def tt(s, out, a, b, op, eng="dve"):
        e = s.E[eng]
        s.op(eng, lambda: e.tensor_tensor(out=_ap(out), in0=_ap(a), in1=_ap(b), op=op), _keys(a, b), _keys(out))

    def ts(s, out, a, s1, s2, op0, op1=None, accum=None, eng="dve"):
        e = s.E[eng]
        kw = {}
        if accum is not None:
            kw["accum_out"] = _ap(accum)
        if op1 is None:
            s.op(eng, lambda: e.tensor_scalar(out=_ap(out), in0=_ap(a), scalar1=_ap(s1), scalar2=None, op0=op0, **kw),
                 _keys(a, s1), _keys(out, accum))
        else:
            s.op(eng, lambda: e.tensor_scalar(out=_ap(out), in0=_ap(a), scalar1=_ap(s1), scalar2=_ap(s2), op0=op0, op1=op1, **kw),
                 _keys(a, s1, s2), _keys(out, accum))

    def stt(s, out, a, sc, b, op0, op1, eng="dve"):
        e = s.E[eng]
        s.op(eng, lambda: e.scalar_tensor_tensor(out=_ap(out), in0=_ap(a), scalar=_ap(sc), in1=_ap(b), op0=op0, op1=op1),
             _keys(a, sc, b), _keys(out))

    def cp(s, out, in_, eng="dve"):
        e = s.E[eng]
        s.op(eng, lambda: e.tensor_copy(out=_ap(out), in_=_ap(in_)), _keys(in_), _keys(out))

    def memset(s, out, val, eng="pool"):
        e = s.E[eng]
        s.op(eng, lambda: e.memset(_ap(out), val), [], _keys(out))

    def rmax(s, out, in_):
        s.op("dve", lambda: s.nc.vector.reduce_max(out=_ap(out), in_=_ap(in_), axis=AX.X), _keys(in_), _keys(out))

    def recip(s, out, in_):
        s.op("dve", lambda: s.nc.vector.reciprocal(out=_ap(out), in_=_ap(in_)), _keys(in_), _keys(out))

    def scan(s, out, d0, d1, init, op0, op1):
        s.op("dve", lambda: s.nc.vector.tensor_tensor_scan(out=_ap(out), data0=_ap(d0), data1=_ap(d1), initial=init, op0=op0, op1=op1),
             _keys(d0, d1), _keys(out))

    def ld(s, out, in_, q="sp"):
        e = s.E[q]
        s.dma(q, lambda: e.dma_start(out=_ap(out), in_=_ap(in_)), _keys(in_), _keys(out))

    def gather(s, out, src, idx):
        s.dma("pool", lambda: s.nc.gpsimd.indirect_dma_start(
            out=_ap(out), out_offset=None, in_=_ap(src),
            in_offset=bass.IndirectOffsetOnAxis(ap=_ap(idx), axis=0)), _keys(src, idx), _keys(out))

    def scatter_add(s, dst, src, idx):
        s.dma("pool", lambda: s.nc.gpsimd.indirect_dma_start(
            out=_ap(dst), out_offset=bass.IndirectOffsetOnAxis(ap=_ap(idx), axis=0),
            in_=_ap(src), in_offset=None, compute_op=ALU.add), _keys(src, idx, dst), _keys(dst))


def build(debug=False):
    nc = bass.Bass("TRN2", target_bir_lowering=False)
    top = ExitStack()
    P = Prog(nc, top)

    import os
    KSTOP = os.environ.get("KSTOP", "")
    open_stacks = []

    def stop(tag):
        if KSTOP == tag:
            P.barrier()
            P.halt = True

    def din(name, shape, dt=F32):
        return V(nc.dram_tensor(name, list(shape), dt, kind="ExternalInput").ap(), name)

    def dscr(name, shape, dt=F32):
        return V(nc.dram_tensor(name, list(shape), dt).ap(), name)

    uq = [0]

    def sb(st, name, shape, dt=F32, side=None):
        uq[0] += 1
        name = "%s_%d" % (name, uq[0])
        return V(st.enter_context(nc.sbuf_tensor(name, list(shape), dt, side=side))[:], name)

    def ps(st, name, shape, dt=F32):
        uq[0] += 1
        name = "PS%s_%d" % (name, uq[0])
        return V(st.enter_context(nc.psum_tensor(name, list(shape), dt))[:], name)

    xh = din("xh", [HTOK, D])
    ctxb = din("ctxb", [CTX, D])
    crep = din("crep", [128, KC * 128])
    ccrep = din("ccrep", [128, KC * 128])
    wmod = din("wmod", [D, 6 * D])
    bmodb = din("bmodb", [128, 6 * D])
    g1b = din("g1b", [128, D])
    gp1b = din("gp1b", [128, D])
    g2b = din("g2b", [128, D])
    gp2b = din("gp2b", [128, D])
    winx = din("winx", [D, 5120])
    wout = din("wout", [D, D])
    cosT = din("cosT", [128, HTOK])
    sinT = din("sinT", [128, HTOK])
    masks = din("masks", [8, 128, 5 * 1024])
    lbl = din("lbl", [128, 8])
    hgnb = din("hgnb", [128, 512])
    wr = din("wr", [D, 16])
    mfold = din("mfold", [128, 8])
    wg4 = din("wg4", [4, D, DE])
    wu4 = din("wu4", [4, D, DE])
    wd4 = din("wd4", [4, DE, D])
    selM = din("selM", [64, 16])
    selO = din("selO", [64, 16])
    Gm = din("Gm", [64, 64])
    Gpre = din("Gpre", [64, 64])
    identf = din("identf", [128, 128])
    maskA = din("maskA", [128, 256])
    rowmask = din("rowmask", [128, 4])
    resetm = din("resetm", [128, TOK])
    iota1k = din("iota1k", [128, 1024])
    tokc = din("tokc", [128, 768])
    out = V(nc.dram_tensor("out", [TOK, D], F32, kind="ExternalOutput").ap(), "out")
    if debug:
        dbg_x1 = V(nc.dram_tensor("dbg_x1", [TOK, D], F32, kind="ExternalOutput").ap(), "dbg_x1")
        dbg_mix = V(nc.dram_tensor("dbg_mix", [TOK, D], F32, kind="ExternalOutput").ap(), "dbg_mix")
        dbg_aff = V(nc.dram_tensor("dbg_aff", [16, TOK], F32, kind="ExternalOutput").ap(), "dbg_aff")
        dbg_idx = V(nc.dram_tensor("dbg_idx", [128, 64], F32, kind="ExternalOutput").ap(), "dbg_idx")

    na_d = dscr("na_d", [TOK, 512], BF16)
    sg_d = dscr("sg_d", [TOK, 512], BF16)
    x1_d = dscr("x1_d", [TOK, D])
    h2_in = dscr("h2_in", [TOK, D], BF16)
    h2_all = dscr("h2_all", [4 * TOK, D], BF16)
    st_in = dscr("st_in", [1032, 128])
    st_all = dscr("st_all", [4 * 1032, 128])
    af_in = dscr("af_in", [16, TOK])
    af_all = dscr("af_all", [64, TOK])
    acc = dscr("acc", [4 * TOK, D])
    rs_out = dscr("rs_out", [TOK, D])

    try:
        idf = sb(top, "idf", [128, 128])
        idb = sb(top, "idb", [128, 128], BF16)
        mst = ExitStack()
        gt1g = sb(top, "gt1g", [128, D])
        a2 = sb(top, "a2", [128, D])
        sh2 = sb(top, "sh2", [128, D])
        gt2g = sb(top, "gt2g", [128, D])
        cols = sb(top, "cols", [128, 32])
        zero4k = sb(top, "zero4k", [128, D])
        epsc = sb(top, "epsc", [128, 1])
        modB = sb(mst, "modB", [128, 6 * D])
        P.memset(epsc, EPS)
        P.ld(idf, identf)
        P.cp(idb, idf)
        P.memset(zero4k, 0.0)
        accv = acc.re("(n p) d -> n p d", p=128)
        for n in range(64):
            P.ld(accv[n], zero4k)

        with ExitStack() as st:
            sc_ = sb(st, "siluc", [128, KC * 128])
            scc = sb(st, "silucc", [128, KC * 128])
            modC = sb(st, "modC", [128, 2 * D])
            wmb = [sb(st, "wmb%d" % i, [128, KC, 512]) for i in range(2)]
            bmb = [sb(st, "bmb%d" % i, [128, 512]) for i in range(2)]
            pm = [ps(st, "pm%d" % i, [128, 512]) for i in range(2)]
            tmpb = sb(st, "tmpb", [128, D])
            ptr = ps(st, "ptr", [128, 128])
            P.ld(sc_, crep)
            P.ld(scc, ccrep)
            P.act(sc_, sc_, AF.Silu)
            P.act(scc, scc, AF.Silu)
            wmv = wmod.re("(kc p) n -> p kc n", p=128)
            for nb in range(12):
                wb = wmb[nb % 2]
                P.ld(wb, wmv[:, :, nb * 512:(nb + 1) * 512])
                P.ld(bmb[nb % 2], bmodb[:, nb * 512:(nb + 1) * 512])
                for kc in range(KC):
                    P.mm(pm[0], sc_[:, kc * 128:(kc + 1) * 128], wb[:, kc, :], start=(kc == 0), stop=(kc == KC - 1))
                P.tt(modB[:, nb * 512:(nb + 1) * 512], pm[0], bmb[nb % 2], ALU.add)
                if nb < 4:
                    for kc in range(KC):
                        P.mm(pm[1], scc[:, kc * 128:(kc + 1) * 128], wb[:, kc, :], start=(kc == 0), stop=(kc == KC - 1))
                    P.tt(modC[:, nb * 512:(nb + 1) * 512], pm[1], bmb[nb % 2], ALU.add)
            g1t = sb(st, "g1t", [128, D])
            P.ld(g1t, g1b)
            for (src_sh, src_sc, c0) in ((modB[:, 0:D], modB[:, D:2 * D], 0), (modC[:, 0:D], modC[:, D:2 * D], 16)):
                P.stt(tmpb, src_sc, 1.0, g1t, ALU.add, ALU.mult)
                for kc in range(KC):
                    P.tr(ptr, tmpb[:, kc * 128:(kc + 1) * 128], idf)
                    P.cp(cols[:, c0 + kc:c0 + kc + 1], ptr[:, 0:1])
                    P.tr(ptr, src_sh[:, kc * 128:(kc + 1) * 128], idf)
                    P.cp(cols[:, c0 + 8 + kc:c0 + 8 + kc + 1], ptr[:, 0:1])
            P.ld(tmpb, gp1b)
            P.tt(gt1g, modB[:, 2 * D:3 * D], tmpb, ALU.mult)
            P.ld(tmpb, g2b)
            P.stt(a2, modB[:, 4 * D:5 * D], 1.0, tmpb, ALU.add, ALU.mult)
            P.cp(sh2, modB[:, 3 * D:4 * D])
            P.ld(tmpb, gp2b)
            P.tt(gt2g, modB[:, 5 * D:6 * D], tmpb, ALU.mult)
        P.barrier()
        mst.close()
        stop("a")

        s1 = ExitStack()
        open_stacks.append(s1)
        hT = sb(s1, "hT", [128, KC, HTOK], BF16, side="right")
        hcT = sb(s1, "hcT", [128, KC, CTX], BF16, side="right")
        with ExitStack() as st:
            xt = [sb(st, "xt%d" % i, [128, D]) for i in range(2)]
            xs = [sb(st, "xs%d" % i, [128, D], BF16) for i in range(2)]
            junk = sb(st, "junk", [128, D])
            ss = sb(st, "ss", [128, 2])
            pt = [ps(st, "pt%d" % i, [128, D], BF16) for i in range(2)]
            xhv = xh.re("(n p) d -> n p d", p=128)
            cxv = ctxb.re("(n p) d -> n p d", p=128)
            for i in range(NTH + 2):
                isctx = i >= NTH
                src = cxv[i - NTH] if isctx else xhv[i]
                x_ = xt[i % 2]
                P.ld(x_, src)
                P.act(junk, x_, AF.Square, accum=ss[:, 0:1])
                P.act(ss[:, 1:2], ss[:, 0:1], AF.Sqrt, bias=epsc[:, 0:1], scale=1.0 / D)
                P.recip(ss[:, 1:2], ss[:, 1:2])
                P.ts(xs[i % 2], x_, ss[:, 1:2], None, ALU.mult)
                for kc in range(KC):
                    P.tr(pt[i % 2][:, kc * 128:(kc + 1) * 128], xs[i % 2][:, kc * 128:(kc + 1) * 128], idb)
                c0 = 16 if isctx else 0
                for kc in range(KC):
                    dst = hcT[:, kc, (i - NTH) * 128:(i - NTH + 1) * 128] if isctx else hT[:, kc, i * 128:(i + 1) * 128]
                    if kc % 2 == 0:
                        P.act(dst, pt[i % 2][:, kc * 128:(kc + 1) * 128], AF.Identity,
                              bias=cols[:, c0 + 8 + kc:c0 + 9 + kc], scale=cols[:, c0 + kc:c0 + kc + 1])
                    else:
                        P.ts(dst, pt[i % 2][:, kc * 128:(kc + 1) * 128], cols[:, c0 + kc:c0 + kc + 1],
                             cols[:, c0 + 8 + kc:c0 + 9 + kc], ALU.mult, ALU.add)
        P.barrier()
        stop("b")

        winv = winx.re("(kc p) n -> p kc n", p=128)

        def load_w(st_w, wst, wbf, blk, ncols=512, col0=None):
            c0 = blk * 512 if col0 is None else col0
            for kc in range(KC):
                P.ld(wbf[:, kc, 0:ncols], winv[:, kc, c0:c0 + ncols], q="pool")

        with ExitStack() as st:
            na_tok = sb(st, "na_tok", [128, NTO, 512], BF16)
            for half in range(2):
                with ExitStack() as sth:
                    qT = sb(sth, "qT", [128, 2, TOK], BF16)
                    qrT = sb(sth, "qrT", [128, 2, TOK], BF16)
                    krT = sb(sth, "krT", [128, 2, HTOK], BF16)
                    vtk = sb(sth, "vtk", [128, NTH, 256], BF16)
                    kcT = sb(sth, "kcT", [128, 2, CTX], BF16)
                    vck = sb(sth, "vck", [128, 2, 256], BF16)
                    with ExitStack() as st2:
                        wst = [sb(st2, "wst%d" % i, [128, 512]) for i in range(2)]
                        wA = sb(st2, "wA", [128, KC, 512], BF16)
                        wB = sb(st2, "wB", [128, KC, 512], BF16)
                        cs = sb(st2, "cs", [128, HTOK])
                        sn = sb(st2, "sn", [128, HTOK])
                        t1 = sb(st2, "t1", [128, 512])
                        t2 = sb(st2, "t2", [128, 512])
                        pa = [ps(st2, "pa%d" % i, [128, 512]) for i in range(2)]
                        pb = [ps(st2, "pb%d" % i, [128, 512]) for i in range(2)]
                        P.ld(cs, cosT)
                        P.ld(sn, sinT)
                        for (blk, pblk, ntok, tok0, dstp, dstr) in ((0, 8, TOK, OWN0 * 128, qT, qrT), (1, 9, HTOK, 0, None, krT)):
                            load_w(st2, wst, wA, blk, 256, blk * 512 + half * 256)
                            load_w(st2, wst, wB, pblk, 256, pblk * 512 + half * 256)
                            if half == 0 and blk == 0:
                                stop("c1a")
                            it = 0
                            for hp in range(2):
                                for nb in range(ntok // 512):
                                    a_, b_ = pa[it % 2], pb[it % 2]
                                    it += 1
                                    tk = tok0 + nb * 512
                                    for kc in range(KC):
                                        P.mm(a_, wA[:, kc, hp * 128:(hp + 1) * 128], hT[:, kc, tk:tk + 512], start=(kc == 0), stop=(kc == KC - 1))
                                    for kc in range(KC):
                                        P.mm(b_, wB[:, kc, hp * 128:(hp + 1) * 128], hT[:, kc, tk:tk + 512], start=(kc == 0), stop=(kc == KC - 1))
                                    KV = os.environ.get("KVAR", "")
                                    if dstp is not None and KV not in ("1", "3"):
                                        P.act(dstp[:, hp, nb * 512:(nb + 1) * 512], a_, AF.Copy)
                                    if KV not in ("2", "3"):
                                        P.tt(t1, a_, cs[:, tk:tk + 512], ALU.mult)
                                        P.tt(t2, b_, sn[:, tk:tk + 512], ALU.mult)
                                        P.tt(dstr[:, hp, nb * 512:(nb + 1) * 512], t1, t2, ALU.add)
                            if half == 0 and blk == 0:
                                stop("c1b")
                            if half == 0 and blk == 1:
                                stop("c1c")
                        for hp in range(2):
                            for kc in range(KC):
                                P.mm(pa[0][:, 0:CTX], wA[:, kc, hp * 128:(hp + 1) * 128], hcT[:, kc, :], start=(kc == 0), stop=(kc == KC - 1))
                            P.cp(kcT[:, hp, :], pa[0][:, 0:CTX])
                        load_w(st2, wst, wA, 2, 256, 2 * 512 + half * 256)
                        for i in range(NTH + 2):
                            a_ = pa[i % 2]
                            for kc in range(KC):
                                lhs = hcT[:, kc, (i - NTH) * 128:(i - NTH + 1) * 128] if i >= NTH else hT[:, kc, i * 128:(i + 1) * 128]
                                P.mm(a_[:, 0:256], lhs, wA[:, kc, 0:256], start=(kc == 0), stop=(kc == KC - 1))
                            dst = vck[:, i - NTH, :] if i >= NTH else vtk[:, i, :]
                            if i % 2 == 0:
                                P.act(dst, a_[:, 0:256], AF.Copy)
                            else:
                                P.cp(dst, a_[:, 0:256])
                    P.barrier()
                    if half == 0:
                        stop("c1")
                    with ExitStack() as st2:
                        mk = [sb(st2, "mk%d" % i, [128, 5, 1024]) for i in range(2)]
                        scb = [sb(st2, "scb%d" % i, [128, 1024]) for i in range(2)]
                        pb_ = [sb(st2, "pbf%d" % i, [128, 1024], BF16) for i in range(2)]
                        ptb = [sb(st2, "ptb%d" % i, [128, 8, 128], BF16) for i in range(2)]
                        sm = sb(st2, "smx", [128, 8])
                        psS = [ps(st2, "psS%d" % i, [128, 1024]) for i in range(2)]
                        psT = [ps(st2, "psT%d" % i, [128, 1024], BF16) for i in range(2)]
                        psO = [ps(st2, "psO%d" % i, [128, 64]) for i in range(2)]
                        smd = [sb(st2, "smd%d" % i, [128, 8]) for i in range(2)]

                        def geom(rp):
                            cls = 0 if rp == 0 else 1 if rp == 1 else 3 if rp == 14 else 4 if rp == 15 else 2
                            brow = 0 if rp <= 1 else 28 if rp >= 14 else 2 * rp
                            return cls, brow * 64

                        def stA1(hl, rp, j):
                            hp, hf = hl // 2, (hl % 2) * 64
                            mkh = mk[hl % 2]
                            cls, k0 = geom(rp)
                            S_ = psS[j]
                            sm_ = smd[j]
                            qsl = slice(rp * 128, (rp + 1) * 128)
                            P.mm(S_[:, 0:256], qT[hf:hf + 64, hp, qsl], kcT[hf:hf + 64, hp, :])
                            P.mm(S_[:, 256:512], qrT[hf:hf + 64, hp, qsl], krT[hf:hf + 64, hp, k0:k0 + 256])
                            P.mm(S_[:, 512:1024], qrT[hf:hf + 64, hp, qsl], krT[hf:hf + 64, hp, k0 + 256:k0 + 768])
                            P.stt(scb[j], S_, 0.125, mkh[:, cls, :], ALU.mult, ALU.add)
                            P.rmax(sm_[:, 0:1], scb[j])
                            P.ts(sm_[:, 1:2], sm_[:, 0:1], -1.0, None, ALU.mult)

                        def stA2(hl, rp, j):
                            sm_ = smd[j]
                            P.act(pb_[j], scb[j], AF.Exp, bias=sm_[:, 1:2], accum=sm_[:, 2:3])

                        def stB1(hl, rp, j):
                            for c in range(8):
                                P.tr(psT[j][:, c * 128:(c + 1) * 128], pb_[j][:, c * 128:(c + 1) * 128], idb)
                            P.act(ptb[j].re("p c n -> p (c n)"), psT[j], AF.Copy)

                        def stB2(hl, rp, j):
                            h = half * 4 + hl
                            cls, k0 = geom(rp)
                            sm_ = smd[j]
                            t0 = k0 // 128
                            for c in range(8):
                                rhs = vck[:, c, hl * 64:(hl + 1) * 64] if c < 2 else vtk[:, t0 + c - 2, hl * 64:(hl + 1) * 64]
                                P.mm(psO[j], ptb[j][:, c, :], rhs, start=(c == 0), stop=(c == 7))
                            P.recip(sm_[:, 3:4], sm_[:, 2:3])
                            P.ts(na_tok[:, rp, h * 64:(h + 1) * 64], psO[j], sm_[:, 3:4], None, ALU.mult)

                        seq = [(hl, rp) for hl in range(4) for rp in range(NTO)]
                        for n_ in range(len(seq) + 1):
                            nxt = seq[n_] if n_ < len(seq) else None
                            cur = seq[n_ - 1] if n_ >= 1 else None
                            if nxt is not None:
                                if nxt[1] == 0:
                                    P.ld(mk[nxt[0] % 2].re("p c n -> p (c n)"), masks[half * 4 + nxt[0]])
                                stA1(nxt[0], nxt[1], n_ % 2)
                            if cur is not None:
                                stB1(cur[0], cur[1], (n_ - 1) % 2)
                            if nxt is not None:
                                stA2(nxt[0], nxt[1], n_ % 2)
                            if cur is not None:
                                stB2(cur[0], cur[1], (n_ - 1) % 2)
                    P.barrier()
            for i_ in range(NTO):
                P.ld(na_d[i_ * 128:(i_ + 1) * 128, :], na_tok[:, i_, :])
        P.barrier()
        stop("c")
        hg = ExitStack()
        open_stacks.append(hg)
        o0 = sb(hg, "o0", [128, NTO, 512], BF16)
        qseg = sb(hg, "qseg", [128, 8, TOK], BF16)
        Send = sb(hg, "Send", [128, 8, 128])
        Sctx = sb(hg, "Sctx", [128, 8, 128])
        Dtot = sb(hg, "Dtot", [128, 8])
        lbt = sb(hg, "lbt", [128, 16])
        lbn = sb(hg, "lbn", [128, 4])
        ones64 = sb(hg, "ones64", [128, 64])
        rmk = sb(hg, "rmk", [128, 4])
        mAt = sb(hg, "mAt", [128, 256])
        mAb = sb(hg, "mAb", [128, 256], BF16)
        P.ld(lbt[:, 0:8], lbl)
        P.ld(rmk, rowmask)
        P.ld(mAt, maskA)
        P.cp(mAb, mAt)
        P.memset(ones64, 1.0)
        P.tt(lbt[:, 8:12], lbt[:, 0:4], lbt[:, 4:8], ALU.subtract)
        P.act(lbt[:, 12:16], lbt[:, 8:12], AF.Sigmoid, scale=-1.0)
        P.act(lbt[:, 8:12], lbt[:, 8:12], AF.Sigmoid)
        P.ts(lbn, lbt[:, 12:16], -1.0, None, ALU.mult)
        with ExitStack() as st:
            ih = sb(st, "ih", [128, NTO, 512], BF16)
            ihc = sb(st, "ihc", [128, 2, 512], BF16)
            wst = [sb(st, "wst%d" % i, [128, 512]) for i in range(2)]
            rsm = sb(st, "rsm", [128, TOK])
            P.ld(rsm, resetm)
            hTo = hT[:, :, OWN0 * 128:OWN0 * 128 + TOK]
            with ExitStack() as st2:
                pa = [ps(st2, "pa%d" % i, [128, 512]) for i in range(2)]
                sgt = [sb(st2, "sgt%d" % i, [128, 512], BF16) for i in range(2)]
                wA = sb(st2, "wA", [128, KC, 512], BF16)
                for (blk, dstt, dstc, fn) in ((6, ih, ihc, AF.Copy), (7, None, None, AF.Sigmoid)):
                    load_w(st2, wst, wA, blk)
                    for i in range(NTO + (2 if dstc is not None else 0)):
                        a_ = pa[i % 2]
                        for kc in range(KC):
                            lhs = hcT[:, kc, (i - NTO) * 128:(i - NTO + 1) * 128] if i >= NTO else hTo[:, kc, i * 128:(i + 1) * 128]
                            P.mm(a_, lhs, wA[:, kc, :], start=(kc == 0), stop=(kc == KC - 1))
                        if dstt is None:
                            P.act(sgt[i % 2], a_, fn)
                            P.ld(sg_d[i * 128:(i + 1) * 128, :], sgt[i % 2])
                        else:
                            P.act(dstc[:, i - NTO, :] if i >= NTO else dstt[:, i, :], a_, fn)
            P.barrier()
            with ExitStack() as st2:
                wz = sb(st2, "wz", [128, KC, 128], BF16)
                sg_ = sb(st2, "s_", [128, TOK])
                binc = sb(st2, "binc", [128, TOK])
                qhh = sb(st2, "qhh", [128, TOK], BF16)
                wq = sb(st2, "wq", [128, KC, 128], BF16)
                kk = sb(st2, "kk", [128, TOK], BF16)
                kdec = sb(st2, "kdec", [128, TOK], BF16)
                kend = sb(st2, "kend", [128, TOK], BF16)
                qdec = sb(st2, "qdec", [128, TOK], BF16)
                sm = sb(st2, "hsm", [128, 4, 64])
                S = sb(st2, "S", [128, 128])
                tot = sb(st2, "tot", [128, 64])
                Sb = [sb(st2, "Sb%d" % i, [128, 4, 128], BF16) for i in range(2)]
                QM = [sb(st2, "QM%d" % i, [128, 640], BF16) for i in range(2)]
                kendT = [sb(st2, "kendT%d" % i, [128, 128], BF16) for i in range(2)]
                vm = [sb(st2, "vm%d" % i, [128, 4, 128], BF16) for i in range(2)]
                Am = [sb(st2, "Am%d" % i, [128, 128], BF16) for i in range(2)]
                pz = [ps(st2, "pz%d" % i, [128, 512]) for i in range(2)]
                pT = ps(st2, "pT", [128, 1024], BF16)
                pKV = [ps(st2, "pKV%d" % i, [128, 4, 128]) for i in range(2)]
                pA = ps(st2, "pA", [128, 128])
                pO = [ps(st2, "pO%d" % i, [128, 128]) for i in range(2)]
                P.memset(QM[0], 0.0)
                P.memset(QM[1], 0.0)
                for isctx in (True, False):
                    ntok = CTX if isctx else TOK
                    nt = ntok // 128
                    nch = ntok // 32
                    hsrc = hcT if isctx else hTo
                    vsrc = ihc if isctx else ih
                    for d in range(2):
                        for h in range(4):
                            k = d * 4 + h
                            load_w(st2, wst, wz, None, 128, (4 + d) * 512 + h * 128)
                            nbs = [(0, 256)] if isctx else [(i * 512, 512) for i in range(4)]
                            for bi, (c0, cn) in enumerate(nbs):
                                a_ = pz[bi % 2]
                                for kc in range(KC):
                                    P.mm(a_[:, 0:cn], wz[:, kc, :], hsrc[:, kc, c0:c0 + cn], start=(kc == 0), stop=(kc == KC - 1))
                                P.act(sg_[:, c0:c0 + cn], a_[:, 0:cn], AF.Sigmoid)
                            sv = sg_[:, 0:ntok]
                            P.ts(kk[:, 0:ntok], sv, lbn[:, h:h + 1], lbt[:, 12 + h:13 + h], ALU.mult, ALU.add)
                            P.act(sv, sv, AF.Ln, bias=lbt[:, 8 + h:9 + h], scale=lbt[:, 12 + h:13 + h])
                            P.scan(binc[:, 0:ntok], rsm[:, 0:ntok], sv, 0.0, ALU.mult, ALU.add)
                            b3 = binc[:, 0:ntok].re("p (c t) -> p c t", t=32)
                            bend = b3[:, :, 31]
                            P.cp(tot[:, 0:nch], bend)
                            bend = tot[:, 0:nch]
                            B = binc[:, 0:ntok]
                            if d == 1:
                                P.tt(B, sv, B, ALU.subtract)
                                P.tt(b3, b3, bend.re("p (c o) -> p c o", o=1).bc([128, nch, 32]), ALU.add)
                            Ee = sg_
                            P.act(sm[:, 2, 0:nch], bend, AF.Exp)
                            P.scan(sm[:, 0, 0:nch], ones64[:, 0:nch], bend, 0.0, ALU.mult, ALU.add)
                            if d == 0:
                                P.tt(sm[:, 1, 0:nch], sm[:, 0, 0:nch], bend, ALU.subtract)
                            else:
                                P.ts(sm[:, 1, 0:nch], sm[:, 0, 0:nch], -1.0, sm[:, 0, nch - 1:nch], ALU.mult, ALU.add)
                            P.act(sm[:, 3, 0:nch], sm[:, 1, 0:nch], AF.Exp)
                            if not isctx:
                                P.act(Dtot[:, k:k + 1], sm[:, 0, nch - 1:nch], AF.Exp)
                                P.act(Ee[:, 0:ntok], B, AF.Exp)
                                load_w(st2, wst, wq, None, 128, 3 * 512 + h * 128)
                                for nb in range(4):
                                    a_ = pz[nb % 2]
                                    for kc in range(KC):
                                        P.mm(a_, wq[:, kc, :], hTo[:, kc, nb * 512:(nb + 1) * 512], start=(kc == 0), stop=(kc == KC - 1))
                                    P.cp(qhh[:, nb * 512:(nb + 1) * 512], a_)
                                P.tt(qdec[:, 0:ntok], qhh, Ee[:, 0:ntok], ALU.mult)
                                P.tt(qseg[:, k, :].re("p (c t) -> p c t", t=32), qdec.re("p (c t) -> p c t", t=32),
                                     sm[:, 3, 0:nch].re("p (c o) -> p c o", o=1).bc([128, nch, 32]), ALU.mult)
                            P.act(Ee[:, 0:ntok], B, AF.Exp, scale=-1.0)
                            P.tt(kdec[:, 0:ntok], kk[:, 0:ntok], Ee[:, 0:ntok], ALU.mult)
                            P.tt(kend[:, 0:ntok].re("p (c t) -> p c t", t=32), kdec[:, 0:ntok].re("p (c t) -> p c t", t=32),
                                 sm[:, 2, 0:nch].re("p (c o) -> p c o", o=1).bc([128, nch, 32]), ALU.mult)
                            P.memset(S, 0.0, eng="dve")
                            tiles = list(range(nt)) if d == 0 else list(range(nt - 1, -1, -1))
                            chs = [0, 1, 2, 3] if d == 0 else [3, 2, 1, 0]
                            for ti, i in enumerate(tiles):
                                j2 = ti % 2
                                tsl = slice(i * 128, (i + 1) * 128)
                                hc = slice(h * 128, (h + 1) * 128)
                                P.tr(pT[:, 0:128], kend[:, tsl], idb)
                                P.act(kendT[j2], pT[:, 0:128], AF.Copy)
                                P.tt(vm[j2], vsrc[:, i, hc].re("p (o v) -> p o v", o=1).bc([128, 4, 128]),
                                     rmk.re("p (j o) -> p j o", o=1).bc([128, 4, 128]), ALU.mult)
                                for j in range(4):
                                    P.mm(pKV[j2][:, j, :], kendT[j2], vm[j2][:, j, :])
                                if not isctx:
                                    P.mm(pA, kdec[:, tsl], qdec[:, tsl])
                                    P.tt(Am[j2], pA, mAb[:, d * 128:(d + 1) * 128], ALU.mult)
                                    P.cp(QM[j2].re("p (j x) -> p j x", x=160)[:, :, 0:32],
                                         qdec[:, tsl].re("p (j t) -> p j t", t=32), eng="pool")
                                for j in chs:
                                    if not isctx:
                                        P.act(Sb[j2][:, j, :], S, AF.Copy)
                                    P.stt(S, S, sm[:, 2, i * 4 + j:i * 4 + j + 1], pKV[j2][:, j, :], ALU.mult, ALU.add)
                                if not isctx:
                                    P.mm(pO[j2], Am[j2], vsrc[:, i, hc], start=True, stop=False)
                                    for j in range(4):
                                        P.mm(pO[j2], QM[j2][:, j * 128:(j + 1) * 128], Sb[j2][:, j, :], start=False, stop=(j == 3))
                                    if d == 0:
                                        P.cp(o0[:, i, hc], pO[j2])
                                    else:
                                        P.tt(o0[:, i, hc], o0[:, i, hc], pO[j2], ALU.add)
                            P.cp((Sctx if isctx else Send)[:, k, :], S)
        P.barrier()
        stop("d")
        s1.close()
        open_stacks.remove(s1)
        Ssb = sb(hg, "Ssb", [128, 8, 128], BF16)
        with ExitStack() as st:
            pD = ps(st, "pD", [8, 128])
            dT = sb(st, "dT", [8, 128])
            mf = sb(st, "mf", [128, 8])
            U = [sb(st, "U%d" % i, [128, 128]) for i in range(2)]
            Dj = [sb(st, "Dj%d" % i, [128, 4]) for i in range(2)]
            P.ld(mf, mfold)
            P.tr(pD, Dtot, idf)
            P.cp(dT, pD)
            for k in range(8):
                P.ld(st_in[k * 128:(k + 1) * 128, :], Send[:, k, :])
            P.ld(st_in[1024:1032, :], dT)
            P.cc("AllGather", ALU.bypass, st_in, st_all)
            it = 0
            for k in range(8):
                d = k // 4
                Sk = Sctx[:, k, :]
                for j in ([0, 1, 2, 3] if d == 0 else [3, 2, 1, 0]):
                    u_, d_ = U[it % 2], Dj[it % 2]
                    it += 1
                    P.ld(u_, st_all[j * 1032 + k * 128:j * 1032 + (k + 1) * 128, :])
                    P.ld(d_[:, 0:1], st_all[j * 1032 + 1024 + k:j * 1032 + 1025 + k, :].re("o d -> d o"))
                    m_ = mf[:, d * 4 + j:d * 4 + j + 1]
                    P.ts(d_[:, 1:2], d_[:, 0:1], -1.0, m_, ALU.add, ALU.mult)
                    P.ts(d_[:, 1:2], d_[:, 1:2], 1.0, None, ALU.add)
                    P.ts(u_, u_, m_, None, ALU.mult)
                    P.stt(Sk, Sk, d_[:, 1:2], u_, ALU.mult, ALU.add)
                P.cp(Ssb[:, k, :], Sk)
        P.barrier()
        stop("e")
        with ExitStack() as st:
            woutb = sb(st, "woutb", [128, KC, D], BF16)
            wst2 = [sb(st, "wst2%d" % i, [128, D]) for i in range(2)]
            wrs = sb(st, "wrs", [128, KC, 16])
            hgn = sb(st, "hgn", [128, 512])
            affT = sb(st, "affT", [16, TOK])
            ot = sb(st, "ot", [128, 512])
            hgt = sb(st, "hgt", [128, 512], BF16)
            nat = [sb(st, "nat%d" % i, [128, 512], BF16) for i in range(2)]
            sgl = [sb(st, "sgl%d" % i, [128, 512], BF16) for i in range(2)]
            xt = [sb(st, "xt%d" % i, [128, D]) for i in range(2)]
            x1t = [sb(st, "x1t%d" % i, [128, D]) for i in range(2)]
            h2t = [sb(st, "h2t%d" % i, [128, D]) for i in range(2)]
            h2b = [sb(st, "h2b%d" % i, [128, D], BF16) for i in range(2)]
            mixT = sb(st, "mixT", [128, KC, 128], BF16)
            h2T = sb(st, "h2T", [128, KC, 128])
            junk = sb(st, "junk2", [128, D])
            sm = sb(st, "rsm2", [128, 16])
            lg = sb(st, "lg", [128, 16])
            psC = ps(st, "psC", [128, 512])
            psT = ps(st, "psT", [128, 1024], BF16)
            psM = ps(st, "psM", [128, 1024])
            psT32 = ps(st, "psT32", [128, 1024])
            psL = ps(st, "psL", [128, 128])
            woutv = wout.re("(kc p) n -> p kc n", p=128)
            for kc in range(KC):
                P.ld(woutb[:, kc, :], woutv[:, kc, :], q="pool")
            P.ld(wrs, wr.re("(kc p) n -> p kc n", p=128))
            P.ld(hgn, hgnb)
            xhv = xh.re("(n p) d -> n p d", p=128)
            from functools import partial as F_
            from itertools import zip_longest
            smA = [sb(st, "smA%d" % i, [128, 8]) for i in range(2)]
            smB = [sb(st, "smB%d" % i, [128, 8]) for i in range(2)]
            smC = [sb(st, "smC%d" % i, [128, 8]) for i in range(2)]
            junkA = sb(st, "junkA", [128, 128])

            def S1(i):
                ops = []
                A = ops.append
                j2 = i % 2
                tsl = slice(i * 128, (i + 1) * 128)
                sm_ = smA[j2]
                A(F_(P.ld, nat[j2], na_d[tsl, :]))
                A(F_(P.ld, sgl[j2], sg_d[tsl, :]))
                A(F_(P.ld, xt[j2], xhv[i + OWN0]))
                for h in range(4):
                    A(F_(P.mm, psC[:, h * 128:(h + 1) * 128], qseg[:, h, tsl], Ssb[:, h, :], start=True, stop=False))
                    A(F_(P.mm, psC[:, h * 128:(h + 1) * 128], qseg[:, 4 + h, tsl], Ssb[:, 4 + h, :], start=False, stop=True))
                A(F_(P.tt, ot, o0[:, i, :], psC, ALU.add))
                for h in range(4):
                    A(F_(P.act, junkA, ot[:, h * 128:(h + 1) * 128], AF.Square, accum=sm_[:, h:h + 1]))
                A(F_(P.act, sm_[:, 4:8], sm_[:, 0:4], AF.Sqrt, bias=epsc[:, 0:1], scale=1.0 / 128))
                A(F_(P.recip, sm_[:, 4:8], sm_[:, 4:8]))
                o3 = ot.re("p (h v) -> p h v", v=128)
                A(F_(P.tt, o3, o3, sm_[:, 4:8].re("p (h o) -> p h o", o=1).bc([128, 4, 128]), ALU.mult))
                A(F_(P.tt, ot, ot, hgn, ALU.mult))
                A(F_(P.tt, hgt, ot, sgl[j2], ALU.mult))
                for c in range(4):
                    A(F_(P.tr, psT[:, c * 128:(c + 1) * 128], nat[j2][:, c * 128:(c + 1) * 128], idb))
                    A(F_(P.tr, psT[:, (4 + c) * 128:(5 + c) * 128], hgt[:, c * 128:(c + 1) * 128], idb))
                A(F_(P.act, mixT.re("p c n -> p (c n)"), psT, AF.Copy))
                for nb in range(2):
                    for mc in range(KC):
                        A(F_(P.mm, psM[:, nb * 512:(nb + 1) * 512], mixT[:, mc, :], woutb[:, mc, nb * 512:(nb + 1) * 512], start=(mc == 0), stop=(mc == KC - 1)))
                return ops

            def S2(i):
                ops = []
                A = ops.append
                j2 = i % 2
                tsl = slice(i * 128, (i + 1) * 128)
                sm_ = smB[j2]
                A(F_(P.act, junk, psM, AF.Square, accum=sm_[:, 0:1]))
                A(F_(P.act, sm_[:, 1:2], sm_[:, 0:1], AF.Sqrt, bias=epsc[:, 0:1], scale=1.0 / D))
                A(F_(P.recip, sm_[:, 1:2], sm_[:, 1:2]))
                A(F_(P.stt, x1t[j2], psM, sm_[:, 1:2], gt1g, ALU.mult, ALU.mult))
                A(F_(P.tt, x1t[j2], x1t[j2], xt[j2], ALU.add))
                A(F_(P.ld, x1_d[tsl, :], x1t[j2]))
                if debug:
                    A(F_(P.ld, dbg_x1[tsl, :], x1t[j2]))
                A(F_(P.act, junk, x1t[j2], AF.Square, accum=sm_[:, 2:3]))
                A(F_(P.act, sm_[:, 3:4], sm_[:, 2:3], AF.Sqrt, bias=epsc[:, 0:1], scale=1.0 / D))
                A(F_(P.recip, sm_[:, 3:4], sm_[:, 3:4]))
                A(F_(P.stt, h2t[j2], x1t[j2], sm_[:, 3:4], a2, ALU.mult, ALU.mult))
                A(F_(P.tt, h2t[j2], h2t[j2], sh2, ALU.add))
                A(F_(P.act, h2b[j2], h2t[j2], AF.Copy))
                A(F_(P.ld, h2_in[tsl, :], h2b[j2]))
                sm_ = smC[j2]
                for kc in range(KC):
                    A(F_(P.tr, psT32[:, kc * 128:(kc + 1) * 128], h2t[j2][:, kc * 128:(kc + 1) * 128], idf))
                A(F_(P.cp, h2T.re("p c n -> p (c n)"), psT32))
                for kc in range(KC):
                    A(F_(P.mm, psL[:, 0:16], h2T[:, kc, :], wrs[:, kc, :], start=(kc == 0), stop=(kc == KC - 1)))
                A(F_(P.rmax, sm_[:, 0:1], psL[:, 0:16]))
                A(F_(P.ts, sm_[:, 1:2], sm_[:, 0:1], -1.0, None, ALU.mult))
                A(F_(P.act, lg, psL[:, 0:16], AF.Exp, bias=sm_[:, 1:2], accum=sm_[:, 2:3]))
                A(F_(P.recip, sm_[:, 3:4], sm_[:, 2:3]))
                A(F_(P.ts, lg, lg, sm_[:, 3:4], None, ALU.mult))
                A(F_(P.tr, psL[0:16, :], lg, idf))
                A(F_(P.cp, affT[:, tsl], psL[0:16, :]))
                return ops

            for i in range(NTO + 1):
                oa = S1(i) if i < NTO else []
                ob = S2(i - 1) if i >= 1 else []
                for x_, y_ in zip_longest(oa, ob):
                    if x_ is not None:
                        x_()
                    if y_ is not None:
                        y_()
            P.ld(af_in, affT)
            if debug:
                P.ld(dbg_aff, affT)
        hg.close()
        open_stacks.remove(hg)
        stop("f")
        P.cc("AllGather", ALU.bypass, af_in, af_all)
        for ch in range(4):
            P.cc("AllGather", ALU.bypass, h2_in[ch * 512:(ch + 1) * 512, :], h2_all[ch * 2048:(ch + 1) * 2048, :])
        P.barrier()
        stop("1")
        rt = ExitStack()
        idx_i = sb(rt, "idx_i", [128, 4, 8], I32)
        idx_t = sb(rt, "idx_t", [128, 4, 8], I32)
        gate = sb(rt, "gate", [128, 4, 8])
        with ExitStack() as st:
            A = sb(st, "A", [64, TOK])
            selb = sb(st, "selb", [64, TOK])
            incl = sb(st, "incl", [64, TOK])
            ones = sb(st, "ones", [64, TOK])
            Gs = sb(st, "Gs", [64, 64])
            Gp = sb(st, "Gp", [64, 64])
            sM = sb(st, "sM", [64, 16])
            bs = sb(st, "bs", [64, 8])
            rkM = sb(st, "rkM", [128, 16, 16])
            afM = sb(st, "afM", [128, 16, 16])
            res = sb(st, "res", [128, 256])
            gb = sb(st, "gb", [128, 256], BF16)
            vals0 = sb(st, "vals0", [128, 768])
            VALS = sb(st, "VALS", [128, 256, 8], BF16)
            iot = sb(st, "iot", [128, 1024])
            oh = [sb(st, "oh%d" % i, [128, 1024], BF16) for i in range(2)]
            idxf = sb(st, "idxf", [128, 8, 8])
            tokf = sb(st, "tokf", [128, 8])
            P.ld(A, af_all)
            P.ld(Gs, Gm)
            P.ld(Gp, Gpre)
            P.ld(sM, selM)
            P.ld(iot, iota1k)
            P.ld(vals0, tokc)
            P.memset(ones, 1.0)
            P.memset(bs, 0.0)
            P.memset(bs[:, 1:2], 1.0)
            with ExitStack() as st2:
                psb = ps(st2, "psb", [64, 8])
                psr = [ps(st2, "psr%d" % i, [128, 16]) for i in range(2)]
                lo, hi, mid, cpart, cond, tmp = (bs[:, i:i + 1] for i in range(6))
                for it in range(32):
                    P.tt(mid, lo, hi, ALU.add)
                    P.ts(mid, mid, 0.5, None, ALU.mult)
                    P.ts(selb, A, mid, 0.0, ALU.is_gt, ALU.add, accum=cpart)
                    P.mm(psb[:, 0:1], Gs, cpart)
                    P.ts(cond, psb[:, 0:1], 1024.0, None, ALU.is_ge)
                    P.tt(tmp, mid, lo, ALU.subtract)
                    P.stt(lo, tmp, cond, lo, ALU.mult, ALU.add)
                    P.tt(tmp, hi, mid, ALU.subtract)
                    P.stt(hi, tmp, cond, mid, ALU.mult, ALU.add)
                P.ts(selb, A, lo, None, ALU.is_gt)
                P.scan(incl, ones, selb, 0.0, ALU.mult, ALU.add)
                P.mm(psb[:, 1:2], Gp, incl[:, TOK - 1:TOK])
                P.cp(tmp, psb[:, 1:2])
                P.stt(incl, incl, tmp, selb, ALU.add, ALU.mult)
                P.ts(incl, incl, -1.0, None, ALU.add)
                for j in range(16):
                    P.mm(psr[0], incl[:, j * 128:(j + 1) * 128], sM)
                    P.cp(rkM[:, j, :], psr[0])
                    P.mm(psr[1], A[:, j * 128:(j + 1) * 128], sM)
                    P.cp(afM[:, j, :], psr[1])
            af2 = afM.re("p j c -> p (j c)")
            V3 = VALS
            P.cp(V3[:, :, 0], vals0[:, 0:256])
            P.cp(V3[:, :, 1], vals0[:, 256:512])
            P.cp(gb, af2)
            P.cp(V3[:, :, 2], gb)
            P.tt(res, af2, gb, ALU.subtract)
            P.cp(gb, res)
            P.cp(V3[:, :, 3], gb)
            P.tt(res, res, gb, ALU.subtract)
            P.cp(V3[:, :, 4], res)
            P.cp(V3[:, :, 5], vals0[:, 512:768])
            P.memset(V3[:, :, 6:8], 0.0)
            with ExitStack() as st2:
                pI = [ps(st2, "pI%d" % g, [128, 512]) for g in range(8)]
                n = 0
                for i in range(4):
                    cnt = 0
                    for r in range(4):
                        for j in range(16):
                            col = r * 4 + i
                            o_ = oh[n % 2]
                            P.ts(o_, iot, rkM[:, j, col:col + 1], None, ALU.is_equal)
                            n += 1
                            for g in range(8):
                                P.mm(pI[g][:, 0:8], o_[:, g * 128:(g + 1) * 128], V3[:, j * 16 + col, :], start=(cnt == 0), stop=(cnt == 63))
                            cnt += 1
                    for g in range(8):
                        P.cp(idxf[:, g, :], pI[g][:, 0:8])
                    P.stt(tokf, idxf[:, :, 0], 128.0, idxf[:, :, 1], ALU.mult, ALU.add)
                    P.cp(idx_i[:, i, :], tokf)
                    P.stt(tokf, idxf[:, :, 5], 128.0, idxf[:, :, 1], ALU.mult, ALU.add)
                    P.cp(idx_t[:, i, :], tokf)
                    P.tt(gate[:, i, :], idxf[:, :, 2], idxf[:, :, 3], ALU.add)
                    P.tt(gate[:, i, :], gate[:, i, :], idxf[:, :, 4], ALU.add)
        if debug:
            dbt = sb(rt, "dbt", [128, 64])
            P.cp(dbt[:, 0:32], idx_t.re("p a b -> p (a b)"))
            P.cp(dbt[:, 32:64], gate.re("p a b -> p (a b)"))
            P.ld(dbg_idx, dbt)
        P.barrier()
        stop("r")
        with ExitStack() as st:
            xsT = sb(st, "xsT", [128, KC, 1024], BF16)
            hidT = sb(st, "hidT", [128, NFC, 1024], BF16)
            wdb = sb(st, "wdb", [128, NFC, D], BF16)
            wgb = [sb(st, "wgb%d" % i, [128, KC, 256], BF16) for i in range(2)]
            wub = [sb(st, "wub%d" % i, [128, KC, 256], BF16) for i in range(2)]
            xg = [sb(st, "xg%d" % i, [128, D], BF16) for i in range(2)]
            yt = [sb(st, "yt%d" % i, [128, D]) for i in range(2)]
            sil = [sb(st, "sil%d" % i, [128, 512]) for i in range(2)]
            pT = ps(st, "pTx", [128, 1024], BF16)
            pg = [ps(st, "pg%d" % i, [128, 512]) for i in range(2)]
            pu = [ps(st, "pu%d" % i, [128, 512]) for i in range(2)]
            py = [ps(st, "py%d" % i, [128, 512]) for i in range(2)]
            nq = 0
            for i in range(4):
                for g in range(8):
                    P.gather(xg[g % 2], h2_all, idx_i[:, i, g:g + 1])
                    for kc in range(KC):
                        P.tr(pT[:, kc * 128:(kc + 1) * 128], xg[g % 2][:, kc * 128:(kc + 1) * 128], idb)
                    P.act(xsT[:, :, g * 128:(g + 1) * 128], pT.re("p (c n) -> p c n", n=128), AF.Copy)
                wgv = wg4[i].re("(kc p) f -> p kc f", p=128)
                wuv = wu4[i].re("(kc p) f -> p kc f", p=128)
                for fb in range(11):
                    f0 = fb * 256
                    fn = min(256, DE - f0)
                    gb_, ub_ = wgb[fb % 2], wub[fb % 2]
                    P.ld(gb_[:, :, 0:fn], wgv[:, :, f0:f0 + fn], q="pool")
                    P.ld(ub_[:, :, 0:fn], wuv[:, :, f0:f0 + fn], q="pool")
                    for fc_ in (2 * fb, 2 * fb + 1):
                        m_ = 128 if fc_ < 21 else 64
                        P.ld(wdb[0:m_, fc_, :], wd4[i][fc_ * 128:fc_ * 128 + m_, :], q="pool")
                    for c in range((fn + 127) // 128):
                        fc = fb * 2 + c
                        m = min(128, fn - c * 128)
                        for half in range(2):
                            a_, b_ = pg[nq % 2], pu[nq % 2]
                            s_ = sil[nq % 2]
                            nq += 1
                            for kc in range(KC):
                                P.mm(a_[0:m, :], gb_[:, kc, c * 128:c * 128 + m], xsT[:, kc, half * 512:(half + 1) * 512], start=(kc == 0), stop=(kc == KC - 1))
                            for kc in range(KC):
                                P.mm(b_[0:m, :], ub_[:, kc, c * 128:c * 128 + m], xsT[:, kc, half * 512:(half + 1) * 512], start=(kc == 0), stop=(kc == KC - 1))
                            P.act(s_[0:m, :], a_[0:m, :], AF.Silu)
                            P.tt(hidT[0:m, fc, half * 512:(half + 1) * 512], s_[0:m, :], b_[0:m, :], ALU.mult)
                for ct in range(8):
                    y_ = yt[ct % 2]
                    for nb in range(2):
                        p_ = py[nb]
                        for fc in range(NFC):
                            m = 128 if fc < 21 else 64
                            P.mm(p_, hidT[0:m, fc, ct * 128:(ct + 1) * 128], wdb[0:m, fc, nb * 512:(nb + 1) * 512], start=(fc == 0), stop=(fc == NFC - 1))
                        P.ts(y_[:, nb * 512:(nb + 1) * 512], p_, gate[:, i, ct:ct + 1], None, ALU.mult)
                    P.scatter_add(acc, y_, idx_t[:, i, ct:ct + 1])
        rt.close()
        P.cc("ReduceScatter", ALU.add, acc, rs_out)
        with ExitStack() as st:
            mt = [sb(st, "mt%d" % i, [128, D]) for i in range(2)]
            x1l = [sb(st, "x1l%d" % i, [128, D]) for i in range(2)]
            junk = sb(st, "junk3", [128, D])
            sm = sb(st, "fsm", [128, 4])
            for i in range(NTO):
                j2 = i % 2
                tsl = slice(i * 128, (i + 1) * 128)
                P.ld(mt[j2], rs_out[tsl, :])
                P.ld(x1l[j2], x1_d[tsl, :])
                P.act(junk, mt[j2], AF.Square, accum=sm[:, 0:1])
                P.act(sm[:, 1:2], sm[:, 0:1], AF.Sqrt, bias=epsc[:, 0:1], scale=1.0 / D)
                P.recip(sm[:, 1:2], sm[:, 1:2])
                P.stt(mt[j2], mt[j2], sm[:, 1:2], gt2g, ALU.mult, ALU.mult)
                P.tt(mt[j2], mt[j2], x1l[j2], ALU.add)
                P.ld(out[tsl, :], mt[j2])
    except _Stop:
        pass
    for stx in reversed(open_stacks):
        stx.close()
    P.barrier()
    top.close()
    return nc


def _consts():
    f = np.float32
    p = np.arange(128)
    cst = {}
    cst["identf"] = np.eye(128, dtype=f)
    sidx, cidx = p[:, None], p[None, :]
    same = (sidx // 32) == (cidx // 32)
    cst["maskA"] = np.concatenate([(same & (cidx >= sidx)), (same & (cidx <= sidx))], axis=1).astype(f)
    cst["rowmask"] = ((p[:, None] // 32) == np.arange(4)[None, :]).astype(f)
    rm = np.ones((128, TOK), f)
    rm[:, 0::32] = 0.0
    cst["resetm"] = rm
    cst["iota1k"] = np.broadcast_to(np.arange(1024, dtype=f)[None, :], (128, 1024)).copy()
    tok = np.zeros((128, 768), f)
    for j in range(16):
        for col in range(16):
            r = col // 4
            tok[:, j * 16 + col] = (j // 4) * 16 + r * 4 + (j % 4)
            tok[:, 512 + j * 16 + col] = r * 16 + j
    tok[:, 256:512] = p[:, None].astype(f)
    cst["tokc"] = tok
    re_ = np.arange(64)
    r_, e_ = re_ // 16, re_ % 16
    cst["Gm"] = (e_[:, None] == e_[None, :]).astype(f)
    cst["Gpre"] = ((e_[:, None] == e_[None, :]) & (r_[:, None] < r_[None, :])).astype(f)
    return cst


def prep(x, c, ctx, c_ctx, w_mod, b_mod, g_pre1, g_post1, g_pre2, g_post2, w_in, w_out,
         na_rpb, hg_lb_logits, hg_norm, w_router, w_gate, w_up, w_down):
    f = np.float32
    A_ = lambda a: np.ascontiguousarray(np.asarray(a, dtype=f))
    x, c, ctx, c_ctx = A_(x), A_(c), A_(ctx), A_(c_ctx)
    w_mod, b_mod, w_in, w_out = A_(w_mod)[0], A_(b_mod)[0], A_(w_in)[0], A_(w_out)[0]
    rpb = A_(na_rpb)[0]
    lbl_ = A_(hg_lb_logits)
    hgn_ = A_(hg_norm)[0]
    wr_ = A_(w_router)[0]
    wg_, wu_, wd_ = np.asarray(w_gate)[0], np.asarray(w_up)[0], np.asarray(w_down)[0]
    bc = lambda v: np.ascontiguousarray(np.broadcast_to(v[None, :], (128, v.shape[0])))
    pp = np.arange(512)
    perm = np.where(pp % 32 < 16, pp + 16, pp - 16)
    winx = np.ascontiguousarray(np.concatenate([w_in, w_in[:, 0:512][:, perm], w_in[:, 512:1024][:, perm]], axis=1))
    cst = _consts()
    shared = dict(cst)
    shared.update(wmod=w_mod, bmodb=bc(b_mod), g1b=bc(A_(g_pre1)[0]), gp1b=bc(A_(g_post1)[0]),
                  g2b=bc(A_(g_pre2)[0]), gp2b=bc(A_(g_post2)[0]), winx=winx, wout=w_out,
                  hgnb=bc(np.tile(hgn_, 4)), wr=wr_,
                  lbl=np.ascontiguousarray(lbl_.reshape(2, 4, 128).transpose(2, 0, 1).reshape(128, 8)))
    ccrep = np.ascontiguousarray(np.broadcast_to(c_ctx.reshape(KC, 128).T[:, :, None], (128, KC, 128)).reshape(128, KC * 128))
    d64 = np.arange(128) % 64
    seg = d64 // 32
    first = (d64 % 32) < 16
    inv = (10000.0 ** (-(2.0 * (d64 % 16)) / 32.0)).astype(f)
    in_maps = []
    for core in range(8):
        b, s = core // 4, core % 4
        m = dict(shared)
        xh = np.zeros((HTOK, D), f)
        g0 = TOK * s - 256
        lo, hi = max(g0, 0), min(g0 + HTOK, 8192)
        xh[lo - g0:hi - g0] = x[b, lo:hi]
        m["xh"] = xh
        m["ctxb"] = ctx[b]
        m["crep"] = np.ascontiguousarray(np.broadcast_to(c[b].reshape(KC, 128).T[:, :, None], (128, KC, 128)).reshape(128, KC * 128))
        m["ccrep"] = ccrep
        tl = np.arange(HTOK)
        row = (32 * s - 4 + tl // 64).astype(f)
        colp = (tl % 64).astype(f)
        pos = np.where(seg[:, None] == 0, row[None, :], colp[None, :]).astype(f)
        ang = (pos * inv[:, None]).astype(f)
        m["cosT"] = np.cos(ang).astype(f)
        m["sinT"] = np.where(first[:, None], -np.sin(ang), np.sin(ang)).astype(f)
        mk = np.zeros((8, 128, 5, 1024), f)
        qp = np.arange(128)
        a_, qc = qp // 64, qp % 64
        nn = np.arange(768)
        for cls, rp in enumerate((0, 1, 4, 14, 15)):
            brow = 0 if rp <= 1 else 28 if rp >= 14 else 2 * rp
            r = 32 * s + 2 * rp + a_
            grow = 32 * s - 4 + brow + nn // 64
            kc_ = nn % 64
            r0 = np.clip(r - 4, 0, 120)
            vrow = (grow[None, :] >= r0[:, None]) & (grow[None, :] < r0[:, None] + 8)
            wc0 = np.clip(qc - 8, 0, 48)
            vcol = (kc_[None, :] >= wc0[:, None]) & (kc_[None, :] < wc0[:, None] + 16)
            dr = np.clip(grow[None, :] - r[:, None] + 7, 0, 14)
            dc = np.clip(kc_[None, :] - qc[:, None] + 15, 0, 30)
            bias = rpb[:, dr, dc]
            mk[:, :, cls, 256:1024] = np.where((vrow & vcol)[None], bias, f(NEG))
        m["masks"] = mk.reshape(8, 128, 5 * 1024)
        mf = np.zeros((128, 8), f)
        for j in range(4):
            mf[:, j] = 1.0 if j < s else 0.0
            mf[:, 4 + j] = 1.0 if j > s else 0.0
        m["mfold"] = mf
        sel = np.zeros((64, 16), f)
        for r in range(4):
            for i in range(4):
                sel[r * 16 + 4 * s + i, r * 4 + i] = 1.0
        m["selM"] = sel
        m["selO"] = np.zeros((64, 16), f)
        m["wg4"] = np.ascontiguousarray(wg_[4 * s:4 * s + 4], dtype=f)
        m["wu4"] = np.ascontiguousarray(wu_[4 * s:4 * s + 4], dtype=f)
        m["wd4"] = np.ascontiguousarray(wd_[4 * s:4 * s + 4], dtype=f)
        in_maps.append(m)
    return in_maps


def kernel(**inputs):
    f = np.float32
    in_maps = prep(**inputs)
    import os
    dbg = os.environ.get("KDEBUG", "") == "1"
    nc = build(debug=dbg)
    res = run_bass_kernel_spmd(nc, in_maps, core_ids=list(range(8)))
    if dbg:
        global LAST
        LAST = res.results
    out = np.zeros((2, 8192, D), f)
    for core in range(8):
        b, s = core // 4, core % 4
        out[b, TOK * s:TOK * (s + 1)] = res.results[core]["out"]
    return out
```

```python
import numpy as np
from contextlib import ExitStack
import concourse.bass as bass
import concourse.mybir as mybir
from concourse.bass_utils import run_bass_kernel_spmd

F32 = mybir.dt.float32
BF16 = mybir.dt.bfloat16
I32 = mybir.dt.int32
ALU = mybir.AluOpType
AF = mybir.ActivationFunctionType
AX = mybir.AxisListType

D = 1024
KC = 8
NTO = 16
NTH = 20
OWN0 = 2
TOK = 2048
HTOK = 2560
CTX = 256
DE = 2752
NFC = 22
EPS = 1e-6
GROUPS = [[0, 1, 2, 3], [4, 5, 6, 7]]
NDS = 24
NEG = -30000.0


class _Stop(Exception):
    pass


class V:
    def __init__(s, ap, key):
        s.ap = ap
        s.key = key

    def __getitem__(s, idx):
        return V(s.ap[idx], s.key)

    def sub(s, k):
        return V(s.ap, s.key + "/" + str(k))

    def re(s, pat, **kw):
        return V(s.ap.rearrange(pat, **kw), s.key)

    def bc(s, shape):
        return V(s.ap.to_broadcast(shape), s.key)


def _ap(x):
    return x.ap if isinstance(x, V) else x


def _keys(*xs):
    return [x.key for x in xs if isinstance(x, V)]


def _ovl(a, b):
    return a == b or a.startswith(b + "/") or b.startswith(a + "/")


class Prog:
    def __init__(s, nc, st):
        s.nc = nc
        s.E = {"pe": nc.tensor, "act": nc.scalar, "dve": nc.vector, "pool": nc.gpsimd, "sp": nc.sync}
        s.sems = []
        s.esem = {}
        s.ecnt = {}
        for e in ["pe", "act", "dve", "pool"]:
            s.esem[e] = s._new(st, "s_" + e)
            s.ecnt[e] = 0
        s.dq = {}
        for q in ["sp", "pool"]:
            s.dq[q] = {"sems": [s._new(st, "d_%s%d" % (q, i)) for i in range(NDS)], "use": [0] * NDS, "nxt": 0}
        s.ccsem = s._new(st, "ccs")
        s.cccnt = 0
        s.waited = {e: {} for e in s.E}
        s.bufs = {}
        s.alltok = {}
        s.halt = False

    def _new(s, st, name):
        s.sems.append(st.enter_context(s.nc.semaphore(name)))
        return len(s.sems) - 1

    def _collect(s, reads, writes):
        t = {}

        def add(d):
            for k, v in d.items():
                if t.get(k, 0) < v:
                    t[k] = v
        for k in reads:
            for k2, stt in s.bufs.get(k.split("/")[0], {}).items():
                if _ovl(k, k2):
                    add(stt["w"])
        for k in writes:
            for k2, stt in s.bufs.get(k.split("/")[0], {}).items():
                if _ovl(k, k2):
                    add(stt["w"])
                    add(stt["r"])
        return t

    def _update(s, reads, writes, tok):
        for k in reads:
            stt = s.bufs.setdefault(k.split("/")[0], {}).setdefault(k, {"w": {}, "r": {}})
            if stt["r"].get(tok[0], 0) < tok[1]:
                stt["r"][tok[0]] = tok[1]
        for k in writes:
            d = s.bufs.setdefault(k.split("/")[0], {})
            for k2 in list(d):
                if k2 != k and k2.startswith(k + "/"):
                    del d[k2]
            d[k] = {"w": {tok[0]: tok[1]}, "r": {}}
        if s.alltok.get(tok[0], 0) < tok[1]:
            s.alltok[tok[0]] = tok[1]

    def _wait(s, eng, toks, skip=None):
        e = s.E[eng]
        for sm, v in toks.items():
            if sm == skip:
                continue
            if s.waited[eng].get(sm, 0) < v:
                e.wait_ge(s.sems[sm], v)
                s.waited[eng][sm] = v

    def op(s, eng, fn, reads=(), writes=()):
        if s.halt:
            return
        if eng != "pe":
            pr = [k for k in reads if k.startswith("PS")]
            if pr:
                writes = list(writes) + pr
        toks = s._collect(reads, writes)
        s._wait(eng, toks, skip=s.esem[eng] if eng == "pe" else None)
        ins = fn()
        s.ecnt[eng] += 1
        ins.then_inc(s.sems[s.esem[eng]], 1)
        s._update(reads, writes, (s.esem[eng], s.ecnt[eng]))

    def dma(s, q, fn, reads=(), writes=()):
        if s.halt:
            return
        dq = s.dq[q]
        i = dq["nxt"]
        dq["nxt"] = (i + 1) % NDS
        sm = dq["sems"][i]
        toks = s._collect(reads, writes)
        if dq["use"][i] > 0 and toks.get(sm, 0) < 16 * dq["use"][i]:
            toks[sm] = 16 * dq["use"][i]
        s._wait(q, toks)
        ins = fn()
        dq["use"][i] += 1
        ins.then_inc(s.sems[sm], 16)
        s._update(reads, writes, (sm, 16 * dq["use"][i]))

    def cc(s, kind, op, src, dst):
        if s.halt:
            return
        import os
        if os.environ.get("KNOCC", "") == "1" or (os.environ.get("KNOCC", "") == kind):
            n = min(src.ap.shape[0], dst.ap.shape[0])
            s.ld(dst[0:n, :], src[0:n, :])
            return
        toks = s._collect([src.key], [dst.key])
        s._wait("pool", toks)
        ins = s.nc.gpsimd.collective_compute(kind, op, replica_groups=GROUPS, ins=[src.ap.opt()], outs=[dst.ap.opt()])
        s.cccnt += 1
        ins.then_inc(s.sems[s.ccsem])
        s._update([src.key], [dst.key], (s.ccsem, s.cccnt))

    def barrier(s):
        if s.halt:
            return
        for eng in s.E:
            s._wait(eng, dict(s.alltok), skip=None)

    def mm(s, out, lhsT, rhs, start=True, stop=True):
        s.op("pe", lambda: s.nc.tensor.matmul(_ap(out), _ap(lhsT), _ap(rhs), start=start, stop=stop),
             _keys(lhsT, rhs), _keys(out))

    def tr(s, out, in_, ident):
        s.op("pe", lambda: s.nc.tensor.transpose(_ap(out), _ap(in_), _ap(ident)), _keys(in_, ident), _keys(out))

    def act(s, out, in_, func, bias=None, scale=1.0, accum=None):
        kw = {}
        if bias is not None:
            kw["bias"] = _ap(bias)
        if accum is not None:
            kw["accum_out"] = _ap(accum)
        s.op("act", lambda: s.nc.scalar.activation(out=_ap(out), in_=_ap(in_), func=func, scale=_ap(scale), **kw),
             _keys(in_, bias, scale), _keys(out, accum))

    def tt(s, out, a, b, op, eng="dve"):
        e = s.E[eng]
        s.op(eng, lambda: e.tensor_tensor(out=_ap(out), in0=_ap(a), in1=_ap(b), op=op), _keys(a, b), _keys(out))

    def ts(s, out, a, s1, s2, op0, op1=None, accum=None, eng="dve"):
        e = s.E[eng]
        kw = {}
        if accum is not None:
            kw["accum_out"] = _ap(accum)
        if op1 is None:
            s.op(eng, lambda: e.tensor_scalar(out=_ap(out), in0=_ap(a), scalar1=_ap(s1), scalar2=None, op0=op0, **kw),
                 _keys(a, s1), _keys(out, accum))
        else:
            s.op(eng, lambda: e.tensor_scalar(out=_ap(out), in0=_ap(a), scalar1=_ap(s1), scalar2=_ap(s2), op0=op0, op1=op1, **kw),
                 _keys(a, s1, s2), _keys(out, accum))

    def stt(s, out, a, sc, b, op0, op1, eng="dve"):
        e = s.E[eng]
        s.op(eng, lambda: e.scalar_tensor_tensor(out=_ap(out), in0=_ap(a), scalar=_ap(sc), in1=_ap(b), op0=op0, op1=op1),
             _keys(a, sc, b), _keys(out))

    def cp(s, out, in_, eng="dve"):
        e = s.E[eng]
        s.op(eng, lambda: e.tensor_copy(out=_ap(out), in_=_ap(in_)), _keys(in_), _keys(out))

    def memset(s, out, val, eng="pool"):
        e = s.E[eng]
        s.op(eng, lambda: e.memset(_ap(out), val), [], _keys(out))

    def rmax(s, out, in_):
        s.op("dve", lambda: s.nc.vector.reduce_max(out=_ap(out), in_=_ap(in_), axis=AX.X), _keys(in_), _keys(out))

    def recip(s, out, in_):
        s.op("dve", lambda: s.nc.vector.reciprocal(out=_ap(out), in_=_ap(in_)), _keys(in_), _keys(out))

    def scan(s, out, d0, d1, init, op0, op1):
        s.op("dve", lambda: s.nc.vector.tensor_tensor_scan(out=_ap(out), data0=_ap(d0), data1=_ap(d1), initial=init, op0=op0, op1=op1),
             _keys(d0, d1), _keys(out))

    def ld(s, out, in_, q="sp"):
        e = s.E[q]
        s.dma(q, lambda: e.dma_start(out=_ap(out), in_=_ap(in_)), _keys(in_), _keys(out))

    def gather(s, out, src, idx):
        s.dma("pool", lambda: s.nc.gpsimd.indirect_dma_start(
            out=_ap(out), out_offset=None, in_=_ap(src),
            in_offset=bass.IndirectOffsetOnAxis(ap=_ap(idx), axis=0)), _keys(src, idx), _keys(out))

    def scatter_add(s, dst, src, idx):
        s.dma("pool", lambda: s.nc.gpsimd.indirect_dma_start(
            out=_ap(dst), out_offset=bass.IndirectOffsetOnAxis(ap=_ap(idx), axis=0),
            in_=_ap(src), in_offset=None, compute_op=ALU.add), _keys(src, idx, dst), _keys(dst))


def build(debug=False):
    nc = bass.Bass("TRN2", target_bir_lowering=False)
    top = ExitStack()
    P = Prog(nc, top)

    import os
    KSTOP = os.environ.get("KSTOP", "")
    open_stacks = []

    def stop(tag):
        if KSTOP == tag:
            P.barrier()
            P.halt = True

    def din(name, shape, dt=F32):
        return V(nc.dram_tensor(name, list(shape), dt, kind="ExternalInput").ap(), name)

    def dscr(name, shape, dt=F32):
        return V(nc.dram_tensor(name, list(shape), dt).ap(), name)

    uq = [0]

    def sb(st, name, shape, dt=F32, side=None):
        uq[0] += 1
        name = "%s_%d" % (name, uq[0])
        return V(st.enter_context(nc.sbuf_tensor(name, list(shape), dt, side=side))[:], name)

    def ps(st, name, shape, dt=F32):
        uq[0] += 1
        name = "PS%s_%d" % (name, uq[0])
        return V(st.enter_context(nc.psum_tensor(name, list(shape), dt))[:], name)

    xh = din("xh", [HTOK, D])
    ctxb = din("ctxb", [CTX, D])
    crep = din("crep", [128, KC * 128])
    ccrep = din("ccrep", [128, KC * 128])
    wmod = din("wmod", [D, 6 * D])
    bmodb = din("bmodb", [128, 6 * D])
    g1b = din("g1b", [128, D])
    gp1b = din("gp1b", [128, D])
    g2b = din("g2b", [128, D])
    gp2b = din("gp2b", [128, D])
    winx = din("winx", [D, 5120])
    wout = din("wout", [D, D])
    cosT = din("cosT", [128, HTOK])
    sinT = din("sinT", [128, HTOK])
    masks = din("masks", [8, 128, 5 * 1024])
    lbl = din("lbl", [128, 8])
    hgnb = din("hgnb", [128, 512])
    wr = din("wr", [D, 16])
    mfold = din("mfold", [128, 8])
    wg4 = din("wg4", [4, D, DE])
    wu4 = din("wu4", [4, D, DE])
    wd4 = din("wd4", [4, DE, D])
    selM = din("selM", [64, 16])
    selO = din("selO", [64, 16])
    Gm = din("Gm", [64, 64])
    Gpre = din("Gpre", [64, 64])
    identf = din("identf", [128, 128])
    maskA = din("maskA", [128, 256])
    rowmask = din("rowmask", [128, 4])
    resetm = din("resetm", [128, TOK])
    iota1k = din("iota1k", [128, 1024])
    tokc = din("tokc", [128, 768])
    out = V(nc.dram_tensor("out", [TOK, D], F32, kind="ExternalOutput").ap(), "out")
    if debug:
        dbg_x1 = V(nc.dram_tensor("dbg_x1", [TOK, D], F32, kind="ExternalOutput").ap(), "dbg_x1")
        dbg_mix = V(nc.dram_tensor("dbg_mix", [TOK, D], F32, kind="ExternalOutput").ap(), "dbg_mix")
        dbg_aff = V(nc.dram_tensor("dbg_aff", [16, TOK], F32, kind="ExternalOutput").ap(), "dbg_aff")
        dbg_idx = V(nc.dram_tensor("dbg_idx", [128, 64], F32, kind="ExternalOutput").ap(), "dbg_idx")

    na_d = dscr("na_d", [TOK, 512], BF16)
    sg_d = dscr("sg_d", [TOK, 512], BF16)
    x1_d = dscr("x1_d", [TOK, D])
    h2_in = dscr("h2_in", [TOK, D], BF16)
    h2_all = dscr("h2_all", [4 * TOK, D], BF16)
    st_in = dscr("st_in", [1032, 128])
    st_all = dscr("st_all", [4 * 1032, 128])
    af_in = dscr("af_in", [16, TOK])
    af_all = dscr("af_all", [64, TOK])
    acc = dscr("acc", [4 * TOK, D])
    rs_out = dscr("rs_out", [TOK, D])

    try:
        idf = sb(top, "idf", [128, 128])
        idb = sb(top, "idb", [128, 128], BF16)
        mst = ExitStack()
        gt1g = sb(top, "gt1g", [128, D])
        a2 = sb(top, "a2", [128, D])
        sh2 = sb(top, "sh2", [128, D])
        gt2g = sb(top, "gt2g", [128, D])
        cols = sb(top, "cols", [128, 32])
        zero4k = sb(top, "zero4k", [128, D])
        epsc = sb(top, "epsc", [128, 1])
        modB = sb(mst, "modB", [128, 6 * D])
        P.memset(epsc, EPS)
        P.ld(idf, identf)
        P.cp(idb, idf)
        P.memset(zero4k, 0.0)
        accv = acc.re("(n p) d -> n p d", p=128)
        for n in range(64):
            P.ld(accv[n], zero4k)

        with ExitStack() as st:
            sc_ = sb(st, "siluc", [128, KC * 128])
            scc = sb(st, "silucc", [128, KC * 128])
            modC = sb(st, "modC", [128, 2 * D])
            wmb = [sb(st, "wmb%d" % i, [128, KC, 512]) for i in range(2)]
            bmb = [sb(st, "bmb%d" % i, [128, 512]) for i in range(2)]
            pm = [ps(st, "pm%d" % i, [128, 512]) for i in range(2)]
            tmpb = sb(st, "tmpb", [128, D])
            ptr = ps(st, "ptr", [128, 128])
            P.ld(sc_, crep)
            P.ld(scc, ccrep)
            P.act(sc_, sc_, AF.Silu)
            P.act(scc, scc, AF.Silu)
            wmv = wmod.re("(kc p) n -> p kc n", p=128)
            for nb in range(12):
                wb = wmb[nb % 2]
                P.ld(wb, wmv[:, :, nb * 512:(nb + 1) * 512])
                P.ld(bmb[nb % 2], bmodb[:, nb * 512:(nb + 1) * 512])
                for kc in range(KC):
                    P.mm(pm[0], sc_[:, kc * 128:(kc + 1) * 128], wb[:, kc, :], start=(kc == 0), stop=(kc == KC - 1))
                P.tt(modB[:, nb * 512:(nb + 1) * 512], pm[0], bmb[nb % 2], ALU.add)
                if nb < 4:
                    for kc in range(KC):
                        P.mm(pm[1], scc[:, kc * 128:(kc + 1) * 128], wb[:, kc, :], start=(kc == 0), stop=(kc == KC - 1))
                    P.tt(modC[:, nb * 512:(nb + 1) * 512], pm[1], bmb[nb % 2], ALU.add)
            g1t = sb(st, "g1t", [128, D])
            P.ld(g1t, g1b)
            for (src_sh, src_sc, c0) in ((modB[:, 0:D], modB[:, D:2 * D], 0), (modC[:, 0:D], modC[:, D:2 * D], 16)):
                P.stt(tmpb, src_sc, 1.0, g1t, ALU.add, ALU.mult)
                for kc in range(KC):
                    P.tr(ptr, tmpb[:, kc * 128:(kc + 1) * 128], idf)
                    P.cp(cols[:, c0 + kc:c0 + kc + 1], ptr[:, 0:1])
                    P.tr(ptr, src_sh[:, kc * 128:(kc + 1) * 128], idf)
                    P.cp(cols[:, c0 + 8 + kc:c0 + 8 + kc + 1], ptr[:, 0:1])
            P.ld(tmpb, gp1b)
            P.tt(gt1g, modB[:, 2 * D:3 * D], tmpb, ALU.mult)
            P.ld(tmpb, g2b)
            P.stt(a2, modB[:, 4 * D:5 * D], 1.0, tmpb, ALU.add, ALU.mult)
            P.cp(sh2, modB[:, 3 * D:4 * D])
            P.ld(tmpb, gp2b)
            P.tt(gt2g, modB[:, 5 * D:6 * D], tmpb, ALU.mult)
        P.barrier()
        mst.close()
        stop("a")

        s1 = ExitStack()
        open_stacks.append(s1)
        hT = sb(s1, "hT", [128, KC, HTOK], BF16, side="right")
        hcT = sb(s1, "hcT", [128, KC, CTX], BF16, side="right")
        with ExitStack() as st:
            xt = [sb(st, "xt%d" % i, [128, D]) for i in range(2)]
            xs = [sb(st, "xs%d" % i, [128, D], BF16) for i in range(2)]
            junk = sb(st, "junk", [128, D])
            ss = sb(st, "ss", [128, 2])
            pt = [ps(st, "pt%d" % i, [128, D], BF16) for i in range(2)]
            xhv = xh.re("(n p) d -> n p d", p=128)
            cxv = ctxb.re("(n p) d -> n p d", p=128)
            for i in range(NTH + 2):
                isctx = i >= NTH
                src = cxv[i - NTH] if isctx else xhv[i]
                x_ = xt[i % 2]
                P.ld(x_, src)
                P.act(junk, x_, AF.Square, accum=ss[:, 0:1])
                P.act(ss[:, 1:2], ss[:, 0:1], AF.Sqrt, bias=epsc[:, 0:1], scale=1.0 / D)
                P.recip(ss[:, 1:2], ss[:, 1:2])
                P.ts(xs[i % 2], x_, ss[:, 1:2], None, ALU.mult)
                for kc in range(KC):
                    P.tr(pt[i % 2][:, kc * 128:(kc + 1) * 128], xs[i % 2][:, kc * 128:(kc + 1) * 128], idb)
                c0 = 16 if isctx else 0
                for kc in range(KC):
                    dst = hcT[:, kc, (i - NTH) * 128:(i - NTH + 1) * 128] if isctx else hT[:, kc, i * 128:(i + 1) * 128]
                    if kc % 2 == 0:
                        P.act(dst, pt[i % 2][:, kc * 128:(kc + 1) * 128], AF.Identity,
                              bias=cols[:, c0 + 8 + kc:c0 + 9 + kc], scale=cols[:, c0 + kc:c0 + kc + 1])
                    else:
                        P.ts(dst, pt[i % 2][:, kc * 128:(kc + 1) * 128], cols[:, c0 + kc:c0 + kc + 1],
                             cols[:, c0 + 8 + kc:c0 + 9 + kc], ALU.mult, ALU.add)
        P.barrier()
        stop("b")

        winv = winx.re("(kc p) n -> p kc n", p=128)

        def load_w(st_w, wst, wbf, blk, ncols=512, col0=None):
            c0 = blk * 512 if col0 is None else col0
            for kc in range(KC):
                P.ld(wst[kc % 2][:, 0:ncols], winv[:, kc, c0:c0 + ncols])
                P.cp(wbf[:, kc, 0:ncols], wst[kc % 2][:, 0:ncols], eng="pool")

        with ExitStack() as st:
            na_tok = sb(st, "na_tok", [128, NTO, 512], BF16)
            for half in range(2):
                with ExitStack() as sth:
                    qT = sb(sth, "qT", [128, 2, TOK], BF16)
                    qrT = sb(sth, "qrT", [128, 2, TOK], BF16)
                    krT = sb(sth, "krT", [128, 2, HTOK], BF16)
                    vtk = sb(sth, "vtk", [128, NTH, 256], BF16)
                    kcT = sb(sth, "kcT", [128, 2, CTX], BF16)
                    vck = sb(sth, "vck", [128, 2, 256], BF16)
                    with ExitStack() as st2:
                        wst = [sb(st2, "wst%d" % i, [128, 512]) for i in range(2)]
                        wAs = [sb(st2, "wA%d" % i_, [128, KC, 256], BF16) for i_ in range(2)]
                        wBs = [sb(st2, "wB%d" % i_, [128, KC, 256], BF16) for i_ in range(2)]
                        cs = sb(st2, "cs", [128, HTOK])
                        sn = sb(st2, "sn", [128, HTOK])
                        t1 = sb(st2, "t1", [128, 512])
                        t2 = sb(st2, "t2", [128, 512])
                        pa = [ps(st2, "pa%d" % i, [128, 512]) for i in range(2)]
                        pb = [ps(st2, "pb%d" % i, [128, 512]) for i in range(2)]
                        P.ld(cs, cosT)
                        P.ld(sn, sinT)
                        for (blk, pblk, ntok, tok0, dstp, dstr) in ((0, 8, TOK, OWN0 * 128, qT, qrT), (1, 9, HTOK, 0, None, krT)):
                            wA, wB = wAs[blk], wBs[blk]
                            load_w(st2, wst, wA, blk, 256, blk * 512 + half * 256)
                            load_w(st2, wst, wB, pblk, 256, pblk * 512 + half * 256)
                            if half == 0 and blk == 0:
                                stop("c1a")
                            it = 0
                            for hp in range(2):
                                for nb in range(ntok // 512):
                                    a_, b_ = pa[it % 2], pb[it % 2]
                                    it += 1
                                    tk = tok0 + nb * 512
                                    for kc in range(KC):
                                        P.mm(a_, wA[:, kc, hp * 128:(hp + 1) * 128], hT[:, kc, tk:tk + 512], start=(kc == 0), stop=(kc == KC - 1))
                                    for kc in range(KC):
                                        P.mm(b_, wB[:, kc, hp * 128:(hp + 1) * 128], hT[:, kc, tk:tk + 512], start=(kc == 0), stop=(kc == KC - 1))
                                    KV = os.environ.get("KVAR", "")
                                    if dstp is not None and KV not in ("1", "3"):
                                        P.act(dstp[:, hp, nb * 512:(nb + 1) * 512], a_, AF.Copy)
                                    if KV not in ("2", "3"):
                                        P.tt(t1, a_, cs[:, tk:tk + 512], ALU.mult)
                                        P.tt(t2, b_, sn[:, tk:tk + 512], ALU.mult)
                                        P.tt(dstr[:, hp, nb * 512:(nb + 1) * 512], t1, t2, ALU.add)
                            if half == 0 and blk == 0:
                                stop("c1b")
                            if half == 0 and blk == 1:
                                stop("c1c")
                        for hp in range(2):
                            for kc in range(KC):
                                P.mm(pa[0][:, 0:CTX], wA[:, kc, hp * 128:(hp + 1) * 128], hcT[:, kc, :], start=(kc == 0), stop=(kc == KC - 1))
                            P.cp(kcT[:, hp, :], pa[0][:, 0:CTX])
                        wA = wAs[0]
                        load_w(st2, wst, wA, 2, 256, 2 * 512 + half * 256)
                        for i in range(NTH + 2):
                            a_ = pa[i % 2]
                            for kc in range(KC):
                                lhs = hcT[:, kc, (i - NTH) * 128:(i - NTH + 1) * 128] if i >= NTH else hT[:, kc, i * 128:(i + 1) * 128]
                                P.mm(a_[:, 0:256], lhs, wA[:, kc, 0:256], start=(kc == 0), stop=(kc == KC - 1))
                            dst = vck[:, i - NTH, :] if i >= NTH else vtk[:, i, :]
                            if i % 2 == 0:
                                P.act(dst, a_[:, 0:256], AF.Copy)
                            else:
                                P.cp(dst, a_[:, 0:256])
                    P.barrier()
                    if half == 0:
                        stop("c1")
                    with ExitStack() as st2:
                        mk = [sb(st2, "mk%d" % i, [128, 5, 1024]) for i in range(2)]
                        scb = [sb(st2, "scb%d" % i, [128, 1024]) for i in range(2)]
                        pb_ = [sb(st2, "pbf%d" % i, [128, 1024], BF16) for i in range(2)]
                        ptb = [sb(st2, "ptb%d" % i, [128, 8, 128], BF16) for i in range(2)]
                        sm = sb(st2, "smx", [128, 8])
                        psS = [ps(st2, "psS%d" % i, [128, 1024]) for i in range(2)]
                        psT = [ps(st2, "psT%d" % i, [128, 1024], BF16) for i in range(2)]
                        psO = [ps(st2, "psO%d" % i, [128, 64]) for i in range(2)]
                        smd = [sb(st2, "smd%d" % i, [128, 8]) for i in range(2)]

                        def geom(rp):
                            cls = 0 if rp == 0 else 1 if rp == 1 else 3 if rp == 14 else 4 if rp == 15 else 2
                            brow = 0 if rp <= 1 else 28 if rp >= 14 else 2 * rp
                            return cls, brow * 64

                        def stA1(hl, rp, j):
                            hp, hf = hl // 2, (hl % 2) * 64
                            mkh = mk[hl % 2]
                            cls, k0 = geom(rp)
                            S_ = psS[j]
                            sm_ = smd[j]
                            qsl = slice(rp * 128, (rp + 1) * 128)
                            P.mm(S_[:, 0:256], qT[hf:hf + 64, hp, qsl], kcT[hf:hf + 64, hp, :])
                            P.mm(S_[:, 256:512], qrT[hf:hf + 64, hp, qsl], krT[hf:hf + 64, hp, k0:k0 + 256])
                            P.mm(S_[:, 512:1024], qrT[hf:hf + 64, hp, qsl], krT[hf:hf + 64, hp, k0 + 256:k0 + 768])
                            P.stt(scb[j], S_, 0.125, mkh[:, cls, :], ALU.mult, ALU.add)
                            P.rmax(sm_[:, 0:1], scb[j])
                            P.ts(sm_[:, 1:2], sm_[:, 0:1], -1.0, None, ALU.mult)

                        def stA2(hl, rp, j):
                            sm_ = smd[j]
                            P.act(pb_[j], scb[j], AF.Exp, bias=sm_[:, 1:2], accum=sm_[:, 2:3])

                        def stB1(hl, rp, j):
                            for c in range(8):
                                P.tr(psT[j][:, c * 128:(c + 1) * 128], pb_[j][:, c * 128:(c + 1) * 128], idb)
                            P.act(ptb[j].re("p c n -> p (c n)"), psT[j], AF.Copy)

                        def stB2(hl, rp, j):
                            h = half * 4 + hl
                            cls, k0 = geom(rp)
                            sm_ = smd[j]
                            t0 = k0 // 128
                            for c in range(8):
                                rhs = vck[:, c, hl * 64:(hl + 1) * 64] if c < 2 else vtk[:, t0 + c - 2, hl * 64:(hl + 1) * 64]
                                P.mm(psO[j], ptb[j][:, c, :], rhs, start=(c == 0), stop=(c == 7))
                            P.recip(sm_[:, 3:4], sm_[:, 2:3])
                            P.ts(na_tok[:, rp, h * 64:(h + 1) * 64], psO[j], sm_[:, 3:4], None, ALU.mult)

                        seq = [(hl, rp) for hl in range(4) for rp in range(NTO)]
                        for n_ in range(len(seq) + 1):
                            nxt = seq[n_] if n_ < len(seq) else None
                            cur = seq[n_ - 1] if n_ >= 1 else None
                            if nxt is not None:
                                if nxt[1] == 0:
                                    P.ld(mk[nxt[0] % 2].re("p c n -> p (c n)"), masks[half * 4 + nxt[0]])
                                stA1(nxt[0], nxt[1], n_ % 2)
                            if cur is not None:
                                stB1(cur[0], cur[1], (n_ - 1) % 2)
                            if nxt is not None:
                                stA2(nxt[0], nxt[1], n_ % 2)
                            if cur is not None:
                                stB2(cur[0], cur[1], (n_ - 1) % 2)
                    P.barrier()
            for i_ in range(NTO):
                P.ld(na_d[i_ * 128:(i_ + 1) * 128, :], na_tok[:, i_, :])
        P.barrier()
        stop("c")
        hg = ExitStack()
        open_stacks.append(hg)
        o0 = sb(hg, "o0", [128, NTO, 512], BF16)
        qseg = sb(hg, "qseg", [128, 8, TOK], BF16)
        Send = sb(hg, "Send", [128, 8, 128])
        Sctx = sb(hg, "Sctx", [128, 8, 128])
        Dtot = sb(hg, "Dtot", [128, 8])
        lbt = sb(hg, "lbt", [128, 16])
        lbn = sb(hg, "lbn", [128, 4])
        ones64 = sb(hg, "ones64", [128, 64])
        rmk = sb(hg, "rmk", [128, 4])
        mAt = sb(hg, "mAt", [128, 256])
        mAb = sb(hg, "mAb", [128, 256], BF16)
        P.ld(lbt[:, 0:8], lbl)
        P.ld(rmk, rowmask)
        P.ld(mAt, maskA)
        P.cp(mAb, mAt)
        P.memset(ones64, 1.0)
        P.tt(lbt[:, 8:12], lbt[:, 0:4], lbt[:, 4:8], ALU.subtract)
        P.act(lbt[:, 12:16], lbt[:, 8:12], AF.Sigmoid, scale=-1.0)
        P.act(lbt[:, 8:12], lbt[:, 8:12], AF.Sigmoid)
        P.ts(lbn, lbt[:, 12:16], -1.0, None, ALU.mult)
        with ExitStack() as st:
            ih = sb(st, "ih", [128, NTO, 512], BF16)
            ihc = sb(st, "ihc", [128, 2, 512], BF16)
            wst = [sb(st, "wst%d" % i, [128, 512]) for i in range(2)]
            rsm = sb(st, "rsm", [128, TOK])
            P.ld(rsm, resetm)
            hTo = hT[:, :, OWN0 * 128:OWN0 * 128 + TOK]
            with ExitStack() as st2:
                pa = [ps(st2, "pa%d" % i, [128, 512]) for i in range(2)]
                sgt = [sb(st2, "sgt%d" % i, [128, 512], BF16) for i in range(2)]
                wA = sb(st2, "wA", [128, KC, 512], BF16)
                for (blk, dstt, dstc, fn) in ((6, ih, ihc, AF.Copy), (7, None, None, AF.Sigmoid)):
                    load_w(st2, wst, wA, blk)
                    for i in range(NTO + (2 if dstc is not None else 0)):
                        a_ = pa[i % 2]
                        for kc in range(KC):
                            lhs = hcT[:, kc, (i - NTO) * 128:(i - NTO + 1) * 128] if i >= NTO else hTo[:, kc, i * 128:(i + 1) * 128]
                            P.mm(a_, lhs, wA[:, kc, :], start=(kc == 0), stop=(kc == KC - 1))
                        if dstt is None:
                            P.act(sgt[i % 2], a_, fn)
                            P.ld(sg_d[i * 128:(i + 1) * 128, :], sgt[i % 2])
                        else:
                            P.act(dstc[:, i - NTO, :] if i >= NTO else dstt[:, i, :], a_, fn)
            P.barrier()
            with ExitStack() as st2:
                wzs = [sb(st2, "wz%d" % i_, [128, KC, 128], BF16) for i_ in range(2)]
                sg_ = sb(st2, "s_", [128, TOK])
                binc = sb(st2, "binc", [128, TOK])
                qhh = sb(st2, "qhh", [128, TOK], BF16)
                wqs = [sb(st2, "wq%d" % i_, [128, KC, 128], BF16) for i_ in range(2)]
                kk = sb(st2, "kk", [128, TOK], BF16)
                kdec = sb(st2, "kdec", [128, TOK], BF16)
                kend = sb(st2, "kend", [128, TOK], BF16)
                qdec = sb(st2, "qdec", [128, TOK], BF16)
                sm = sb(st2, "hsm", [128, 4, 64])
                S = sb(st2, "S", [128, 128])
                tot = sb(st2, "tot", [128, 64])
                Sb = [sb(st2, "Sb%d" % i, [128, 4, 128], BF16) for i in range(2)]
                QM = [sb(st2, "QM%d" % i, [128, 640], BF16) for i in range(2)]
                kendT = [sb(st2, "kendT%d" % i, [128, 128], BF16) for i in range(2)]
                vm = [sb(st2, "vm%d" % i, [128, 4, 128], BF16) for i in range(2)]
                Am = [sb(st2, "Am%d" % i, [128, 128], BF16) for i in range(2)]
                pz = [ps(st2, "pz%d" % i, [128, 512]) for i in range(2)]
                pT = ps(st2, "pT", [128, 1024], BF16)
                pKV = [ps(st2, "pKV%d" % i, [128, 4, 128]) for i in range(2)]
                pA = ps(st2, "pA", [128, 128])
                pO = [ps(st2, "pO%d" % i, [128, 128]) for i in range(2)]
                P.memset(QM[0], 0.0)
                P.memset(QM[1], 0.0)
                for isctx in (True, False):
                    ntok = CTX if isctx else TOK
                    nt = ntok // 128
                    nch = ntok // 32
                    hsrc = hcT if isctx else hTo
                    vsrc = ihc if isctx else ih
                    for d in range(2):
                        for h in range(4):
                            k = d * 4 + h
                            wz, wq = wzs[k % 2], wqs[k % 2]
                            load_w(st2, wst, wz, None, 128, (4 + d) * 512 + h * 128)
                            nbs = [(0, 256)] if isctx else [(i * 512, 512) for i in range(4)]
                            for bi, (c0, cn) in enumerate(nbs):
                                a_ = pz[bi % 2]
                                for kc in range(KC):
                                    P.mm(a_[:, 0:cn], wz[:, kc, :], hsrc[:, kc, c0:c0 + cn], start=(kc == 0), stop=(kc == KC - 1))
                                P.act(sg_[:, c0:c0 + cn], a_[:, 0:cn], AF.Sigmoid)
                            sv = sg_[:, 0:ntok]
                            P.ts(kk[:, 0:ntok], sv, lbn[:, h:h + 1], lbt[:, 12 + h:13 + h], ALU.mult, ALU.add)
                            P.act(sv, sv, AF.Ln, bias=lbt[:, 8 + h:9 + h], scale=lbt[:, 12 + h:13 + h])
                            P.scan(binc[:, 0:ntok], rsm[:, 0:ntok], sv, 0.0, ALU.mult, ALU.add)
                            b3 = binc[:, 0:ntok].re("p (c t) -> p c t", t=32)
                            bend = b3[:, :, 31]
                            P.cp(tot[:, 0:nch], bend)
                            bend = tot[:, 0:nch]
                            B = binc[:, 0:ntok]
                            if d == 1:
                                P.tt(B, sv, B, ALU.subtract)
                                P.tt(b3, b3, bend.re("p (c o) -> p c o", o=1).bc([128, nch, 32]), ALU.add)
                            Ee = sg_
                            P.act(sm[:, 2, 0:nch], bend, AF.Exp)
                            P.scan(sm[:, 0, 0:nch], ones64[:, 0:nch], bend, 0.0, ALU.mult, ALU.add)
                            if d == 0:
                                P.tt(sm[:, 1, 0:nch], sm[:, 0, 0:nch], bend, ALU.subtract)
                            else:
                                P.ts(sm[:, 1, 0:nch], sm[:, 0, 0:nch], -1.0, sm[:, 0, nch - 1:nch], ALU.mult, ALU.add)
                            P.act(sm[:, 3, 0:nch], sm[:, 1, 0:nch], AF.Exp)
                            if not isctx:
                                P.act(Dtot[:, k:k + 1], sm[:, 0, nch - 1:nch], AF.Exp)
                                P.act(Ee[:, 0:ntok], B, AF.Exp)
                                load_w(st2, wst, wq, None, 128, 3 * 512 + h * 128)
                                for nb in range(4):
                                    a_ = pz[nb % 2]
                                    for kc in range(KC):
                                        P.mm(a_, wq[:, kc, :], hTo[:, kc, nb * 512:(nb + 1) * 512], start=(kc == 0), stop=(kc == KC - 1))
                                    P.cp(qhh[:, nb * 512:(nb + 1) * 512], a_)
                                P.tt(qdec[:, 0:ntok], qhh, Ee[:, 0:ntok], ALU.mult)
                                P.tt(qseg[:, k, :].re("p (c t) -> p c t", t=32), qdec.re("p (c t) -> p c t", t=32),
                                     sm[:, 3, 0:nch].re("p (c o) -> p c o", o=1).bc([128, nch, 32]), ALU.mult)
                            P.act(Ee[:, 0:ntok], B, AF.Exp, scale=-1.0)
                            P.tt(kdec[:, 0:ntok], kk[:, 0:ntok], Ee[:, 0:ntok], ALU.mult)
                            P.tt(kend[:, 0:ntok].re("p (c t) -> p c t", t=32), kdec[:, 0:ntok].re("p (c t) -> p c t", t=32),
                                 sm[:, 2, 0:nch].re("p (c o) -> p c o", o=1).bc([128, nch, 32]), ALU.mult)
                            P.memset(S, 0.0, eng="dve")
                            tiles = list(range(nt)) if d == 0 else list(range(nt - 1, -1, -1))
                            chs = [0, 1, 2, 3] if d == 0 else [3, 2, 1, 0]
                            for ti, i in enumerate(tiles):
                                j2 = ti % 2
                                tsl = slice(i * 128, (i + 1) * 128)
                                hc = slice(h * 128, (h + 1) * 128)
                                P.tr(pT[:, 0:128], kend[:, tsl], idb)
                                P.act(kendT[j2], pT[:, 0:128], AF.Copy)
                                P.tt(vm[j2], vsrc[:, i, hc].re("p (o v) -> p o v", o=1).bc([128, 4, 128]),
                                     rmk.re("p (j o) -> p j o", o=1).bc([128, 4, 128]), ALU.mult)
                                for j in range(4):
                                    P.mm(pKV[j2][:, j, :], kendT[j2], vm[j2][:, j, :])
                                if not isctx:
                                    P.mm(pA, kdec[:, tsl], qdec[:, tsl])
                                    P.tt(Am[j2], pA, mAb[:, d * 128:(d + 1) * 128], ALU.mult)
                                    P.act(QM[j2].re("p (j x) -> p j x", x=160)[:, :, 0:32],
                                          qdec[:, tsl].re("p (j t) -> p j t", t=32), AF.Copy)
                                for j in chs:
                                    if not isctx:
                                        P.act(Sb[j2][:, j, :], S, AF.Copy)
                                    P.stt(S, S, sm[:, 2, i * 4 + j:i * 4 + j + 1], pKV[j2][:, j, :], ALU.mult, ALU.add)
                                if not isctx:
                                    P.mm(pO[j2], Am[j2], vsrc[:, i, hc], start=True, stop=False)
                                    for j in range(4):
                                        P.mm(pO[j2], QM[j2][:, j * 128:(j + 1) * 128], Sb[j2][:, j, :], start=False, stop=(j == 3))
                                    if d == 0:
                                        P.cp(o0[:, i, hc], pO[j2])
                                    else:
                                        P.tt(o0[:, i, hc], o0[:, i, hc], pO[j2], ALU.add)
                            P.cp((Sctx if isctx else Send)[:, k, :], S)
        P.barrier()
        stop("d")
        s1.close()
        open_stacks.remove(s1)
        Ssb = sb(hg, "Ssb", [128, 8, 128], BF16)
        with ExitStack() as st:
            pD = ps(st, "pD", [8, 128])
            dT = sb(st, "dT", [8, 128])
            mf = sb(st, "mf", [128, 8])
            U = [sb(st, "U%d" % i, [128, 128]) for i in range(2)]
            Dj = [sb(st, "Dj%d" % i, [128, 4]) for i in range(2)]
            P.ld(mf, mfold)
            P.tr(pD, Dtot, idf)
            P.cp(dT, pD)
            for k in range(8):
                P.ld(st_in[k * 128:(k + 1) * 128, :], Send[:, k, :])
            P.ld(st_in[1024:1032, :], dT)
            P.cc("AllGather", ALU.bypass, st_in, st_all)
            it = 0
            for k in range(8):
                d = k // 4
                Sk = Sctx[:, k, :]
                for j in ([0, 1, 2, 3] if d == 0 else [3, 2, 1, 0]):
                    u_, d_ = U[it % 2], Dj[it % 2]
                    it += 1
                    P.ld(u_, st_all[j * 1032 + k * 128:j * 1032 + (k + 1) * 128, :])
                    P.ld(d_[:, 0:1], st_all[j * 1032 + 1024 + k:j * 1032 + 1025 + k, :].re("o d -> d o"))
                    m_ = mf[:, d * 4 + j:d * 4 + j + 1]
                    P.ts(d_[:, 1:2], d_[:, 0:1], -1.0, m_, ALU.add, ALU.mult)
                    P.ts(d_[:, 1:2], d_[:, 1:2], 1.0, None, ALU.add)
                    P.ts(u_, u_, m_, None, ALU.mult)
                    P.stt(Sk, Sk, d_[:, 1:2], u_, ALU.mult, ALU.add)
                P.cp(Ssb[:, k, :], Sk)
        P.barrier()
        stop("e")
        with ExitStack() as st:
            woutb = sb(st, "woutb", [128, KC, D], BF16)
            wst2 = [sb(st, "wst2%d" % i, [128, D]) for i in range(2)]
            wrs = sb(st, "wrs", [128, KC, 16])
            hgn = sb(st, "hgn", [128, 512])
            affT = sb(st, "affT", [16, TOK])
            ot = sb(st, "ot", [128, 512])
            hgt = sb(st, "hgt", [128, 512], BF16)
            nat = [sb(st, "nat%d" % i, [128, 512], BF16) for i in range(2)]
            sgl = [sb(st, "sgl%d" % i, [128, 512], BF16) for i in range(2)]
            xt = [sb(st, "xt%d" % i, [128, D]) for i in range(2)]
            x1t = [sb(st, "x1t%d" % i, [128, D]) for i in range(2)]
            h2t = [sb(st, "h2t%d" % i, [128, D]) for i in range(2)]
            h2b = [sb(st, "h2b%d" % i, [128, D], BF16) for i in range(2)]
            mixT = sb(st, "mixT", [128, KC, 128], BF16)
            h2T = sb(st, "h2T", [128, KC, 128])
            junk = sb(st, "junk2", [128, D])
            sm = sb(st, "rsm2", [128, 16])
            lg = sb(st, "lg", [128, 16])
            psC = ps(st, "psC", [128, 512])
            psT = ps(st, "psT", [128, 1024], BF16)
            psM = ps(st, "psM", [128, 1024])
            psT32 = ps(st, "psT32", [128, 1024])
            psL = ps(st, "psL", [128, 128])
            woutv = wout.re("(kc p) n -> p kc n", p=128)
            for kc in range(KC):
                P.ld(wst2[kc % 2], woutv[:, kc, :])
                P.cp(woutb[:, kc, :], wst2[kc % 2], eng="pool")
            P.ld(wrs, wr.re("(kc p) n -> p kc n", p=128))
            P.ld(hgn, hgnb)
            xhv = xh.re("(n p) d -> n p d", p=128)
            from functools import partial as F_
            from itertools import zip_longest
            smA = [sb(st, "smA%d" % i, [128, 8]) for i in range(2)]
            smB = [sb(st, "smB%d" % i, [128, 8]) for i in range(2)]
            smC = [sb(st, "smC%d" % i, [128, 8]) for i in range(2)]
            junkA = sb(st, "junkA", [128, 128])

            def S1(i):
                ops = []
                A = ops.append
                j2 = i % 2
                tsl = slice(i * 128, (i + 1) * 128)
                sm_ = smA[j2]
                A(F_(P.ld, nat[j2], na_d[tsl, :]))
                A(F_(P.ld, sgl[j2], sg_d[tsl, :]))
                A(F_(P.ld, xt[j2], xhv[i + OWN0]))
                for h in range(4):
                    A(F_(P.mm, psC[:, h * 128:(h + 1) * 128], qseg[:, h, tsl], Ssb[:, h, :], start=True, stop=False))
                    A(F_(P.mm, psC[:, h * 128:(h + 1) * 128], qseg[:, 4 + h, tsl], Ssb[:, 4 + h, :], start=False, stop=True))
                A(F_(P.tt, ot, o0[:, i, :], psC, ALU.add))
                for h in range(4):
                    A(F_(P.act, junkA, ot[:, h * 128:(h + 1) * 128], AF.Square, accum=sm_[:, h:h + 1]))
                A(F_(P.act, sm_[:, 4:8], sm_[:, 0:4], AF.Sqrt, bias=epsc[:, 0:1], scale=1.0 / 128))
                A(F_(P.recip, sm_[:, 4:8], sm_[:, 4:8]))
                o3 = ot.re("p (h v) -> p h v", v=128)
                A(F_(P.tt, o3, o3, sm_[:, 4:8].re("p (h o) -> p h o", o=1).bc([128, 4, 128]), ALU.mult))
                A(F_(P.tt, ot, ot, hgn, ALU.mult))
                A(F_(P.tt, hgt, ot, sgl[j2], ALU.mult))
                for c in range(4):
                    A(F_(P.tr, psT[:, c * 128:(c + 1) * 128], nat[j2][:, c * 128:(c + 1) * 128], idb))
                    A(F_(P.tr, psT[:, (4 + c) * 128:(5 + c) * 128], hgt[:, c * 128:(c + 1) * 128], idb))
                A(F_(P.act, mixT.re("p c n -> p (c n)"), psT, AF.Copy))
                for nb in range(2):
                    for mc in range(KC):
                        A(F_(P.mm, psM[:, nb * 512:(nb + 1) * 512], mixT[:, mc, :], woutb[:, mc, nb * 512:(nb + 1) * 512], start=(mc == 0), stop=(mc == KC - 1)))
                return ops

            def S2(i):
                ops = []
                A = ops.append
                j2 = i % 2
                tsl = slice(i * 128, (i + 1) * 128)
                sm_ = smB[j2]
                A(F_(P.act, junk, psM, AF.Square, accum=sm_[:, 0:1]))
                A(F_(P.act, sm_[:, 1:2], sm_[:, 0:1], AF.Sqrt, bias=epsc[:, 0:1], scale=1.0 / D))
                A(F_(P.recip, sm_[:, 1:2], sm_[:, 1:2]))
                A(F_(P.stt, x1t[j2], psM, sm_[:, 1:2], gt1g, ALU.mult, ALU.mult))
                A(F_(P.tt, x1t[j2], x1t[j2], xt[j2], ALU.add))
                A(F_(P.ld, x1_d[tsl, :], x1t[j2]))
                if debug:
                    A(F_(P.ld, dbg_x1[tsl, :], x1t[j2]))
                A(F_(P.act, junk, x1t[j2], AF.Square, accum=sm_[:, 2:3]))
                A(F_(P.act, sm_[:, 3:4], sm_[:, 2:3], AF.Sqrt, bias=epsc[:, 0:1], scale=1.0 / D))
                A(F_(P.recip, sm_[:, 3:4], sm_[:, 3:4]))
                A(F_(P.stt, h2t[j2], x1t[j2], sm_[:, 3:4], a2, ALU.mult, ALU.mult))
                A(F_(P.tt, h2t[j2], h2t[j2], sh2, ALU.add))
                A(F_(P.act, h2b[j2], h2t[j2], AF.Copy))
                A(F_(P.ld, h2_in[tsl, :], h2b[j2]))
                sm_ = smC[j2]
                for kc in range(KC):
                    A(F_(P.tr, psT32[:, kc * 128:(kc + 1) * 128], h2t[j2][:, kc * 128:(kc + 1) * 128], idf))
                A(F_(P.cp, h2T.re("p c n -> p (c n)"), psT32))
                for kc in range(KC):
                    A(F_(P.mm, psL[:, 0:16], h2T[:, kc, :], wrs[:, kc, :], start=(kc == 0), stop=(kc == KC - 1)))
                A(F_(P.rmax, sm_[:, 0:1], psL[:, 0:16]))
                A(F_(P.ts, sm_[:, 1:2], sm_[:, 0:1], -1.0, None, ALU.mult))
                A(F_(P.act, lg, psL[:, 0:16], AF.Exp, bias=sm_[:, 1:2], accum=sm_[:, 2:3]))
                A(F_(P.recip, sm_[:, 3:4], sm_[:, 2:3]))
                A(F_(P.ts, lg, lg, sm_[:, 3:4], None, ALU.mult))
                A(F_(P.tr, psL[0:16, :], lg, idf))
                A(F_(P.cp, affT[:, tsl], psL[0:16, :]))
                return ops

            for i in range(NTO + 1):
                oa = S1(i) if i < NTO else []
                ob = S2(i - 1) if i >= 1 else []
                for x_, y_ in zip_longest(oa, ob):
                    if x_ is not None:
                        x_()
                    if y_ is not None:
                        y_()
            P.ld(af_in, affT)
            if debug:
                P.ld(dbg_aff, affT)
        hg.close()
        open_stacks.remove(hg)
        stop("f")
        P.cc("AllGather", ALU.bypass, af_in, af_all)
        for ch in range(4):
            P.cc("AllGather", ALU.bypass, h2_in[ch * 512:(ch + 1) * 512, :], h2_all[ch * 2048:(ch + 1) * 2048, :])
        P.barrier()
        stop("1")
        rt = ExitStack()
        idx_i = sb(rt, "idx_i", [128, 4, 8], I32)
        idx_t = sb(rt, "idx_t", [128, 4, 8], I32)
        gate = sb(rt, "gate", [128, 4, 8])
        with ExitStack() as st:
            A = sb(st, "A", [64, TOK])
            selb = sb(st, "selb", [64, TOK])
            incl = sb(st, "incl", [64, TOK])
            ones = sb(st, "ones", [64, TOK])
            Gs = sb(st, "Gs", [64, 64])
            Gp = sb(st, "Gp", [64, 64])
            sM = sb(st, "sM", [64, 16])
            bs = sb(st, "bs", [64, 8])
            rkM = sb(st, "rkM", [128, 16, 16])
            afM = sb(st, "afM", [128, 16, 16])
            res = sb(st, "res", [128, 256])
            gb = sb(st, "gb", [128, 256], BF16)
            vals0 = sb(st, "vals0", [128, 768])
            VALS = sb(st, "VALS", [128, 256, 8], BF16)
            iot = sb(st, "iot", [128, 1024])
            oh = [sb(st, "oh%d" % i, [128, 1024], BF16) for i in range(2)]
            idxf = sb(st, "idxf", [128, 8, 8])
            tokf = sb(st, "tokf", [128, 8])
            P.ld(A, af_all)
            P.ld(Gs, Gm)
            P.ld(Gp, Gpre)
            P.ld(sM, selM)
            P.ld(iot, iota1k)
            P.ld(vals0, tokc)
            P.memset(ones, 1.0)
            P.memset(bs, 0.0)
            P.memset(bs[:, 1:2], 1.0)
            with ExitStack() as st2:
                psb = ps(st2, "psb", [64, 8])
                psr = [ps(st2, "psr%d" % i, [128, 16]) for i in range(2)]
                lo, hi, mid, cpart, cond, tmp = (bs[:, i:i + 1] for i in range(6))
                for it in range(32):
                    P.tt(mid, lo, hi, ALU.add)
                    P.ts(mid, mid, 0.5, None, ALU.mult)
                    P.ts(selb, A, mid, 0.0, ALU.is_gt, ALU.add, accum=cpart)
                    P.mm(psb[:, 0:1], Gs, cpart)
                    P.ts(cond, psb[:, 0:1], 1024.0, None, ALU.is_ge)
                    P.tt(tmp, mid, lo, ALU.subtract)
                    P.stt(lo, tmp, cond, lo, ALU.mult, ALU.add)
                    P.tt(tmp, hi, mid, ALU.subtract)
                    P.stt(hi, tmp, cond, mid, ALU.mult, ALU.add)
                P.ts(selb, A, lo, None, ALU.is_gt)
                P.scan(incl, ones, selb, 0.0, ALU.mult, ALU.add)
                P.mm(psb[:, 1:2], Gp, incl[:, TOK - 1:TOK])
                P.cp(tmp, psb[:, 1:2])
                P.stt(incl, incl, tmp, selb, ALU.add, ALU.mult)
                P.ts(incl, incl, -1.0, None, ALU.add)
                for j in range(16):
                    P.mm(psr[0], incl[:, j * 128:(j + 1) * 128], sM)
                    P.cp(rkM[:, j, :], psr[0])
                    P.mm(psr[1], A[:, j * 128:(j + 1) * 128], sM)
                    P.cp(afM[:, j, :], psr[1])
            af2 = afM.re("p j c -> p (j c)")
            V3 = VALS
            P.cp(V3[:, :, 0], vals0[:, 0:256])
            P.cp(V3[:, :, 1], vals0[:, 256:512])
            P.cp(gb, af2)
            P.cp(V3[:, :, 2], gb)
            P.tt(res, af2, gb, ALU.subtract)
            P.cp(gb, res)
            P.cp(V3[:, :, 3], gb)
            P.tt(res, res, gb, ALU.subtract)
            P.cp(V3[:, :, 4], res)
            P.cp(V3[:, :, 5], vals0[:, 512:768])
            P.memset(V3[:, :, 6:8], 0.0)
            with ExitStack() as st2:
                pI = [ps(st2, "pI%d" % g, [128, 512]) for g in range(8)]
                n = 0
                for i in range(4):
                    cnt = 0
                    for r in range(4):
                        for j in range(16):
                            col = r * 4 + i
                            o_ = oh[n % 2]
                            P.ts(o_, iot, rkM[:, j, col:col + 1], None, ALU.is_equal)
                            n += 1
                            for g in range(8):
                                P.mm(pI[g][:, 0:8], o_[:, g * 128:(g + 1) * 128], V3[:, j * 16 + col, :], start=(cnt == 0), stop=(cnt == 63))
                            cnt += 1
                    for g in range(8):
                        P.cp(idxf[:, g, :], pI[g][:, 0:8])
                    P.stt(tokf, idxf[:, :, 0], 128.0, idxf[:, :, 1], ALU.mult, ALU.add)
                    P.cp(idx_i[:, i, :], tokf)
                    P.stt(tokf, idxf[:, :, 5], 128.0, idxf[:, :, 1], ALU.mult, ALU.add)
                    P.cp(idx_t[:, i, :], tokf)
                    P.tt(gate[:, i, :], idxf[:, :, 2], idxf[:, :, 3], ALU.add)
                    P.tt(gate[:, i, :], gate[:, i, :], idxf[:, :, 4], ALU.add)
        if debug:
            dbt = sb(rt, "dbt", [128, 64])
            P.cp(dbt[:, 0:32], idx_t.re("p a b -> p (a b)"))
            P.cp(dbt[:, 32:64], gate.re("p a b -> p (a b)"))
            P.ld(dbg_idx, dbt)
        P.barrier()
        stop("r")
        with ExitStack() as st:
            xsT = sb(st, "xsT", [128, KC, 1024], BF16)
            hidT = sb(st, "hidT", [128, NFC, 1024], BF16)
            wdb = sb(st, "wdb", [128, NFC, D], BF16)
            wgb = [sb(st, "wgb%d" % i, [128, KC, 256], BF16) for i in range(2)]
            wub = [sb(st, "wub%d" % i, [128, KC, 256], BF16) for i in range(2)]
            xg = [sb(st, "xg%d" % i, [128, D], BF16) for i in range(2)]
            yt = [sb(st, "yt%d" % i, [128, D]) for i in range(2)]
            sil = [sb(st, "sil%d" % i, [128, 512]) for i in range(2)]
            pT = ps(st, "pTx", [128, 1024], BF16)
            pg = [ps(st, "pg%d" % i, [128, 512]) for i in range(2)]
            pu = [ps(st, "pu%d" % i, [128, 512]) for i in range(2)]
            py = [ps(st, "py%d" % i, [128, 512]) for i in range(2)]
            nq = 0
            for i in range(4):
                for g in range(8):
                    P.gather(xg[g % 2], h2_all, idx_i[:, i, g:g + 1])
                    for kc in range(KC):
                        P.tr(pT[:, kc * 128:(kc + 1) * 128], xg[g % 2][:, kc * 128:(kc + 1) * 128], idb)
                    P.act(xsT[:, :, g * 128:(g + 1) * 128], pT.re("p (c n) -> p c n", n=128), AF.Copy)
                wgv = wg4[i].re("(kc p) f -> p kc f", p=128)
                wuv = wu4[i].re("(kc p) f -> p kc f", p=128)
                for fb in range(11):
                    f0 = fb * 256
                    fn = min(256, DE - f0)
                    gb_, ub_ = wgb[fb % 2], wub[fb % 2]
                    P.ld(gb_[:, :, 0:fn], wgv[:, :, f0:f0 + fn], q="pool")
                    P.ld(ub_[:, :, 0:fn], wuv[:, :, f0:f0 + fn], q="pool")
                    for fc_ in (2 * fb, 2 * fb + 1):
                        m_ = 128 if fc_ < 21 else 64
                        P.ld(wdb[0:m_, fc_, :], wd4[i][fc_ * 128:fc_ * 128 + m_, :], q="pool")
                    for c in range((fn + 127) // 128):
                        fc = fb * 2 + c
                        m = min(128, fn - c * 128)
                        for half in range(2):
                            a_, b_ = pg[nq % 2], pu[nq % 2]
                            s_ = sil[nq % 2]
                            nq += 1
                            for kc in range(KC):
                                P.mm(a_[0:m, :], gb_[:, kc, c * 128:c * 128 + m], xsT[:, kc, half * 512:(half + 1) * 512], start=(kc == 0), stop=(kc == KC - 1))
                            for kc in range(KC):
                                P.mm(b_[0:m, :], ub_[:, kc, c * 128:c * 128 + m], xsT[:, kc, half * 512:(half + 1) * 512], start=(kc == 0), stop=(kc == KC - 1))
                            P.act(s_[0:m, :], a_[0:m, :], AF.Silu)
                            P.tt(hidT[0:m, fc, half * 512:(half + 1) * 512], s_[0:m, :], b_[0:m, :], ALU.mult)
                for ct in range(8):
                    y_ = yt[ct % 2]
                    for nb in range(2):
                        p_ = py[nb]
                        for fc in range(NFC):
                            m = 128 if fc < 21 else 64
                            P.mm(p_, hidT[0:m, fc, ct * 128:(ct + 1) * 128], wdb[0:m, fc, nb * 512:(nb + 1) * 512], start=(fc == 0), stop=(fc == NFC - 1))
                        P.ts(y_[:, nb * 512:(nb + 1) * 512], p_, gate[:, i, ct:ct + 1], None, ALU.mult)
                    P.scatter_add(acc, y_, idx_t[:, i, ct:ct + 1])
        rt.close()
        P.cc("ReduceScatter", ALU.add, acc, rs_out)
        with ExitStack() as st:
            mt = [sb(st, "mt%d" % i, [128, D]) for i in range(2)]
            x1l = [sb(st, "x1l%d" % i, [128, D]) for i in range(2)]
            junk = sb(st, "junk3", [128, D])
            sm = sb(st, "fsm", [128, 4])
            for i in range(NTO):
                j2 = i % 2
                tsl = slice(i * 128, (i + 1) * 128)
                P.ld(mt[j2], rs_out[tsl, :])
                P.ld(x1l[j2], x1_d[tsl, :])
                P.act(junk, mt[j2], AF.Square, accum=sm[:, 0:1])
                P.act(sm[:, 1:2], sm[:, 0:1], AF.Sqrt, bias=epsc[:, 0:1], scale=1.0 / D)
                P.recip(sm[:, 1:2], sm[:, 1:2])
                P.stt(mt[j2], mt[j2], sm[:, 1:2], gt2g, ALU.mult, ALU.mult)
                P.tt(mt[j2], mt[j2], x1l[j2], ALU.add)
                P.ld(out[tsl, :], mt[j2])
    except _Stop:
        pass
    for stx in reversed(open_stacks):
        stx.close()
    P.barrier()
    top.close()
    return nc


def _consts():
    f = np.float32
    p = np.arange(128)
    cst = {}
    cst["identf"] = np.eye(128, dtype=f)
    sidx, cidx = p[:, None], p[None, :]
    same = (sidx // 32) == (cidx // 32)
    cst["maskA"] = np.concatenate([(same & (cidx >= sidx)), (same & (cidx <= sidx))], axis=1).astype(f)
    cst["rowmask"] = ((p[:, None] // 32) == np.arange(4)[None, :]).astype(f)
    rm = np.ones((128, TOK), f)
    rm[:, 0::32] = 0.0
    cst["resetm"] = rm
    cst["iota1k"] = np.broadcast_to(np.arange(1024, dtype=f)[None, :], (128, 1024)).copy()
    tok = np.zeros((128, 768), f)
    for j in range(16):
        for col in range(16):
            r = col // 4
            tok[:, j * 16 + col] = (j // 4) * 16 + r * 4 + (j % 4)
            tok[:, 512 + j * 16 + col] = r * 16 + j
    tok[:, 256:512] = p[:, None].astype(f)
    cst["tokc"] = tok
    re_ = np.arange(64)
    r_, e_ = re_ // 16, re_ % 16
    cst["Gm"] = (e_[:, None] == e_[None, :]).astype(f)
    cst["Gpre"] = ((e_[:, None] == e_[None, :]) & (r_[:, None] < r_[None, :])).astype(f)
    return cst


def prep(x, c, ctx, c_ctx, w_mod, b_mod, g_pre1, g_post1, g_pre2, g_post2, w_in, w_out,
         na_rpb, hg_lb_logits, hg_norm, w_router, w_gate, w_up, w_down):
    f = np.float32
    A_ = lambda a: np.ascontiguousarray(np.asarray(a, dtype=f))
    x, c, ctx, c_ctx = A_(x), A_(c), A_(ctx), A_(c_ctx)
    w_mod, b_mod, w_in, w_out = A_(w_mod)[0], A_(b_mod)[0], A_(w_in)[0], A_(w_out)[0]
    rpb = A_(na_rpb)[0]
    lbl_ = A_(hg_lb_logits)
    hgn_ = A_(hg_norm)[0]
    wr_ = A_(w_router)[0]
    wg_, wu_, wd_ = np.asarray(w_gate)[0], np.asarray(w_up)[0], np.asarray(w_down)[0]
    bc = lambda v: np.ascontiguousarray(np.broadcast_to(v[None, :], (128, v.shape[0])))
    pp = np.arange(512)
    perm = np.where(pp % 32 < 16, pp + 16, pp - 16)
    winx = np.ascontiguousarray(np.concatenate([w_in, w_in[:, 0:512][:, perm], w_in[:, 512:1024][:, perm]], axis=1))
    cst = _consts()
    shared = dict(cst)
    shared.update(wmod=w_mod, bmodb=bc(b_mod), g1b=bc(A_(g_pre1)[0]), gp1b=bc(A_(g_post1)[0]),
                  g2b=bc(A_(g_pre2)[0]), gp2b=bc(A_(g_post2)[0]), winx=winx, wout=w_out,
                  hgnb=bc(np.tile(hgn_, 4)), wr=wr_,
                  lbl=np.ascontiguousarray(lbl_.reshape(2, 4, 128).transpose(2, 0, 1).reshape(128, 8)))
    ccrep = np.ascontiguousarray(np.broadcast_to(c_ctx.reshape(KC, 128).T[:, :, None], (128, KC, 128)).reshape(128, KC * 128))
    d64 = np.arange(128) % 64
    seg = d64 // 32
    first = (d64 % 32) < 16
    inv = (10000.0 ** (-(2.0 * (d64 % 16)) / 32.0)).astype(f)
    in_maps = []
    for core in range(8):
        b, s = core // 4, core % 4
        m = dict(shared)
        xh = np.zeros((HTOK, D), f)
        g0 = TOK * s - 256
        lo, hi = max(g0, 0), min(g0 + HTOK, 8192)
        xh[lo - g0:hi - g0] = x[b, lo:hi]
        m["xh"] = xh
        m["ctxb"] = ctx[b]
        m["crep"] = np.ascontiguousarray(np.broadcast_to(c[b].reshape(KC, 128).T[:, :, None], (128, KC, 128)).reshape(128, KC * 128))
        m["ccrep"] = ccrep
        tl = np.arange(HTOK)
        row = (32 * s - 4 + tl // 64).astype(f)
        colp = (tl % 64).astype(f)
        pos = np.where(seg[:, None] == 0, row[None, :], colp[None, :]).astype(f)
        ang = (pos * inv[:, None]).astype(f)
        m["cosT"] = np.cos(ang).astype(f)
        m["sinT"] = np.where(first[:, None], -np.sin(ang), np.sin(ang)).astype(f)
        mk = np.zeros((8, 128, 5, 1024), f)
        qp = np.arange(128)
        a_, qc = qp // 64, qp % 64
        nn = np.arange(768)
        for cls, rp in enumerate((0, 1, 4, 14, 15)):
            brow = 0 if rp <= 1 else 28 if rp >= 14 else 2 * rp
            r = 32 * s + 2 * rp + a_
            grow = 32 * s - 4 + brow + nn // 64
            kc_ = nn % 64
            r0 = np.clip(r - 4, 0, 120)
            vrow = (grow[None, :] >= r0[:, None]) & (grow[None, :] < r0[:, None] + 8)
            wc0 = np.clip(qc - 8, 0, 48)
            vcol = (kc_[None, :] >= wc0[:, None]) & (kc_[None, :] < wc0[:, None] + 16)
            dr = np.clip(grow[None, :] - r[:, None] + 7, 0, 14)
            dc = np.clip(kc_[None, :] - qc[:, None] + 15, 0, 30)
            bias = rpb[:, dr, dc]
            mk[:, :, cls, 256:1024] = np.where((vrow & vcol)[None], bias, f(NEG))
        m["masks"] = mk.reshape(8, 128, 5 * 1024)
        mf = np.zeros((128, 8), f)
        for j in range(4):
            mf[:, j] = 1.0 if j < s else 0.0
            mf[:, 4 + j] = 1.0 if j > s else 0.0
        m["mfold"] = mf
        sel = np.zeros((64, 16), f)
        for r in range(4):
            for i in range(4):
                sel[r * 16 + 4 * s + i, r * 4 + i] = 1.0
        m["selM"] = sel
        m["selO"] = np.zeros((64, 16), f)
        m["wg4"] = np.ascontiguousarray(wg_[4 * s:4 * s + 4], dtype=f)
        m["wu4"] = np.ascontiguousarray(wu_[4 * s:4 * s + 4], dtype=f)
        m["wd4"] = np.ascontiguousarray(wd_[4 * s:4 * s + 4], dtype=f)
        in_maps.append(m)
    return in_maps


def kernel(**inputs):
    f = np.float32
    in_maps = prep(**inputs)
    import os
    dbg = os.environ.get("KDEBUG", "") == "1"
    nc = build(debug=dbg)
    res = run_bass_kernel_spmd(nc, in_maps, core_ids=list(range(8)))
    if dbg:
        global LAST
        LAST = res.results
    out = np.zeros((2, 8192, D), f)
    for core in range(8):
        b, s = core // 4, core % 4
        out[b, TOK * s:TOK * (s + 1)] = res.results[core]["out"]
    return out
```

```python
import numpy as np
from contextlib import ExitStack
import concourse.bass as bass
import concourse.mybir as mybir
from concourse.bass_utils import run_bass_kernel_spmd

F32 = mybir.dt.float32
BF16 = mybir.dt.bfloat16
I32 = mybir.dt.int32
ALU = mybir.AluOpType
AF = mybir.ActivationFunctionType
AX = mybir.AxisListType

D = 1024
KC = 8
NTO = 16
NTH = 20
OWN0 = 2
TOK = 2048
HTOK = 2560
CTX = 256
DE = 2752
NFC = 22
EPS = 1e-6
GROUPS = [[0, 1, 2, 3], [4, 5, 6, 7]]
NDS = 24
NEG = -30000.0


class _Stop(Exception):
    pass


class V:
    def __init__(s, ap, key):
        s.ap = ap
        s.key = key

    def __getitem__(s, idx):
        return V(s.ap[idx], s.key)

    def sub(s, k):
        return V(s.ap, s.key + "/" + str(k))

    def re(s, pat, **kw):
        return V(s.ap.rearrange(pat, **kw), s.key)

    def bc(s, shape):
        return V(s.ap.to_broadcast(shape), s.key)


def _ap(x):
    return x.ap if isinstance(x, V) else x


def _keys(*xs):
    return [x.key for x in xs if isinstance(x, V)]


def _ovl(a, b):
    return a == b or a.startswith(b + "/") or b.startswith(a + "/")


class Prog:
    def __init__(s, nc, st):
        s.nc = nc
        s.E = {"pe": nc.tensor, "act": nc.scalar, "dve": nc.vector, "pool": nc.gpsimd, "sp": nc.sync}
        s.sems = []
        s.esem = {}
        s.ecnt = {}
        for e in ["pe", "act", "dve", "pool"]:
            s.esem[e] = s._new(st, "s_" + e)
            s.ecnt[e] = 0
        s.dq = {}
        for q in ["sp", "pool"]:
            s.dq[q] = {"sems": [s._new(st, "d_%s%d" % (q, i)) for i in range(NDS)], "use": [0] * NDS, "nxt": 0}
        s.ccsem = s._new(st, "ccs")
        s.cccnt = 0
        s.waited = {e: {} for e in s.E}
        s.bufs = {}
        s.alltok = {}
        s.halt = False

    def _new(s, st, name):
        s.sems.append(st.enter_context(s.nc.semaphore(name)))
        return len(s.sems) - 1

    def _collect(s, reads, writes):
        t = {}

        def add(d):
            for k, v in d.items():
                if t.get(k, 0) < v:
                    t[k] = v
        for k in reads:
            for k2, stt in s.bufs.get(k.split("/")[0], {}).items():
                if _ovl(k, k2):
                    add(stt["w"])
        for k in writes:
            for k2, stt in s.bufs.get(k.split("/")[0], {}).items():
                if _ovl(k, k2):
                    add(stt["w"])
                    add(stt["r"])
        return t

    def _update(s, reads, writes, tok):
        for k in reads:
            stt = s.bufs.setdefault(k.split("/")[0], {}).setdefault(k, {"w": {}, "r": {}})
            if stt["r"].get(tok[0], 0) < tok[1]:
                stt["r"][tok[0]] = tok[1]
        for k in writes:
            d = s.bufs.setdefault(k.split("/")[0], {})
            for k2 in list(d):
                if k2 != k and k2.startswith(k + "/"):
                    del d[k2]
            d[k] = {"w": {tok[0]: tok[1]}, "r": {}}
        if s.alltok.get(tok[0], 0) < tok[1]:
            s.alltok[tok[0]] = tok[1]

    def _wait(s, eng, toks, skip=None):
        e = s.E[eng]
        for sm, v in toks.items():
            if sm == skip:
                continue
            if s.waited[eng].get(sm, 0) < v:
                e.wait_ge(s.sems[sm], v)
                s.waited[eng][sm] = v

    def op(s, eng, fn, reads=(), writes=()):
        if s.halt:
            return
        if eng != "pe":
            pr = [k for k in reads if k.startswith("PS")]
            if pr:
                writes = list(writes) + pr
        toks = s._collect(reads, writes)
        s._wait(eng, toks, skip=s.esem[eng] if eng == "pe" else None)
        ins = fn()
        s.ecnt[eng] += 1
        ins.then_inc(s.sems[s.esem[eng]], 1)
        s._update(reads, writes, (s.esem[eng], s.ecnt[eng]))

    def dma(s, q, fn, reads=(), writes=()):
        if s.halt:
            return
        dq = s.dq[q]
        i = dq["nxt"]
        dq["nxt"] = (i + 1) % NDS
        sm = dq["sems"][i]
        toks = s._collect(reads, writes)
        if dq["use"][i] > 0 and toks.get(sm, 0) < 16 * dq["use"][i]:
            toks[sm] = 16 * dq["use"][i]
        s._wait(q, toks)
        ins = fn()
        dq["use"][i] += 1
        ins.then_inc(s.sems[sm], 16)
        s._update(reads, writes, (sm, 16 * dq["use"][i]))

    def cc(s, kind, op, src, dst):
        if s.halt:
            return
        import os
        if os.environ.get("KNOCC", "") == "1" or (os.environ.get("KNOCC", "") == kind):
            n = min(src.ap.shape[0], dst.ap.shape[0])
            s.ld(dst[0:n, :], src[0:n, :])
            return
        toks = s._collect([src.key], [dst.key])
        s._wait("pool", toks)
        ins = s.nc.gpsimd.collective_compute(kind, op, replica_groups=GROUPS, ins=[src.ap.opt()], outs=[dst.ap.opt()])
        s.cccnt += 1
        ins.then_inc(s.sems[s.ccsem])
        s._update([src.key], [dst.key], (s.ccsem, s.cccnt))

    def barrier(s):
        if s.halt:
            return
        for eng in s.E:
            s._wait(eng, dict(s.alltok), skip=None)

    def mm(s, out, lhsT, rhs, start=True, stop=True):
        s.op("pe", lambda: s.nc.tensor.matmul(_ap(out), _ap(lhsT), _ap(rhs), start=start, stop=stop),
             _keys(lhsT, rhs), _keys(out))

    def tr(s, out, in_, ident):
        s.op("pe", lambda: s.nc.tensor.transpose(_ap(out), _ap(in_), _ap(ident)), _keys(in_, ident), _keys(out))

    def act(s, out, in_, func, bias=None, scale=1.0, accum=None):
        kw = {}
        if bias is not None:
            kw["bias"] = _ap(bias)
        if accum is not None:
            kw["accum_out"] = _ap(accum)
        s.op("act", lambda: s.nc.scalar.activation(out=_ap(out), in_=_ap(in_), func=func, scale=_ap(scale), **kw),
             _keys(in_, bias, scale), _keys(out, accum))

    def tt(s, out, a, b, op, eng="dve"):
        e = s.E[eng]
        s.op(eng, lambda: e.tensor_tensor(out=_ap(out), in0=_ap(a), in1=_ap(b), op=op), _keys(a, b), _keys(out))

    def ts(s, out, a, s1, s2, op0, op1=None, accum=None, eng="dve"):
        e = s.E[eng]
        kw = {}
        if accum is not None:
            kw["accum_out"] = _ap(accum)
        if op1 is None:
            s.op(eng, lambda: e.tensor_scalar(out=_ap(out), in0=_ap(a), scalar1=_ap(s1), scalar2=None, op0=op0, **kw),
                 _keys(a, s1), _keys(out, accum))
        else:
            s.op(eng, lambda: e.tensor_scalar(out=_ap(out), in0=_ap(a), scalar1=_ap(s1), scalar2=_ap(s2), op0=op0, op1=op1, **kw),
                 _keys(a, s1, s2), _keys(out, accum))

    def stt(s, out, a, sc, b, op0, op1, eng="dve"):
        e = s.E[eng]
        s.op(eng, lambda: e.scalar_tensor_tensor(out=_ap(out), in0=_ap(a), scalar=_ap(sc), in1=_ap(b), op0=op0, op1=op1),
             _keys(a, sc, b), _keys(out))

    def cp(s, out, in_, eng="dve"):
        e = s.E[eng]
        s.op(eng, lambda: e.tensor_copy(out=_ap(out), in_=_ap(in_)), _keys(in_), _keys(out))

    def memset(s, out, val, eng="pool"):
        e = s.E[eng]
        s.op(eng, lambda: e.memset(_ap(out), val), [], _keys(out))

    def rmax(s, out, in_):
        s.op("dve", lambda: s.nc.vector.reduce_max(out=_ap(out), in_=_ap(in_), axis=AX.X), _keys(in_), _keys(out))

    def recip(s, out, in_):
        s.op("dve", lambda: s.nc.vector.reciprocal(out=_ap(out), in_=_ap(in_)), _keys(in_), _keys(out))

    def scan(s, out, d0, d1, init, op0, op1):
        s.op("dve", lambda: s.nc.vector.tensor_tensor_scan(out=_ap(out), data0=_ap(d0), data1=_ap(d1), initial=init, op0=op0, op1=op1),
             _keys(d0, d1), _keys(out))

    def ld(s, out, in_, q="sp"):
        e = s.E[q]
        s.dma(q, lambda: e.dma_start(out=_ap(out), in_=_ap(in_)), _keys(in_), _keys(out))

    def gather(s, out, src, idx):
        s.dma("pool", lambda: s.nc.gpsimd.indirect_dma_start(
            out=_ap(out), out_offset=None, in_=_ap(src),
            in_offset=bass.IndirectOffsetOnAxis(ap=_ap(idx), axis=0)), _keys(src, idx), _keys(out))

    def scatter_add(s, dst, src, idx):
        s.dma("pool", lambda: s.nc.gpsimd.indirect_dma_start(
            out=_ap(dst), out_offset=bass.IndirectOffsetOnAxis(ap=_ap(idx), axis=0),
            in_=_ap(src), in_offset=None, compute_op=ALU.add), _keys(src, idx, dst), _keys(dst))


def build(debug=False):
    nc = bass.Bass("TRN2", target_bir_lowering=False)
    top = ExitStack()
    P = Prog(nc, top)

    import os
    KSTOP = os.environ.get("KSTOP", "")
    open_stacks = []

    def stop(tag):
        if KSTOP == tag:
            P.barrier()
            P.halt = True

    def din(name, shape, dt=F32):
        return V(nc.dram_tensor(name, list(shape), dt, kind="ExternalInput").ap(), name)

    def dscr(name, shape, dt=F32):
        return V(nc.dram_tensor(name, list(shape), dt).ap(), name)

    uq = [0]

    def sb(st, name, shape, dt=F32, side=None):
        uq[0] += 1
        name = "%s_%d" % (name, uq[0])
        return V(st.enter_context(nc.sbuf_tensor(name, list(shape), dt, side=side))[:], name)

    def ps(st, name, shape, dt=F32):
        uq[0] += 1
        name = "PS%s_%d" % (name, uq[0])
        return V(st.enter_context(nc.psum_tensor(name, list(shape), dt))[:], name)

    xh = din("xh", [HTOK, D])
    ctxb = din("ctxb", [CTX, D])
    crep = din("crep", [128, KC * 128])
    ccrep = din("ccrep", [128, KC * 128])
    wmod = din("wmod", [D, 6 * D])
    bmodb = din("bmodb", [128, 6 * D])
    g1b = din("g1b", [128, D])
    gp1b = din("gp1b", [128, D])
    g2b = din("g2b", [128, D])
    gp2b = din("gp2b", [128, D])
    winx = din("winx", [D, 5120])
    wout = din("wout", [D, D])
    cosT = din("cosT", [128, HTOK])
    sinT = din("sinT", [128, HTOK])
    masks = din("masks", [8, 128, 5 * 1024])
    lbl = din("lbl", [128, 8])
    hgnb = din("hgnb", [128, 512])
    wr = din("wr", [D, 16])
    mfold = din("mfold", [128, 8])
    wg4 = din("wg4", [4, D, DE])
    wu4 = din("wu4", [4, D, DE])
    wd4 = din("wd4", [4, DE, D])
    selM = din("selM", [64, 16])
    selO = din("selO", [64, 16])
    Gm = din("Gm", [64, 64])
    Gpre = din("Gpre", [64, 64])
    identf = din("identf", [128, 128])
    maskA = din("maskA", [128, 256])
    rowmask = din("rowmask", [128, 4])
    resetm = din("resetm", [128, TOK])
    iota1k = din("iota1k", [128, 1024])
    tokc = din("tokc", [128, 768])
    out = V(nc.dram_tensor("out", [TOK, D], F32, kind="ExternalOutput").ap(), "out")
    if debug:
        dbg_x1 = V(nc.dram_tensor("dbg_x1", [TOK, D], F32, kind="ExternalOutput").ap(), "dbg_x1")
        dbg_mix = V(nc.dram_tensor("dbg_mix", [TOK, D], F32, kind="ExternalOutput").ap(), "dbg_mix")
        dbg_aff = V(nc.dram_tensor("dbg_aff", [16, TOK], F32, kind="ExternalOutput").ap(), "dbg_aff")
        dbg_idx = V(nc.dram_tensor("dbg_idx", [128, 64], F32, kind="ExternalOutput").ap(), "dbg_idx")

    na_d = dscr("na_d", [TOK, 512], BF16)
    sg_d = dscr("sg_d", [TOK, 512], BF16)
    x1_d = dscr("x1_d", [TOK, D])
    h2_in = dscr("h2_in", [TOK, D], BF16)
    h2_all = dscr("h2_all", [4 * TOK, D], BF16)
    st_in = dscr("st_in", [1032, 128])
    st_all = dscr("st_all", [4 * 1032, 128])
    af_in = dscr("af_in", [16, TOK])
    af_all = dscr("af_all", [64, TOK])
    acc = dscr("acc", [4 * TOK, D])
    rs_out = dscr("rs_out", [TOK, D])

    try:
        idf = sb(top, "idf", [128, 128])
        idb = sb(top, "idb", [128, 128], BF16)
        mst = ExitStack()
        gt1g = sb(top, "gt1g", [128, D])
        a2 = sb(top, "a2", [128, D])
        sh2 = sb(top, "sh2", [128, D])
        gt2g = sb(top, "gt2g", [128, D])
        cols = sb(top, "cols", [128, 32])
        zero4k = sb(top, "zero4k", [128, D])
        epsc = sb(top, "epsc", [128, 1])
        modB = sb(mst, "modB", [128, 6 * D])
        P.memset(epsc, EPS)
        P.ld(idf, identf)
        P.cp(idb, idf)
        P.memset(zero4k, 0.0)
        accv = acc.re("(n p) d -> n p d", p=128)
        for n in range(64):
            P.ld(accv[n], zero4k)

        with ExitStack() as st:
            sc_ = sb(st, "siluc", [128, KC * 128])
            scc = sb(st, "silucc", [128, KC * 128])
            modC = sb(st, "modC", [128, 2 * D])
            wmb = [sb(st, "wmb%d" % i, [128, KC, 512]) for i in range(2)]
            bmb = [sb(st, "bmb%d" % i, [128, 512]) for i in range(2)]
            pm = [ps(st, "pm%d" % i, [128, 512]) for i in range(2)]
            tmpb = sb(st, "tmpb", [128, D])
            ptr = ps(st, "ptr", [128, 128])
            P.ld(sc_, crep)
            P.ld(scc, ccrep)
            P.act(sc_, sc_, AF.Silu)
            P.act(scc, scc, AF.Silu)
            wmv = wmod.re("(kc p) n -> p kc n", p=128)
            for nb in range(12):
                wb = wmb[nb % 2]
                P.ld(wb, wmv[:, :, nb * 512:(nb + 1) * 512])
                P.ld(bmb[nb % 2], bmodb[:, nb * 512:(nb + 1) * 512])
                for kc in range(KC):
                    P.mm(pm[0], sc_[:, kc * 128:(kc + 1) * 128], wb[:, kc, :], start=(kc == 0), stop=(kc == KC - 1))
                P.tt(modB[:, nb * 512:(nb + 1) * 512], pm[0], bmb[nb % 2], ALU.add)
                if nb < 4:
                    for kc in range(KC):
                        P.mm(pm[1], scc[:, kc * 128:(kc + 1) * 128], wb[:, kc, :], start=(kc == 0), stop=(kc == KC - 1))
                    P.tt(modC[:, nb * 512:(nb + 1) * 512], pm[1], bmb[nb % 2], ALU.add)
            g1t = sb(st, "g1t", [128, D])
            P.ld(g1t, g1b)
            for (src_sh, src_sc, c0) in ((modB[:, 0:D], modB[:, D:2 * D], 0), (modC[:, 0:D], modC[:, D:2 * D], 16)):
                P.stt(tmpb, src_sc, 1.0, g1t, ALU.add, ALU.mult)
                for kc in range(KC):
                    P.tr(ptr, tmpb[:, kc * 128:(kc + 1) * 128], idf)
                    P.cp(cols[:, c0 + kc:c0 + kc + 1], ptr[:, 0:1])
                    P.tr(ptr, src_sh[:, kc * 128:(kc + 1) * 128], idf)
                    P.cp(cols[:, c0 + 8 + kc:c0 + 8 + kc + 1], ptr[:, 0:1])
            P.ld(tmpb, gp1b)
            P.tt(gt1g, modB[:, 2 * D:3 * D], tmpb, ALU.mult)
            P.ld(tmpb, g2b)
            P.stt(a2, modB[:, 4 * D:5 * D], 1.0, tmpb, ALU.add, ALU.mult)
            P.cp(sh2, modB[:, 3 * D:4 * D])
            P.ld(tmpb, gp2b)
            P.tt(gt2g, modB[:, 5 * D:6 * D], tmpb, ALU.mult)
        P.barrier()
        mst.close()
        stop("a")

        s1 = ExitStack()
        open_stacks.append(s1)
        hT = sb(s1, "hT", [128, KC, HTOK], BF16, side="right")
        hcT = sb(s1, "hcT", [128, KC, CTX], BF16, side="right")
        with ExitStack() as st:
            xt = [sb(st, "xt%d" % i, [128, D]) for i in range(2)]
            xs = [sb(st, "xs%d" % i, [128, D], BF16) for i in range(2)]
            junk = sb(st, "junk", [128, D])
            ss = sb(st, "ss", [128, 2])
            pt = [ps(st, "pt%d" % i, [128, D], BF16) for i in range(2)]
            xhv = xh.re("(n p) d -> n p d", p=128)
            cxv = ctxb.re("(n p) d -> n p d", p=128)
            for i in range(NTH + 2):
                isctx = i >= NTH
                src = cxv[i - NTH] if isctx else xhv[i]
                x_ = xt[i % 2]
                P.ld(x_, src)
                P.act(junk, x_, AF.Square, accum=ss[:, 0:1])
                P.act(ss[:, 1:2], ss[:, 0:1], AF.Sqrt, bias=epsc[:, 0:1], scale=1.0 / D)
                P.recip(ss[:, 1:2], ss[:, 1:2])
                P.ts(xs[i % 2], x_, ss[:, 1:2], None, ALU.mult)
                for kc in range(KC):
                    P.tr(pt[i % 2][:, kc * 128:(kc + 1) * 128], xs[i % 2][:, kc * 128:(kc + 1) * 128], idb)
                c0 = 16 if isctx else 0
                for kc in range(KC):
                    dst = hcT[:, kc, (i - NTH) * 128:(i - NTH + 1) * 128] if isctx else hT[:, kc, i * 128:(i + 1) * 128]
                    if kc % 2 == 0:
                        P.act(dst, pt[i % 2][:, kc * 128:(kc + 1) * 128], AF.Identity,
                              bias=cols[:, c0 + 8 + kc:c0 + 9 + kc], scale=cols[:, c0 + kc:c0 + kc + 1])
                    else:
                        P.ts(dst, pt[i % 2][:, kc * 128:(kc + 1) * 128], cols[:, c0 + kc:c0 + kc + 1],
                             cols[:, c0 + 8 + kc:c0 + 9 + kc], ALU.mult, ALU.add)
        P.barrier()
        stop("b")

        winv = winx.re("(kc p) n -> p kc n", p=128)

        def load_w(st_w, wst, wbf, blk, ncols=512, col0=None):
            c0 = blk * 512 if col0 is None else col0
            for kc in range(KC):
                P.ld(wst[kc % 2][:, 0:ncols], winv[:, kc, c0:c0 + ncols])
                P.cp(wbf[:, kc, 0:ncols], wst[kc % 2][:, 0:ncols], eng="pool")

        with ExitStack() as st:
            na_tok = sb(st, "na_tok", [128, NTO, 512], BF16)
            for half in range(2):
                with ExitStack() as sth:
                    qT = sb(sth, "qT", [128, 2, TOK], BF16)
                    qrT = sb(sth, "qrT", [128, 2, TOK], BF16)
                    krT = sb(sth, "krT", [128, 2, HTOK], BF16)
                    vtk = sb(sth, "vtk", [128, NTH, 256], BF16)
                    kcT = sb(sth, "kcT", [128, 2, CTX], BF16)
                    vck = sb(sth, "vck", [128, 2, 256], BF16)
                    with ExitStack() as st2:
                        wst = [sb(st2, "wst%d" % i, [128, 512]) for i in range(2)]
                        wAs = [sb(st2, "wA%d" % i_, [128, KC, 256], BF16) for i_ in range(2)]
                        wBs = [sb(st2, "wB%d" % i_, [128, KC, 256], BF16) for i_ in range(2)]
                        cs = sb(st2, "cs", [128, HTOK])
                        sn = sb(st2, "sn", [128, HTOK])
                        t1 = sb(st2, "t1", [128, 512])
                        t2 = sb(st2, "t2", [128, 512])
                        pa = [ps(st2, "pa%d" % i, [128, 512]) for i in range(2)]
                        pb = [ps(st2, "pb%d" % i, [128, 512]) for i in range(2)]
                        P.ld(cs, cosT)
                        P.ld(sn, sinT)
                        for (blk, pblk, ntok, tok0, dstp, dstr) in ((0, 8, TOK, OWN0 * 128, qT, qrT), (1, 9, HTOK, 0, None, krT)):
                            wA, wB = wAs[blk], wBs[blk]
                            load_w(st2, wst, wA, blk, 256, blk * 512 + half * 256)
                            load_w(st2, wst, wB, pblk, 256, pblk * 512 + half * 256)
                            if half == 0 and blk == 0:
                                stop("c1a")
                            it = 0
                            for hp in range(2):
                                for nb in range(ntok // 512):
                                    a_, b_ = pa[it % 2], pb[it % 2]
                                    it += 1
                                    tk = tok0 + nb * 512
                                    for kc in range(KC):
                                        P.mm(a_, wA[:, kc, hp * 128:(hp + 1) * 128], hT[:, kc, tk:tk + 512], start=(kc == 0), stop=(kc == KC - 1))
                                    for kc in range(KC):
                                        P.mm(b_, wB[:, kc, hp * 128:(hp + 1) * 128], hT[:, kc, tk:tk + 512], start=(kc == 0), stop=(kc == KC - 1))
                                    KV = os.environ.get("KVAR", "")
                                    if dstp is not None and KV not in ("1", "3"):
                                        P.act(dstp[:, hp, nb * 512:(nb + 1) * 512], a_, AF.Copy)
                                    if KV not in ("2", "3"):
                                        P.tt(t1, a_, cs[:, tk:tk + 512], ALU.mult)
                                        P.tt(t2, b_, sn[:, tk:tk + 512], ALU.mult)
                                        P.tt(dstr[:, hp, nb * 512:(nb + 1) * 512], t1, t2, ALU.add)
                            if half == 0 and blk == 0:
                                stop("c1b")
                            if half == 0 and blk == 1:
                                stop("c1c")
                        for hp in range(2):
                            for kc in range(KC):
                                P.mm(pa[0][:, 0:CTX], wA[:, kc, hp * 128:(hp + 1) * 128], hcT[:, kc, :], start=(kc == 0), stop=(kc == KC - 1))
                            P.cp(kcT[:, hp, :], pa[0][:, 0:CTX])
                        wA = wAs[0]
                        load_w(st2, wst, wA, 2, 256, 2 * 512 + half * 256)
                        for i in range(NTH + 2):
                            a_ = pa[i % 2]
                            for kc in range(KC):
                                lhs = hcT[:, kc, (i - NTH) * 128:(i - NTH + 1) * 128] if i >= NTH else hT[:, kc, i * 128:(i + 1) * 128]
                                P.mm(a_[:, 0:256], lhs, wA[:, kc, 0:256], start=(kc == 0), stop=(kc == KC - 1))
                            dst = vck[:, i - NTH, :] if i >= NTH else vtk[:, i, :]
                            if i % 2 == 0:
                                P.act(dst, a_[:, 0:256], AF.Copy)
                            else:
                                P.cp(dst, a_[:, 0:256])
                    P.barrier()
                    if half == 0:
                        stop("c1")
                    with ExitStack() as st2:
                        mk = [sb(st2, "mk%d" % i, [128, 5, 1024]) for i in range(2)]
                        scb = [sb(st2, "scb%d" % i, [128, 1024]) for i in range(2)]
                        pb_ = [sb(st2, "pbf%d" % i, [128, 1024], BF16) for i in range(2)]
                        ptb = [sb(st2, "ptb%d" % i, [128, 8, 128], BF16) for i in range(2)]
                        sm = sb(st2, "smx", [128, 8])
                        psS = [ps(st2, "psS%d" % i, [128, 1024]) for i in range(2)]
                        psT = [ps(st2, "psT%d" % i, [128, 1024], BF16) for i in range(2)]
                        psO = [ps(st2, "psO%d" % i, [128, 64]) for i in range(2)]
                        smd = [sb(st2, "smd%d" % i, [128, 8]) for i in range(2)]

                        def geom(rp):
                            cls = 0 if rp == 0 else 1 if rp == 1 else 3 if rp == 14 else 4 if rp == 15 else 2
                            brow = 0 if rp <= 1 else 28 if rp >= 14 else 2 * rp
                            return cls, brow * 64

                        def stA1(hl, rp, j):
                            hp, hf = hl // 2, (hl % 2) * 64
                            mkh = mk[hl % 2]
                            cls, k0 = geom(rp)
                            S_ = psS[j]
                            sm_ = smd[j]
                            qsl = slice(rp * 128, (rp + 1) * 128)
                            P.mm(S_[:, 0:256], qT[hf:hf + 64, hp, qsl], kcT[hf:hf + 64, hp, :])
                            P.mm(S_[:, 256:512], qrT[hf:hf + 64, hp, qsl], krT[hf:hf + 64, hp, k0:k0 + 256])
                            P.mm(S_[:, 512:1024], qrT[hf:hf + 64, hp, qsl], krT[hf:hf + 64, hp, k0 + 256:k0 + 768])
                            P.stt(scb[j], S_, 0.125, mkh[:, cls, :], ALU.mult, ALU.add)
                            P.rmax(sm_[:, 0:1], scb[j])
                            P.ts(sm_[:, 1:2], sm_[:, 0:1], -1.0, None, ALU.mult)

                        def stA2(hl, rp, j):
                            sm_ = smd[j]
                            P.act(pb_[j], scb[j], AF.Exp, bias=sm_[:, 1:2], accum=sm_[:, 2:3])

                        def stB1(hl, rp, j):
                            for c in range(8):
                                P.tr(psT[j][:, c * 128:(c + 1) * 128], pb_[j][:, c * 128:(c + 1) * 128], idb)
                            P.act(ptb[j].re("p c n -> p (c n)"), psT[j], AF.Copy)

                        def stB2(hl, rp, j):
                            h = half * 4 + hl
                            cls, k0 = geom(rp)
                            sm_ = smd[j]
                            t0 = k0 // 128
                            for c in range(8):
                                rhs = vck[:, c, hl * 64:(hl + 1) * 64] if c < 2 else vtk[:, t0 + c - 2, hl * 64:(hl + 1) * 64]
                                P.mm(psO[j], ptb[j][:, c, :], rhs, start=(c == 0), stop=(c == 7))
                            P.recip(sm_[:, 3:4], sm_[:, 2:3])
                            P.ts(na_tok[:, rp, h * 64:(h + 1) * 64], psO[j], sm_[:, 3:4], None, ALU.mult)

                        seq = [(hl, rp) for hl in range(4) for rp in range(NTO)]
                        for n_ in range(len(seq) + 1):
                            nxt = seq[n_] if n_ < len(seq) else None
                            cur = seq[n_ - 1] if n_ >= 1 else None
                            if nxt is not None:
                                if nxt[1] == 0:
                                    P.ld(mk[nxt[0] % 2].re("p c n -> p (c n)"), masks[half * 4 + nxt[0]])
                                stA1(nxt[0], nxt[1], n_ % 2)
                            if cur is not None:
                                stB1(cur[0], cur[1], (n_ - 1) % 2)
                            if nxt is not None:
                                stA2(nxt[0], nxt[1], n_ % 2)
                            if cur is not None:
                                stB2(cur[0], cur[1], (n_ - 1) % 2)
                    P.barrier()
            for i_ in range(NTO):
                P.ld(na_d[i_ * 128:(i_ + 1) * 128, :], na_tok[:, i_, :])
        P.barrier()
        stop("c")
        hg = ExitStack()
        open_stacks.append(hg)
        o0 = sb(hg, "o0", [128, NTO, 512], BF16)
        qseg = sb(hg, "qseg", [128, 8, TOK], BF16)
        Send = sb(hg, "Send", [128, 8, 128])
        Sctx = sb(hg, "Sctx", [128, 8, 128])
        Dtot = sb(hg, "Dtot", [128, 8])
        lbt = sb(hg, "lbt", [128, 16])
        lbn = sb(hg, "lbn", [128, 4])
        ones64 = sb(hg, "ones64", [128, 64])
        rmk = sb(hg, "rmk", [128, 4])
        mAt = sb(hg, "mAt", [128, 256])
        mAb = sb(hg, "mAb", [128, 256], BF16)
        P.ld(lbt[:, 0:8], lbl)
        P.ld(rmk, rowmask)
        P.ld(mAt, maskA)
        P.cp(mAb, mAt)
        P.memset(ones64, 1.0)
        P.tt(lbt[:, 8:12], lbt[:, 0:4], lbt[:, 4:8], ALU.subtract)
        P.act(lbt[:, 12:16], lbt[:, 8:12], AF.Sigmoid, scale=-1.0)
        P.act(lbt[:, 8:12], lbt[:, 8:12], AF.Sigmoid)
        P.ts(lbn, lbt[:, 12:16], -1.0, None, ALU.mult)
        with ExitStack() as st:
            ih = sb(st, "ih", [128, NTO, 512], BF16)
            ihc = sb(st, "ihc", [128, 2, 512], BF16)
            wst = [sb(st, "wst%d" % i, [128, 512]) for i in range(2)]
            rsm = sb(st, "rsm", [128, TOK])
            P.ld(rsm, resetm)
            hTo = hT[:, :, OWN0 * 128:OWN0 * 128 + TOK]
            with ExitStack() as st2:
                pa = [ps(st2, "pa%d" % i, [128, 512]) for i in range(2)]
                sgt = [sb(st2, "sgt%d" % i, [128, 512], BF16) for i in range(2)]
                wA = sb(st2, "wA", [128, KC, 512], BF16)
                for (blk, dstt, dstc, fn) in ((6, ih, ihc, AF.Copy), (7, None, None, AF.Sigmoid)):
                    load_w(st2, wst, wA, blk)
                    for i in range(NTO + (2 if dstc is not None else 0)):
                        a_ = pa[i % 2]
                        for kc in range(KC):
                            lhs = hcT[:, kc, (i - NTO) * 128:(i - NTO + 1) * 128] if i >= NTO else hTo[:, kc, i * 128:(i + 1) * 128]
                            P.mm(a_, lhs, wA[:, kc, :], start=(kc == 0), stop=(kc == KC - 1))
                        if dstt is None:
                            P.act(sgt[i % 2], a_, fn)
                            P.ld(sg_d[i * 128:(i + 1) * 128, :], sgt[i % 2])
                        else:
                            P.act(dstc[:, i - NTO, :] if i >= NTO else dstt[:, i, :], a_, fn)
            P.barrier()
            with ExitStack() as st2:
                wzs = [sb(st2, "wz%d" % i_, [128, KC, 128], BF16) for i_ in range(2)]
                sg_ = sb(st2, "s_", [128, TOK])
                binc = sb(st2, "binc", [128, TOK])
                qhh = sb(st2, "qhh", [128, TOK], BF16)
                wqs = [sb(st2, "wq%d" % i_, [128, KC, 128], BF16) for i_ in range(2)]
                kk = sb(st2, "kk", [128, TOK], BF16)
                kdec = sb(st2, "kdec", [128, TOK], BF16)
                kend = sb(st2, "kend", [128, TOK], BF16)
                qdec = sb(st2, "qdec", [128, TOK], BF16)
                sm = sb(st2, "hsm", [128, 4, 64])
                S = sb(st2, "S", [128, 128])
                tot = sb(st2, "tot", [128, 64])
                Sb = [sb(st2, "Sb%d" % i, [128, 4, 128], BF16) for i in range(2)]
                QM = [sb(st2, "QM%d" % i, [128, 640], BF16) for i in range(2)]
                kendT = [sb(st2, "kendT%d" % i, [128, 128], BF16) for i in range(2)]
                vm = [sb(st2, "vm%d" % i, [128, 4, 128], BF16) for i in range(2)]
                Am = [sb(st2, "Am%d" % i, [128, 128], BF16) for i in range(2)]
                pz = [ps(st2, "pz%d" % i, [128, 512]) for i in range(2)]
                pT = ps(st2, "pT", [128, 1024], BF16)
                pKV = [ps(st2, "pKV%d" % i, [128, 4, 128]) for i in range(2)]
                pA = ps(st2, "pA", [128, 128])
                pO = [ps(st2, "pO%d" % i, [128, 128]) for i in range(2)]
                P.memset(QM[0], 0.0)
                P.memset(QM[1], 0.0)
                for isctx in (True, False):
                    ntok = CTX if isctx else TOK
                    nt = ntok // 128
                    nch = ntok // 32
                    hsrc = hcT if isctx else hTo
                    vsrc = ihc if isctx else ih
                    for d in range(2):
                        for h in range(4):
                            k = d * 4 + h
                            wz, wq = wzs[k % 2], wqs[k % 2]
                            load_w(st2, wst, wz, None, 128, (4 + d) * 512 + h * 128)
                            nbs = [(0, 256)] if isctx else [(i * 512, 512) for i in range(4)]
                            for bi, (c0, cn) in enumerate(nbs):
                                a_ = pz[bi % 2]
                                for kc in range(KC):
                                    P.mm(a_[:, 0:cn], wz[:, kc, :], hsrc[:, kc, c0:c0 + cn], start=(kc == 0), stop=(kc == KC - 1))
                                P.act(sg_[:, c0:c0 + cn], a_[:, 0:cn], AF.Sigmoid)
                            sv = sg_[:, 0:ntok]
                            P.ts(kk[:, 0:ntok], sv, lbn[:, h:h + 1], lbt[:, 12 + h:13 + h], ALU.mult, ALU.add)
                            P.act(sv, sv, AF.Ln, bias=lbt[:, 8 + h:9 + h], scale=lbt[:, 12 + h:13 + h])
                            P.scan(binc[:, 0:ntok], rsm[:, 0:ntok], sv, 0.0, ALU.mult, ALU.add)
                            b3 = binc[:, 0:ntok].re("p (c t) -> p c t", t=32)
                            bend = b3[:, :, 31]
                            P.cp(tot[:, 0:nch], bend)
                            bend = tot[:, 0:nch]
                            B = binc[:, 0:ntok]
                            if d == 1:
                                P.tt(B, sv, B, ALU.subtract)
                                P.tt(b3, b3, bend.re("p (c o) -> p c o", o=1).bc([128, nch, 32]), ALU.add)
                            Ee = sg_
                            P.act(sm[:, 2, 0:nch], bend, AF.Exp)
                            P.scan(sm[:, 0, 0:nch], ones64[:, 0:nch], bend, 0.0, ALU.mult, ALU.add)
                            if d == 0:
                                P.tt(sm[:, 1, 0:nch], sm[:, 0, 0:nch], bend, ALU.subtract)
                            else:
                                P.ts(sm[:, 1, 0:nch], sm[:, 0, 0:nch], -1.0, sm[:, 0, nch - 1:nch], ALU.mult, ALU.add)
                            P.act(sm[:, 3, 0:nch], sm[:, 1, 0:nch], AF.Exp)
                            if not isctx:
                                P.act(Dtot[:, k:k + 1], sm[:, 0, nch - 1:nch], AF.Exp)
                                P.act(Ee[:, 0:ntok], B, AF.Exp)
                                load_w(st2, wst, wq, None, 128, 3 * 512 + h * 128)
                                for nb in range(4):
                                    a_ = pz[nb % 2]
                                    for kc in range(KC):
                                        P.mm(a_, wq[:, kc, :], hTo[:, kc, nb * 512:(nb + 1) * 512], start=(kc == 0), stop=(kc == KC - 1))
                                    P.cp(qhh[:, nb * 512:(nb + 1) * 512], a_)
                                P.tt(qdec[:, 0:ntok], qhh, Ee[:, 0:ntok], ALU.mult)
                                P.tt(qseg[:, k, :].re("p (c t) -> p c t", t=32), qdec.re("p (c t) -> p c t", t=32),
                                     sm[:, 3, 0:nch].re("p (c o) -> p c o", o=1).bc([128, nch, 32]), ALU.mult)
                            P.act(Ee[:, 0:ntok], B, AF.Exp, scale=-1.0)
                            P.tt(kdec[:, 0:ntok], kk[:, 0:ntok], Ee[:, 0:ntok], ALU.mult)
                            P.tt(kend[:, 0:ntok].re("p (c t) -> p c t", t=32), kdec[:, 0:ntok].re("p (c t) -> p c t", t=32),
                                 sm[:, 2, 0:nch].re("p (c o) -> p c o", o=1).bc([128, nch, 32]), ALU.mult)
                            P.memset(S, 0.0, eng="dve")
                            tiles = list(range(nt)) if d == 0 else list(range(nt - 1, -1, -1))
                            chs = [0, 1, 2, 3] if d == 0 else [3, 2, 1, 0]
                            for ti, i in enumerate(tiles):
                                j2 = ti % 2
                                tsl = slice(i * 128, (i + 1) * 128)
                                hc = slice(h * 128, (h + 1) * 128)
                                P.tr(pT[:, 0:128], kend[:, tsl], idb)
                                P.act(kendT[j2], pT[:, 0:128], AF.Copy)
                                P.tt(vm[j2], vsrc[:, i, hc].re("p (o v) -> p o v", o=1).bc([128, 4, 128]),
                                     rmk.re("p (j o) -> p j o", o=1).bc([128, 4, 128]), ALU.mult)
                                for j in range(4):
                                    P.mm(pKV[j2][:, j, :], kendT[j2], vm[j2][:, j, :])
                                if not isctx:
                                    P.mm(pA, kdec[:, tsl], qdec[:, tsl])
                                    P.tt(Am[j2], pA, mAb[:, d * 128:(d + 1) * 128], ALU.mult)
                                    P.act(QM[j2].re("p (j x) -> p j x", x=160)[:, :, 0:32],
                                          qdec[:, tsl].re("p (j t) -> p j t", t=32), AF.Copy)
                                for j in chs:
                                    if not isctx:
                                        P.act(Sb[j2][:, j, :], S, AF.Copy)
                                    P.stt(S, S, sm[:, 2, i * 4 + j:i * 4 + j + 1], pKV[j2][:, j, :], ALU.mult, ALU.add)
                                if not isctx:
                                    P.mm(pO[j2], Am[j2], vsrc[:, i, hc], start=True, stop=False)
                                    for j in range(4):
                                        P.mm(pO[j2], QM[j2][:, j * 128:(j + 1) * 128], Sb[j2][:, j, :], start=False, stop=(j == 3))
                                    if d == 0:
                                        P.cp(o0[:, i, hc], pO[j2])
                                    else:
                                        P.tt(o0[:, i, hc], o0[:, i, hc], pO[j2], ALU.add)
                            P.cp((Sctx if isctx else Send)[:, k, :], S)
        P.barrier()
        stop("d")
        s1.close()
        open_stacks.remove(s1)
        Ssb = sb(hg, "Ssb", [128, 8, 128], BF16)
        with ExitStack() as st:
            pD = ps(st, "pD", [8, 128])
            dT = sb(st, "dT", [8, 128])
            mf = sb(st, "mf", [128, 8])
            U = [sb(st, "U%d" % i, [128, 128]) for i in range(2)]
            Dj = [sb(st, "Dj%d" % i, [128, 4]) for i in range(2)]
            P.ld(mf, mfold)
            P.tr(pD, Dtot, idf)
            P.cp(dT, pD)
            for k in range(8):
                P.ld(st_in[k * 128:(k + 1) * 128, :], Send[:, k, :])
            P.ld(st_in[1024:1032, :], dT)
            P.cc("AllGather", ALU.bypass, st_in, st_all)
            it = 0
            for k in range(8):
                d = k // 4
                Sk = Sctx[:, k, :]
                for j in ([0, 1, 2, 3] if d == 0 else [3, 2, 1, 0]):
                    u_, d_ = U[it % 2], Dj[it % 2]
                    it += 1
                    P.ld(u_, st_all[j * 1032 + k * 128:j * 1032 + (k + 1) * 128, :])
                    P.ld(d_[:, 0:1], st_all[j * 1032 + 1024 + k:j * 1032 + 1025 + k, :].re("o d -> d o"))
                    m_ = mf[:, d * 4 + j:d * 4 + j + 1]
                    P.ts(d_[:, 1:2], d_[:, 0:1], -1.0, m_, ALU.add, ALU.mult)
                    P.ts(d_[:, 1:2], d_[:, 1:2], 1.0, None, ALU.add)
                    P.ts(u_, u_, m_, None, ALU.mult)
                    P.stt(Sk, Sk, d_[:, 1:2], u_, ALU.mult, ALU.add)
                P.cp(Ssb[:, k, :], Sk)
        P.barrier()
        stop("e")
        with ExitStack() as st:
            woutb = sb(st, "woutb", [128, KC, D], BF16)
            wst2 = [sb(st, "wst2%d" % i, [128, D]) for i in range(2)]
            wrs = sb(st, "wrs", [128, KC, 16])
            hgn = sb(st, "hgn", [128, 512])
            affT = sb(st, "affT", [16, TOK])
            ot = sb(st, "ot", [128, 512])
            hgt = sb(st, "hgt", [128, 512], BF16)
            nat = [sb(st, "nat%d" % i, [128, 512], BF16) for i in range(2)]
            sgl = [sb(st, "sgl%d" % i, [128, 512], BF16) for i in range(2)]
            xt = [sb(st, "xt%d" % i, [128, D]) for i in range(2)]
            x1t = [sb(st, "x1t%d" % i, [128, D]) for i in range(2)]
            h2t = [sb(st, "h2t%d" % i, [128, D]) for i in range(2)]
            h2b = [sb(st, "h2b%d" % i, [128, D], BF16) for i in range(2)]
            mixT = sb(st, "mixT", [128, KC, 128], BF16)
            h2T = sb(st, "h2T", [128, KC, 128])
            junk = sb(st, "junk2", [128, D])
            sm = sb(st, "rsm2", [128, 16])
            lg = sb(st, "lg", [128, 16])
            psC = ps(st, "psC", [128, 512])
            psT = ps(st, "psT", [128, 1024], BF16)
            psM = ps(st, "psM", [128, 1024])
            psT32 = ps(st, "psT32", [128, 1024])
            psL = ps(st, "psL", [128, 128])
            woutv = wout.re("(kc p) n -> p kc n", p=128)
            for kc in range(KC):
                P.ld(wst2[kc % 2], woutv[:, kc, :])
                P.cp(woutb[:, kc, :], wst2[kc % 2], eng="pool")
            P.ld(wrs, wr.re("(kc p) n -> p kc n", p=128))
            P.ld(hgn, hgnb)
            xhv = xh.re("(n p) d -> n p d", p=128)
            from functools import partial as F_
            from itertools import zip_longest
            smA = [sb(st, "smA%d" % i, [128, 8]) for i in range(2)]
            smB = [sb(st, "smB%d" % i, [128, 8]) for i in range(2)]
            smC = [sb(st, "smC%d" % i, [128, 8]) for i in range(2)]
            junkA = sb(st, "junkA", [128, 128])

            def S1(i):
                ops = []
                A = ops.append
                j2 = i % 2
                tsl = slice(i * 128, (i + 1) * 128)
                sm_ = smA[j2]
                A(F_(P.ld, nat[j2], na_d[tsl, :]))
                A(F_(P.ld, sgl[j2], sg_d[tsl, :]))
                A(F_(P.ld, xt[j2], xhv[i + OWN0]))
                for h in range(4):
                    A(F_(P.mm, psC[:, h * 128:(h + 1) * 128], qseg[:, h, tsl], Ssb[:, h, :], start=True, stop=False))
                    A(F_(P.mm, psC[:, h * 128:(h + 1) * 128], qseg[:, 4 + h, tsl], Ssb[:, 4 + h, :], start=False, stop=True))
                A(F_(P.tt, ot, o0[:, i, :], psC, ALU.add))
                for h in range(4):
                    A(F_(P.act, junkA, ot[:, h * 128:(h + 1) * 128], AF.Square, accum=sm_[:, h:h + 1]))
                A(F_(P.act, sm_[:, 4:8], sm_[:, 0:4], AF.Sqrt, bias=epsc[:, 0:1], scale=1.0 / 128))
                A(F_(P.recip, sm_[:, 4:8], sm_[:, 4:8]))
                o3 = ot.re("p (h v) -> p h v", v=128)
                A(F_(P.tt, o3, o3, sm_[:, 4:8].re("p (h o) -> p h o", o=1).bc([128, 4, 128]), ALU.mult))
                A(F_(P.tt, ot, ot, hgn, ALU.mult))
                A(F_(P.tt, hgt, ot, sgl[j2], ALU.mult))
                for c in range(4):
                    A(F_(P.tr, psT[:, c * 128:(c + 1) * 128], nat[j2][:, c * 128:(c + 1) * 128], idb))
                    A(F_(P.tr, psT[:, (4 + c) * 128:(5 + c) * 128], hgt[:, c * 128:(c + 1) * 128], idb))
                A(F_(P.act, mixT.re("p c n -> p (c n)"), psT, AF.Copy))
                for nb in range(2):
                    for mc in range(KC):
                        A(F_(P.mm, psM[:, nb * 512:(nb + 1) * 512], mixT[:, mc, :], woutb[:, mc, nb * 512:(nb + 1) * 512], start=(mc == 0), stop=(mc == KC - 1)))
                return ops

            def S2(i):
                ops = []
                A = ops.append
                j2 = i % 2
                tsl = slice(i * 128, (i + 1) * 128)
                sm_ = smB[j2]
                A(F_(P.act, junk, psM, AF.Square, accum=sm_[:, 0:1]))
                A(F_(P.act, sm_[:, 1:2], sm_[:, 0:1], AF.Sqrt, bias=epsc[:, 0:1], scale=1.0 / D))
                A(F_(P.recip, sm_[:, 1:2], sm_[:, 1:2]))
                A(F_(P.stt, x1t[j2], psM, sm_[:, 1:2], gt1g, ALU.mult, ALU.mult))
                A(F_(P.tt, x1t[j2], x1t[j2], xt[j2], ALU.add))
                A(F_(P.ld, x1_d[tsl, :], x1t[j2]))
                if debug:
                    A(F_(P.ld, dbg_x1[tsl, :], x1t[j2]))
                A(F_(P.act, junk, x1t[j2], AF.Square, accum=sm_[:, 2:3]))
                A(F_(P.act, sm_[:, 3:4], sm_[:, 2:3], AF.Sqrt, bias=epsc[:, 0:1], scale=1.0 / D))
                A(F_(P.recip, sm_[:, 3:4], sm_[:, 3:4]))
                A(F_(P.stt, h2t[j2], x1t[j2], sm_[:, 3:4], a2, ALU.mult, ALU.mult))
                A(F_(P.tt, h2t[j2], h2t[j2], sh2, ALU.add))
                A(F_(P.act, h2b[j2], h2t[j2], AF.Copy))
                A(F_(P.ld, h2_in[tsl, :], h2b[j2]))
                sm_ = smC[j2]
                for kc in range(KC):
                    A(F_(P.tr, psT32[:, kc * 128:(kc + 1) * 128], h2t[j2][:, kc * 128:(kc + 1) * 128], idf))
                A(F_(P.cp, h2T.re("p c n -> p (c n)"), psT32))
                for kc in range(KC):
                    A(F_(P.mm, psL[:, 0:16], h2T[:, kc, :], wrs[:, kc, :], start=(kc == 0), stop=(kc == KC - 1)))
                A(F_(P.rmax, sm_[:, 0:1], psL[:, 0:16]))
                A(F_(P.ts, sm_[:, 1:2], sm_[:, 0:1], -1.0, None, ALU.mult))
                A(F_(P.act, lg, psL[:, 0:16], AF.Exp, bias=sm_[:, 1:2], accum=sm_[:, 2:3]))
                A(F_(P.recip, sm_[:, 3:4], sm_[:, 2:3]))
                A(F_(P.ts, lg, lg, sm_[:, 3:4], None, ALU.mult))
                A(F_(P.tr, psL[0:16, :], lg, idf))
                A(F_(P.cp, affT[:, tsl], psL[0:16, :]))
                return ops

            for i in range(NTO + 1):
                oa = S1(i) if i < NTO else []
                ob = S2(i - 1) if i >= 1 else []
                for x_, y_ in zip_longest(oa, ob):
                    if x_ is not None:
                        x_()
                    if y_ is not None:
                        y_()
            P.ld(af_in, affT)
            if debug:
                P.ld(dbg_aff, affT)
        hg.close()
        open_stacks.remove(hg)
        stop("f")
        P.barrier()
        P.cc("AllGather", ALU.bypass, af_in, af_all)
        for ch in range(4):
            P.cc("AllGather", ALU.bypass, h2_in[ch * 512:(ch + 1) * 512, :], h2_all[ch * 2048:(ch + 1) * 2048, :])
        stop("1")
        rt = ExitStack()
        idx_i = sb(rt, "idx_i", [128, 4, 8], I32)
        idx_t = sb(rt, "idx_t", [128, 4, 8], I32)
        gate = sb(rt, "gate", [128, 4, 8])
        with ExitStack() as st:
            A = sb(st, "A", [64, TOK])
            selb = sb(st, "selb", [64, TOK])
            incl = sb(st, "incl", [64, TOK])
            ones = sb(st, "ones", [64, TOK])
            Gs = sb(st, "Gs", [64, 64])
            Gp = sb(st, "Gp", [64, 64])
            sM = sb(st, "sM", [64, 16])
            bs = sb(st, "bs", [64, 8])
            rkM = sb(st, "rkM", [128, 16, 16])
            afM = sb(st, "afM", [128, 16, 16])
            res = sb(st, "res", [128, 256])
            gb = sb(st, "gb", [128, 256], BF16)
            vals0 = sb(st, "vals0", [128, 768])
            VALS = sb(st, "VALS", [128, 256, 8], BF16)
            iot = sb(st, "iot", [128, 1024])
            oh = [sb(st, "oh%d" % i, [128, 1024], BF16) for i in range(2)]
            idxf = sb(st, "idxf", [128, 8, 8])
            tokf = sb(st, "tokf", [128, 8])
            P.ld(A, af_all)
            P.ld(Gs, Gm)
            P.ld(Gp, Gpre)
            P.ld(sM, selM)
            P.ld(iot, iota1k)
            P.ld(vals0, tokc)
            P.memset(ones, 1.0)
            P.memset(bs, 0.0)
            P.memset(bs[:, 1:2], 1.0)
            with ExitStack() as st2:
                psb = ps(st2, "psb", [64, 8])
                psr = [ps(st2, "psr%d" % i, [128, 16]) for i in range(2)]
                lo, hi, mid, cpart, cond, tmp = (bs[:, i:i + 1] for i in range(6))
                for it in range(32):
                    P.tt(mid, lo, hi, ALU.add)
                    P.ts(mid, mid, 0.5, None, ALU.mult)
                    P.ts(selb, A, mid, 0.0, ALU.is_gt, ALU.add, accum=cpart)
                    P.mm(psb[:, 0:1], Gs, cpart)
                    P.ts(cond, psb[:, 0:1], 1024.0, None, ALU.is_ge)
                    P.tt(tmp, mid, lo, ALU.subtract)
                    P.stt(lo, tmp, cond, lo, ALU.mult, ALU.add)
                    P.tt(tmp, hi, mid, ALU.subtract)
                    P.stt(hi, tmp, cond, mid, ALU.mult, ALU.add)
                P.ts(selb, A, lo, None, ALU.is_gt)
                P.scan(incl, ones, selb, 0.0, ALU.mult, ALU.add)
                P.mm(psb[:, 1:2], Gp, incl[:, TOK - 1:TOK])
                P.cp(tmp, psb[:, 1:2])
                P.stt(incl, incl, tmp, selb, ALU.add, ALU.mult)
                P.ts(incl, incl, -1.0, None, ALU.add)
                for j in range(16):
                    P.mm(psr[0], incl[:, j * 128:(j + 1) * 128], sM)
                    P.cp(rkM[:, j, :], psr[0])
                    P.mm(psr[1], A[:, j * 128:(j + 1) * 128], sM)
                    P.cp(afM[:, j, :], psr[1])
            af2 = afM.re("p j c -> p (j c)")
            V3 = VALS
            P.cp(V3[:, :, 0], vals0[:, 0:256])
            P.cp(V3[:, :, 1], vals0[:, 256:512])
            P.cp(gb, af2)
            P.cp(V3[:, :, 2], gb)
            P.tt(res, af2, gb, ALU.subtract)
            P.cp(gb, res)
            P.cp(V3[:, :, 3], gb)
            P.tt(res, res, gb, ALU.subtract)
            P.cp(V3[:, :, 4], res)
            P.cp(V3[:, :, 5], vals0[:, 512:768])
            P.memset(V3[:, :, 6:8], 0.0)
            with ExitStack() as st2:
                pI = [ps(st2, "pI%d" % g, [128, 512]) for g in range(8)]
                n = 0
                for i in range(4):
                    cnt = 0
                    for r in range(4):
                        for j in range(16):
                            col = r * 4 + i
                            o_ = oh[n % 2]
                            P.ts(o_, iot, rkM[:, j, col:col + 1], None, ALU.is_equal)
                            n += 1
                            for g in range(8):
                                P.mm(pI[g][:, 0:8], o_[:, g * 128:(g + 1) * 128], V3[:, j * 16 + col, :], start=(cnt == 0), stop=(cnt == 63))
                            cnt += 1
                    for g in range(8):
                        P.cp(idxf[:, g, :], pI[g][:, 0:8])
                    P.stt(tokf, idxf[:, :, 0], 128.0, idxf[:, :, 1], ALU.mult, ALU.add)
                    P.cp(idx_i[:, i, :], tokf)
                    P.stt(tokf, idxf[:, :, 5], 128.0, idxf[:, :, 1], ALU.mult, ALU.add)
                    P.cp(idx_t[:, i, :], tokf)
                    P.tt(gate[:, i, :], idxf[:, :, 2], idxf[:, :, 3], ALU.add)
                    P.tt(gate[:, i, :], gate[:, i, :], idxf[:, :, 4], ALU.add)
        if debug:
            dbt = sb(rt, "dbt", [128, 64])
            P.cp(dbt[:, 0:32], idx_t.re("p a b -> p (a b)"))
            P.cp(dbt[:, 32:64], gate.re("p a b -> p (a b)"))
            P.ld(dbg_idx, dbt)
        P.barrier()
        stop("r")
        with ExitStack() as st:
            xsT = sb(st, "xsT", [128, KC, 1024], BF16)
            hidT = sb(st, "hidT", [128, NFC, 1024], BF16)
            wdb = sb(st, "wdb", [128, NFC, D], BF16)
            wgb = [sb(st, "wgb%d" % i, [128, KC, 256], BF16) for i in range(2)]
            wub = [sb(st, "wub%d" % i, [128, KC, 256], BF16) for i in range(2)]
            xg = [sb(st, "xg%d" % i, [128, D], BF16) for i in range(2)]
            yt = [sb(st, "yt%d" % i, [128, D]) for i in range(2)]
            sil = [sb(st, "sil%d" % i, [128, 512]) for i in range(2)]
            pT = ps(st, "pTx", [128, 1024], BF16)
            pg = [ps(st, "pg%d" % i, [128, 512]) for i in range(2)]
            pu = [ps(st, "pu%d" % i, [128, 512]) for i in range(2)]
            py = [ps(st, "py%d" % i, [128, 512]) for i in range(2)]
            nq = 0
            for i in range(4):
                for g in range(8):
                    P.gather(xg[g % 2], h2_all, idx_i[:, i, g:g + 1])
                    for kc in range(KC):
                        P.tr(pT[:, kc * 128:(kc + 1) * 128], xg[g % 2][:, kc * 128:(kc + 1) * 128], idb)
                    P.act(xsT[:, :, g * 128:(g + 1) * 128], pT.re("p (c n) -> p c n", n=128), AF.Copy)
                wgv = wg4[i].re("(kc p) f -> p kc f", p=128)
                wuv = wu4[i].re("(kc p) f -> p kc f", p=128)
                for fb in range(11):
                    f0 = fb * 256
                    fn = min(256, DE - f0)
                    gb_, ub_ = wgb[fb % 2], wub[fb % 2]
                    P.ld(gb_[:, :, 0:fn], wgv[:, :, f0:f0 + fn], q="pool")
                    P.ld(ub_[:, :, 0:fn], wuv[:, :, f0:f0 + fn], q="pool")
                    for fc_ in (2 * fb, 2 * fb + 1):
                        m_ = 128 if fc_ < 21 else 64
                        P.ld(wdb[0:m_, fc_, :], wd4[i][fc_ * 128:fc_ * 128 + m_, :], q="pool")
                    for c in range((fn + 127) // 128):
                        fc = fb * 2 + c
                        m = min(128, fn - c * 128)
                        for half in range(2):
                            a_, b_ = pg[nq % 2], pu[nq % 2]
                            s_ = sil[nq % 2]
                            nq += 1
                            for kc in range(KC):
                                P.mm(a_[0:m, :], gb_[:, kc, c * 128:c * 128 + m], xsT[:, kc, half * 512:(half + 1) * 512], start=(kc == 0), stop=(kc == KC - 1))
                            for kc in range(KC):
                                P.mm(b_[0:m, :], ub_[:, kc, c * 128:c * 128 + m], xsT[:, kc, half * 512:(half + 1) * 512], start=(kc == 0), stop=(kc == KC - 1))
                            P.act(s_[0:m, :], a_[0:m, :], AF.Silu)
                            P.tt(hidT[0:m, fc, half * 512:(half + 1) * 512], s_[0:m, :], b_[0:m, :], ALU.mult)
                for ct in range(8):
                    y_ = yt[ct % 2]
                    for nb in range(2):
                        p_ = py[nb]
                        for fc in range(NFC):
                            m = 128 if fc < 21 else 64
                            P.mm(p_, hidT[0:m, fc, ct * 128:(ct + 1) * 128], wdb[0:m, fc, nb * 512:(nb + 1) * 512], start=(fc == 0), stop=(fc == NFC - 1))
                        P.ts(y_[:, nb * 512:(nb + 1) * 512], p_, gate[:, i, ct:ct + 1], None, ALU.mult)
                    P.scatter_add(acc, y_, idx_t[:, i, ct:ct + 1])
        rt.close()
        P.cc("ReduceScatter", ALU.add, acc, rs_out)
        with ExitStack() as st:
            mt = [sb(st, "mt%d" % i, [128, D]) for i in range(2)]
            x1l = [sb(st, "x1l%d" % i, [128, D]) for i in range(2)]
            junk = sb(st, "junk3", [128, D])
            sm = sb(st, "fsm", [128, 4])
            for i in range(NTO):
                j2 = i % 2
                tsl = slice(i * 128, (i + 1) * 128)
                P.ld(mt[j2], rs_out[tsl, :])
                P.ld(x1l[j2], x1_d[tsl, :])
                P.act(junk, mt[j2], AF.Square, accum=sm[:, 0:1])
                P.act(sm[:, 1:2], sm[:, 0:1], AF.Sqrt, bias=epsc[:, 0:1], scale=1.0 / D)
                P.recip(sm[:, 1:2], sm[:, 1:2])
                P.stt(mt[j2], mt[j2], sm[:, 1:2], gt2g, ALU.mult, ALU.mult)
                P.tt(mt[j2], mt[j2], x1l[j2], ALU.add)
                P.ld(out[tsl, :], mt[j2])
    except _Stop:
        pass
    for stx in reversed(open_stacks):
        stx.close()
    P.barrier()
    top.close()
    return nc


def _consts():
    f = np.float32
    p = np.arange(128)
    cst = {}
    cst["identf"] = np.eye(128, dtype=f)
    sidx, cidx = p[:, None], p[None, :]
    same = (sidx // 32) == (cidx // 32)
    cst["maskA"] = np.concatenate([(same & (cidx >= sidx)), (same & (cidx <= sidx))], axis=1).astype(f)
    cst["rowmask"] = ((p[:, None] // 32) == np.arange(4)[None, :]).astype(f)
    rm = np.ones((128, TOK), f)
    rm[:, 0::32] = 0.0
    cst["resetm"] = rm
    cst["iota1k"] = np.broadcast_to(np.arange(1024, dtype=f)[None, :], (128, 1024)).copy()
    tok = np.zeros((128, 768), f)
    for j in range(16):
        for col in range(16):
            r = col // 4
            tok[:, j * 16 + col] = (j // 4) * 16 + r * 4 + (j % 4)
            tok[:, 512 + j * 16 + col] = r * 16 + j
    tok[:, 256:512] = p[:, None].astype(f)
    cst["tokc"] = tok
    re_ = np.arange(64)
    r_, e_ = re_ // 16, re_ % 16
    cst["Gm"] = (e_[:, None] == e_[None, :]).astype(f)
    cst["Gpre"] = ((e_[:, None] == e_[None, :]) & (r_[:, None] < r_[None, :])).astype(f)
    return cst


def prep(x, c, ctx, c_ctx, w_mod, b_mod, g_pre1, g_post1, g_pre2, g_post2, w_in, w_out,
         na_rpb, hg_lb_logits, hg_norm, w_router, w_gate, w_up, w_down):
    f = np.float32
    A_ = lambda a: np.ascontiguousarray(np.asarray(a, dtype=f))
    x, c, ctx, c_ctx = A_(x), A_(c), A_(ctx), A_(c_ctx)
    w_mod, b_mod, w_in, w_out = A_(w_mod)[0], A_(b_mod)[0], A_(w_in)[0], A_(w_out)[0]
    rpb = A_(na_rpb)[0]
    lbl_ = A_(hg_lb_logits)
    hgn_ = A_(hg_norm)[0]
    wr_ = A_(w_router)[0]
    wg_, wu_, wd_ = np.asarray(w_gate)[0], np.asarray(w_up)[0], np.asarray(w_down)[0]
    bc = lambda v: np.ascontiguousarray(np.broadcast_to(v[None, :], (128, v.shape[0])))
    pp = np.arange(512)
    perm = np.where(pp % 32 < 16, pp + 16, pp - 16)
    winx = np.ascontiguousarray(np.concatenate([w_in, w_in[:, 0:512][:, perm], w_in[:, 512:1024][:, perm]], axis=1))
    cst = _consts()
    shared = dict(cst)
    shared.update(wmod=w_mod, bmodb=bc(b_mod), g1b=bc(A_(g_pre1)[0]), gp1b=bc(A_(g_post1)[0]),
                  g2b=bc(A_(g_pre2)[0]), gp2b=bc(A_(g_post2)[0]), winx=winx, wout=w_out,
                  hgnb=bc(np.tile(hgn_, 4)), wr=wr_,
                  lbl=np.ascontiguousarray(lbl_.reshape(2, 4, 128).transpose(2, 0, 1).reshape(128, 8)))
    ccrep = np.ascontiguousarray(np.broadcast_to(c_ctx.reshape(KC, 128).T[:, :, None], (128, KC, 128)).reshape(128, KC * 128))
    d64 = np.arange(128) % 64
    seg = d64 // 32
    first = (d64 % 32) < 16
    inv = (10000.0 ** (-(2.0 * (d64 % 16)) / 32.0)).astype(f)
    in_maps = []
    for core in range(8):
        b, s = core // 4, core % 4
        m = dict(shared)
        xh = np.zeros((HTOK, D), f)
        g0 = TOK * s - 256
        lo, hi = max(g0, 0), min(g0 + HTOK, 8192)
        xh[lo - g0:hi - g0] = x[b, lo:hi]
        m["xh"] = xh
        m["ctxb"] = ctx[b]
        m["crep"] = np.ascontiguousarray(np.broadcast_to(c[b].reshape(KC, 128).T[:, :, None], (128, KC, 128)).reshape(128, KC * 128))
        m["ccrep"] = ccrep
        tl = np.arange(HTOK)
        row = (32 * s - 4 + tl // 64).astype(f)
        colp = (tl % 64).astype(f)
        pos = np.where(seg[:, None] == 0, row[None, :], colp[None, :]).astype(f)
        ang = (pos * inv[:, None]).astype(f)
        m["cosT"] = np.cos(ang).astype(f)
        m["sinT"] = np.where(first[:, None], -np.sin(ang), np.sin(ang)).astype(f)
        mk = np.zeros((8, 128, 5, 1024), f)
        qp = np.arange(128)
        a_, qc = qp // 64, qp % 64
        nn = np.arange(768)
        for cls, rp in enumerate((0, 1, 4, 14, 15)):
            brow = 0 if rp <= 1 else 28 if rp >= 14 else 2 * rp
            r = 32 * s + 2 * rp + a_
            grow = 32 * s - 4 + brow + nn // 64
            kc_ = nn % 64
            r0 = np.clip(r - 4, 0, 120)
            vrow = (grow[None, :] >= r0[:, None]) & (grow[None, :] < r0[:, None] + 8)
            wc0 = np.clip(qc - 8, 0, 48)
            vcol = (kc_[None, :] >= wc0[:, None]) & (kc_[None, :] < wc0[:, None] + 16)
            dr = np.clip(grow[None, :] - r[:, None] + 7, 0, 14)
            dc = np.clip(kc_[None, :] - qc[:, None] + 15, 0, 30)
            bias = rpb[:, dr, dc]
            mk[:, :, cls, 256:1024] = np.where((vrow & vcol)[None], bias, f(NEG))
        m["masks"] = mk.reshape(8, 128, 5 * 1024)
        mf = np.zeros((128, 8), f)
        for j in range(4):
            mf[:, j] = 1.0 if j < s else 0.0
            mf[:, 4 + j] = 1.0 if j > s else 0.0
        m["mfold"] = mf
        sel = np.zeros((64, 16), f)
        for r in range(4):
            for i in range(4):
                sel[r * 16 + 4 * s + i, r * 4 + i] = 1.0
        m["selM"] = sel
        m["selO"] = np.zeros((64, 16), f)
        m["wg4"] = np.ascontiguousarray(wg_[4 * s:4 * s + 4], dtype=f)
        m["wu4"] = np.ascontiguousarray(wu_[4 * s:4 * s + 4], dtype=f)
        m["wd4"] = np.ascontiguousarray(wd_[4 * s:4 * s + 4], dtype=f)
        in_maps.append(m)
    return in_maps


def kernel(**inputs):
    f = np.float32
    in_maps = prep(**inputs)
    import os
    dbg = os.environ.get("KDEBUG", "") == "1"
    nc = build(debug=dbg)
    res = run_bass_kernel_spmd(nc, in_maps, core_ids=list(range(8)))
    if dbg:
        global LAST
        LAST = res.results
    out = np.zeros((2, 8192, D), f)
    for core in range(8):
        b, s = core // 4, core % 4
        out[b, TOK * s:TOK * (s + 1)] = res.results[core]["out"]
    return out
```

```python
import numpy as np
from contextlib import ExitStack
import concourse.bass as bass
import concourse.mybir as mybir
from concourse.bass_utils import run_bass_kernel_spmd

F32 = mybir.dt.float32
BF16 = mybir.dt.bfloat16
I32 = mybir.dt.int32
ALU = mybir.AluOpType
AF = mybir.ActivationFunctionType
AX = mybir.AxisListType

D = 1024
KC = 8
NTO = 16
NTH = 20
OWN0 = 2
TOK = 2048
HTOK = 2560
CTX = 256
DE = 2752
NFC = 22
EPS = 1e-6
GROUPS = [[0, 1, 2, 3], [4, 5, 6, 7]]
NDS = 24
NEG = -30000.0


class _Stop(Exception):
    pass


class V:
    def __init__(s, ap, key):
        s.ap = ap
        s.key = key

    def __getitem__(s, idx):
        return V(s.ap[idx], s.key)

    def sub(s, k):
        return V(s.ap, s.key + "/" + str(k))

    def re(s, pat, **kw):
        return V(s.ap.rearrange(pat, **kw), s.key)

    def bc(s, shape):
        return V(s.ap.to_broadcast(shape), s.key)


def _ap(x):
    return x.ap if isinstance(x, V) else x


def _keys(*xs):
    return [x.key for x in xs if isinstance(x, V)]


def _ovl(a, b):
    return a == b or a.startswith(b + "/") or b.startswith(a + "/")


class Prog:
    def __init__(s, nc, st):
        s.nc = nc
        s.E = {"pe": nc.tensor, "act": nc.scalar, "dve": nc.vector, "pool": nc.gpsimd, "sp": nc.sync}
        s.sems = []
        s.esem = {}
        s.ecnt = {}
        for e in ["pe", "act", "dve", "pool"]:
            s.esem[e] = s._new(st, "s_" + e)
            s.ecnt[e] = 0
        s.dq = {}
        for q in ["sp", "pool"]:
            s.dq[q] = {"sems": [s._new(st, "d_%s%d" % (q, i)) for i in range(NDS)], "use": [0] * NDS, "nxt": 0}
        s.ccsem = s._new(st, "ccs")
        s.cccnt = 0
        s.waited = {e: {} for e in s.E}
        s.bufs = {}
        s.alltok = {}
        s.halt = False

    def _new(s, st, name):
        s.sems.append(st.enter_context(s.nc.semaphore(name)))
        return len(s.sems) - 1

    def _collect(s, reads, writes):
        t = {}

        def add(d):
            for k, v in d.items():
                if t.get(k, 0) < v:
                    t[k] = v
        for k in reads:
            for k2, stt in s.bufs.get(k.split("/")[0], {}).items():
                if _ovl(k, k2):
                    add(stt["w"])
        for k in writes:
            for k2, stt in s.bufs.get(k.split("/")[0], {}).items():
                if _ovl(k, k2):
                    add(stt["w"])
                    add(stt["r"])
        return t

    def _update(s, reads, writes, tok):
        for k in reads:
            stt = s.bufs.setdefault(k.split("/")[0], {}).setdefault(k, {"w": {}, "r": {}})
            if stt["r"].get(tok[0], 0) < tok[1]:
                stt["r"][tok[0]] = tok[1]
        for k in writes:
            d = s.bufs.setdefault(k.split("/")[0], {})
            for k2 in list(d):
                if k2 != k and k2.startswith(k + "/"):
                    del d[k2]
            d[k] = {"w": {tok[0]: tok[1]}, "r": {}}
        if s.alltok.get(tok[0], 0) < tok[1]:
            s.alltok[tok[0]] = tok[1]

    def _wait(s, eng, toks, skip=None):
        e = s.E[eng]
        for sm, v in toks.items():
            if sm == skip:
                continue
            if s.waited[eng].get(sm, 0) < v:
                e.wait_ge(s.sems[sm], v)
                s.waited[eng][sm] = v

    def op(s, eng, fn, reads=(), writes=()):
        if s.halt:
            return
        if eng != "pe":
            pr = [k for k in reads if k.startswith("PS")]
            if pr:
                writes = list(writes) + pr
        toks = s._collect(reads, writes)
        s._wait(eng, toks, skip=s.esem[eng] if eng == "pe" else None)
        ins = fn()
        s.ecnt[eng] += 1
        ins.then_inc(s.sems[s.esem[eng]], 1)
        s._update(reads, writes, (s.esem[eng], s.ecnt[eng]))

    def dma(s, q, fn, reads=(), writes=()):
        if s.halt:
            return
        dq = s.dq[q]
        i = dq["nxt"]
        dq["nxt"] = (i + 1) % NDS
        sm = dq["sems"][i]
        toks = s._collect(reads, writes)
        if dq["use"][i] > 0 and toks.get(sm, 0) < 16 * dq["use"][i]:
            toks[sm] = 16 * dq["use"][i]
        s._wait(q, toks)
        ins = fn()
        dq["use"][i] += 1
        ins.then_inc(s.sems[sm], 16)
        s._update(reads, writes, (sm, 16 * dq["use"][i]))

    def cc(s, kind, op, src, dst):
        if s.halt:
            return
        import os
        if os.environ.get("KNOCC", "") == "1" or (os.environ.get("KNOCC", "") == kind):
            n = min(src.ap.shape[0], dst.ap.shape[0])
            s.ld(dst[0:n, :], src[0:n, :])
            return
        toks = s._collect([src.key], [dst.key])
        s._wait("pool", toks)
        ins = s.nc.gpsimd.collective_compute(kind, op, replica_groups=GROUPS, ins=[src.ap.opt()], outs=[dst.ap.opt()])
        s.cccnt += 1
        ins.then_inc(s.sems[s.ccsem])
        s._update([src.key], [dst.key], (s.ccsem, s.cccnt))

    def barrier(s):
        if s.halt:
            return
        for eng in s.E:
            s._wait(eng, dict(s.alltok), skip=None)

    def mm(s, out, lhsT, rhs, start=True, stop=True):
        s.op("pe", lambda: s.nc.tensor.matmul(_ap(out), _ap(lhsT), _ap(rhs), start=start, stop=stop),
             _keys(lhsT, rhs), _keys(out))

    def tr(s, out, in_, ident):
        s.op("pe", lambda: s.nc.tensor.transpose(_ap(out), _ap(in_), _ap(ident)), _keys(in_, ident), _keys(out))

    def act(s, out, in_, func, bias=None, scale=1.0, accum=None):
        kw = {}
        if bias is not None:
            kw["bias"] = _ap(bias)
        if accum is not None:
            kw["accum_out"] = _ap(accum)
        s.op("act", lambda: s.nc.scalar.activation(out=_ap(out), in_=_ap(in_), func=func, scale=_ap(scale), **kw),
             _keys(in_, bias, scale), _keys(out, accum))

    def tt(s, out, a, b, op, eng="dve"):
        e = s.E[eng]
        s.op(eng, lambda: e.tensor_tensor(out=_ap(out), in0=_ap(a), in1=_ap(b), op=op), _keys(a, b), _keys(out))

    def ts(s, out, a, s1, s2, op0, op1=None, accum=None, eng="dve"):
        e = s.E[eng]
        kw = {}
        if accum is not None:
            kw["accum_out"] = _ap(accum)
        if op1 is None:
            s.op(eng, lambda: e.tensor_scalar(out=_ap(out), in0=_ap(a), scalar1=_ap(s1), scalar2=None, op0=op0, **kw),
                 _keys(a, s1), _keys(out, accum))
        else:
            s.op(eng, lambda: e.tensor_scalar(out=_ap(out), in0=_ap(a), scalar1=_ap(s1), scalar2=_ap(s2), op0=op0, op1=op1, **kw),
                 _keys(a, s1, s2), _keys(out, accum))

    def stt(s, out, a, sc, b, op0, op1, eng="dve"):
        e = s.E[eng]
        s.op(eng, lambda: e.scalar_tensor_tensor(out=_ap(out), in0=_ap(a), scalar=_ap(sc), in1=_ap(b), op0=op0, op1=op1),
             _keys(a, sc, b), _keys(out))

    def cp(s, out, in_, eng="dve"):
        e = s.E[eng]
        s.op(eng, lambda: e.tensor_copy(out=_ap(out), in_=_ap(in_)), _keys(in_), _keys(out))

    def memset(s, out, val, eng="pool"):
        e = s.E[eng]
        s.op(eng, lambda: e.memset(_ap(out), val), [], _keys(out))

    def rmax(s, out, in_):
        s.op("dve", lambda: s.nc.vector.reduce_max(out=_ap(out), in_=_ap(in_), axis=AX.X), _keys(in_), _keys(out))

    def recip(s, out, in_):
        s.op("dve", lambda: s.nc.vector.reciprocal(out=_ap(out), in_=_ap(in_)), _keys(in_), _keys(out))

    def scan(s, out, d0, d1, init, op0, op1):
        s.op("dve", lambda: s.nc.vector.tensor_tensor_scan(out=_ap(out), data0=_ap(d0), data1=_ap(d1), initial=init, op0=op0, op1=op1),
             _keys(d0, d1), _keys(out))

    def ld(s, out, in_, q="sp"):
        e = s.E[q]
        s.dma(q, lambda: e.dma_start(out=_ap(out), in_=_ap(in_)), _keys(in_), _keys(out))

    def gather(s, out, src, idx):
        s.dma("pool", lambda: s.nc.gpsimd.indirect_dma_start(
            out=_ap(out), out_offset=None, in_=_ap(src),
            in_offset=bass.IndirectOffsetOnAxis(ap=_ap(idx), axis=0)), _keys(src, idx), _keys(out))

    def scatter_add(s, dst, src, idx):
        s.dma("pool", lambda: s.nc.gpsimd.indirect_dma_start(
            out=_ap(dst), out_offset=bass.IndirectOffsetOnAxis(ap=_ap(idx), axis=0),
            in_=_ap(src), in_offset=None, compute_op=ALU.add), _keys(src, idx, dst), _keys(dst))


def build(debug=False):
    nc = bass.Bass("TRN2", target_bir_lowering=False)
    top = ExitStack()
    P = Prog(nc, top)

    import os
    KSTOP = os.environ.get("KSTOP", "")
    open_stacks = []

    def stop(tag):
        if KSTOP == tag:
            P.barrier()
            P.halt = True

    def din(name, shape, dt=F32):
        return V(nc.dram_tensor(name, list(shape), dt, kind="ExternalInput").ap(), name)

    def dscr(name, shape, dt=F32):
        return V(nc.dram_tensor(name, list(shape), dt).ap(), name)

    uq = [0]

    def sb(st, name, shape, dt=F32, side=None):
        uq[0] += 1
        name = "%s_%d" % (name, uq[0])
        return V(st.enter_context(nc.sbuf_tensor(name, list(shape), dt, side=side))[:], name)

    def ps(st, name, shape, dt=F32):
        uq[0] += 1
        name = "PS%s_%d" % (name, uq[0])
        return V(st.enter_context(nc.psum_tensor(name, list(shape), dt))[:], name)

    xh = din("xh", [HTOK, D])
    ctxb = din("ctxb", [CTX, D])
    crep = din("crep", [128, KC * 128])
    ccrep = din("ccrep", [128, KC * 128])
    wmod = din("wmod", [D, 6 * D])
    bmodb = din("bmodb", [128, 6 * D])
    g1b = din("g1b", [128, D])
    gp1b = din("gp1b", [128, D])
    g2b = din("g2b", [128, D])
    gp2b = din("gp2b", [128, D])
    winx = din("winx", [D, 5120])
    wout = din("wout", [D, D])
    cosT = din("cosT", [128, HTOK])
    sinT = din("sinT", [128, HTOK])
    masks = din("masks", [8, 128, 5 * 1024])
    lbl = din("lbl", [128, 8])
    hgnb = din("hgnb", [128, 512])
    wr = din("wr", [D, 16])
    mfold = din("mfold", [128, 8])
    wg4 = din("wg4", [4, D, DE])
    wu4 = din("wu4", [4, D, DE])
    wd4 = din("wd4", [4, DE, D])
    selM = din("selM", [64, 16])
    selO = din("selO", [64, 16])
    Gm = din("Gm", [64, 64])
    Gpre = din("Gpre", [64, 64])
    identf = din("identf", [128, 128])
    maskA = din("maskA", [128, 256])
    rowmask = din("rowmask", [128, 4])
    resetm = din("resetm", [128, TOK])
    iota1k = din("iota1k", [128, 1024])
    tokc = din("tokc", [128, 768])
    out = V(nc.dram_tensor("out", [TOK, D], F32, kind="ExternalOutput").ap(), "out")
    if debug:
        dbg_x1 = V(nc.dram_tensor("dbg_x1", [TOK, D], F32, kind="ExternalOutput").ap(), "dbg_x1")
        dbg_mix = V(nc.dram_tensor("dbg_mix", [TOK, D], F32, kind="ExternalOutput").ap(), "dbg_mix")
        dbg_aff = V(nc.dram_tensor("dbg_aff", [16, TOK], F32, kind="ExternalOutput").ap(), "dbg_aff")
        dbg_idx = V(nc.dram_tensor("dbg_idx", [128, 64], F32, kind="ExternalOutput").ap(), "dbg_idx")

    na_d = dscr("na_d", [TOK, 512], BF16)
    sg_d = dscr("sg_d", [TOK, 512], BF16)
    x1_d = dscr("x1_d", [TOK, D])
    h2_in = dscr("h2_in", [TOK, D], BF16)
    h2_all = dscr("h2_all", [4 * TOK, D], BF16)
    st_in = dscr("st_in", [1032, 128])
    st_all = dscr("st_all", [4 * 1032, 128])
    af_in = dscr("af_in", [16, TOK])
    af_all = dscr("af_all", [64, TOK])
    acc = dscr("acc", [4 * TOK, D])
    rs_out = dscr("rs_out", [TOK, D])
    modsc = dscr("modsc", [4 * 128, D])

    try:
        idf = sb(top, "idf", [128, 128])
        idb = sb(top, "idb", [128, 128], BF16)
        mst = ExitStack()
        cols = sb(top, "cols", [128, 32])
        zero4k = sb(top, "zero4k", [128, D])
        epsc = sb(top, "epsc", [128, 1])
        gt1g = sb(mst, "gt1g", [128, D])
        a2 = sb(mst, "a2", [128, D])
        sh2 = sb(mst, "sh2", [128, D])
        gt2g = sb(mst, "gt2g", [128, D])
        modB = sb(mst, "modB", [128, 6 * D])
        P.memset(epsc, EPS)
        P.ld(idf, identf)
        P.cp(idb, idf)
        P.memset(zero4k, 0.0)
        accv = acc.re("(n p) d -> n p d", p=128)
        for n in range(64):
            P.ld(accv[n], zero4k)

        with ExitStack() as st:
            sc_ = sb(st, "siluc", [128, KC * 128])
            scc = sb(st, "silucc", [128, KC * 128])
            modC = sb(st, "modC", [128, 2 * D])
            wmb = [sb(st, "wmb%d" % i, [128, KC, 512]) for i in range(2)]
            bmb = [sb(st, "bmb%d" % i, [128, 512]) for i in range(2)]
            pm = [ps(st, "pm%d" % i, [128, 512]) for i in range(2)]
            tmpb = sb(st, "tmpb", [128, D])
            ptr = ps(st, "ptr", [128, 128])
            P.ld(sc_, crep)
            P.ld(scc, ccrep)
            P.act(sc_, sc_, AF.Silu)
            P.act(scc, scc, AF.Silu)
            wmv = wmod.re("(kc p) n -> p kc n", p=128)
            for nb in range(12):
                wb = wmb[nb % 2]
                P.ld(wb, wmv[:, :, nb * 512:(nb + 1) * 512])
                P.ld(bmb[nb % 2], bmodb[:, nb * 512:(nb + 1) * 512])
                for kc in range(KC):
                    P.mm(pm[0], sc_[:, kc * 128:(kc + 1) * 128], wb[:, kc, :], start=(kc == 0), stop=(kc == KC - 1))
                P.tt(modB[:, nb * 512:(nb + 1) * 512], pm[0], bmb[nb % 2], ALU.add)
                if nb < 4:
                    for kc in range(KC):
                        P.mm(pm[1], scc[:, kc * 128:(kc + 1) * 128], wb[:, kc, :], start=(kc == 0), stop=(kc == KC - 1))
                    P.tt(modC[:, nb * 512:(nb + 1) * 512], pm[1], bmb[nb % 2], ALU.add)
            g1t = sb(st, "g1t", [128, D])
            P.ld(g1t, g1b)
            for (src_sh, src_sc, c0) in ((modB[:, 0:D], modB[:, D:2 * D], 0), (modC[:, 0:D], modC[:, D:2 * D], 16)):
                P.stt(tmpb, src_sc, 1.0, g1t, ALU.add, ALU.mult)
                for kc in range(KC):
                    P.tr(ptr, tmpb[:, kc * 128:(kc + 1) * 128], idf)
                    P.cp(cols[:, c0 + kc:c0 + kc + 1], ptr[:, 0:1])
                    P.tr(ptr, src_sh[:, kc * 128:(kc + 1) * 128], idf)
                    P.cp(cols[:, c0 + 8 + kc:c0 + 8 + kc + 1], ptr[:, 0:1])
            P.ld(tmpb, gp1b)
            P.tt(gt1g, modB[:, 2 * D:3 * D], tmpb, ALU.mult)
            P.ld(tmpb, g2b)
            P.stt(a2, modB[:, 4 * D:5 * D], 1.0, tmpb, ALU.add, ALU.mult)
            P.cp(sh2, modB[:, 3 * D:4 * D])
            P.ld(tmpb, gp2b)
            P.tt(gt2g, modB[:, 5 * D:6 * D], tmpb, ALU.mult)
            for q_, t_ in enumerate((gt1g, a2, sh2, gt2g)):
                P.ld(modsc[q_ * 128:(q_ + 1) * 128, :], t_)
        P.barrier()
        mst.close()
        stop("a")

        s1 = ExitStack()
        open_stacks.append(s1)
        hT = sb(s1, "hT", [128, KC, HTOK], BF16, side="right")
        hcT = sb(s1, "hcT", [128, KC, CTX], BF16, side="right")
        with ExitStack() as st:
            xt = [sb(st, "xt%d" % i, [128, D]) for i in range(2)]
            xs = [sb(st, "xs%d" % i, [128, D], BF16) for i in range(2)]
            junk = sb(st, "junk", [128, D])
            ss = sb(st, "ss", [128, 2])
            pt = [ps(st, "pt%d" % i, [128, D], BF16) for i in range(2)]
            xhv = xh.re("(n p) d -> n p d", p=128)
            cxv = ctxb.re("(n p) d -> n p d", p=128)
            for i in range(NTH + 2):
                isctx = i >= NTH
                src = cxv[i - NTH] if isctx else xhv[i]
                x_ = xt[i % 2]
                P.ld(x_, src)
                P.act(junk, x_, AF.Square, accum=ss[:, 0:1])
                P.act(ss[:, 1:2], ss[:, 0:1], AF.Sqrt, bias=epsc[:, 0:1], scale=1.0 / D)
                P.recip(ss[:, 1:2], ss[:, 1:2])
                P.ts(xs[i % 2], x_, ss[:, 1:2], None, ALU.mult)
                for kc in range(KC):
                    P.tr(pt[i % 2][:, kc * 128:(kc + 1) * 128], xs[i % 2][:, kc * 128:(kc + 1) * 128], idb)
                c0 = 16 if isctx else 0
                for kc in range(KC):
                    dst = hcT[:, kc, (i - NTH) * 128:(i - NTH + 1) * 128] if isctx else hT[:, kc, i * 128:(i + 1) * 128]
                    if kc % 2 == 0:
                        P.act(dst, pt[i % 2][:, kc * 128:(kc + 1) * 128], AF.Identity,
                              bias=cols[:, c0 + 8 + kc:c0 + 9 + kc], scale=cols[:, c0 + kc:c0 + kc + 1])
                    else:
                        P.ts(dst, pt[i % 2][:, kc * 128:(kc + 1) * 128], cols[:, c0 + kc:c0 + kc + 1],
                             cols[:, c0 + 8 + kc:c0 + 9 + kc], ALU.mult, ALU.add)
        P.barrier()
        stop("b")

        winv = winx.re("(kc p) n -> p kc n", p=128)

        def load_w(st_w, wst, wbf, blk, ncols=512, col0=None):
            c0 = blk * 512 if col0 is None else col0
            for kc in range(KC):
                P.ld(wst[kc % 2][:, 0:ncols], winv[:, kc, c0:c0 + ncols])
                P.cp(wbf[:, kc, 0:ncols], wst[kc % 2][:, 0:ncols], eng="pool")

        with ExitStack() as st:
            na_tok = sb(st, "na_tok", [128, NTO, 512], BF16)
            for half in range(2):
                with ExitStack() as sth:
                    qT = sb(sth, "qT", [128, 2, TOK], BF16)
                    qrT = sb(sth, "qrT", [128, 2, TOK], BF16)
                    krT = sb(sth, "krT", [128, 2, HTOK], BF16)
                    vtk = sb(sth, "vtk", [128, NTH, 256], BF16)
                    kcT = sb(sth, "kcT", [128, 2, CTX], BF16)
                    vck = sb(sth, "vck", [128, 2, 256], BF16)
                    with ExitStack() as st2:
                        wst = [sb(st2, "wst%d" % i, [128, 512]) for i in range(2)]
                        wAs = [sb(st2, "wA%d" % i_, [128, KC, 256], BF16) for i_ in range(2)]
                        wBs = [sb(st2, "wB%d" % i_, [128, KC, 256], BF16) for i_ in range(2)]
                        cs = sb(st2, "cs", [128, HTOK])
                        sn = sb(st2, "sn", [128, HTOK])
                        t1 = sb(st2, "t1", [128, 512])
                        t2 = sb(st2, "t2", [128, 512])
                        pa = [ps(st2, "pa%d" % i, [128, 512]) for i in range(2)]
                        pb = [ps(st2, "pb%d" % i, [128, 512]) for i in range(2)]
                        P.ld(cs, cosT)
                        P.ld(sn, sinT)
                        for (blk, pblk, ntok, tok0, dstp, dstr) in ((0, 8, TOK, OWN0 * 128, qT, qrT), (1, 9, HTOK, 0, None, krT)):
                            wA, wB = wAs[blk], wBs[blk]
                            load_w(st2, wst, wA, blk, 256, blk * 512 + half * 256)
                            load_w(st2, wst, wB, pblk, 256, pblk * 512 + half * 256)
                            if half == 0 and blk == 0:
                                stop("c1a")
                            it = 0
                            for hp in range(2):
                                for nb in range(ntok // 512):
                                    a_, b_ = pa[it % 2], pb[it % 2]
                                    it += 1
                                    tk = tok0 + nb * 512
                                    for kc in range(KC):
                                        P.mm(a_, wA[:, kc, hp * 128:(hp + 1) * 128], hT[:, kc, tk:tk + 512], start=(kc == 0), stop=(kc == KC - 1))
                                    for kc in range(KC):
                                        P.mm(b_, wB[:, kc, hp * 128:(hp + 1) * 128], hT[:, kc, tk:tk + 512], start=(kc == 0), stop=(kc == KC - 1))
                                    KV = os.environ.get("KVAR", "")
                                    if dstp is not None and KV not in ("1", "3"):
                                        P.act(dstp[:, hp, nb * 512:(nb + 1) * 512], a_, AF.Copy)
                                    if KV not in ("2", "3"):
                                        P.tt(t1, a_, cs[:, tk:tk + 512], ALU.mult)
                                        P.tt(t2, b_, sn[:, tk:tk + 512], ALU.mult)
                                        P.tt(dstr[:, hp, nb * 512:(nb + 1) * 512], t1, t2, ALU.add)
                            if half == 0 and blk == 0:
                                stop("c1b")
                            if half == 0 and blk == 1:
                                stop("c1c")
                        for hp in range(2):
                            for kc in range(KC):
                                P.mm(pa[0][:, 0:CTX], wA[:, kc, hp * 128:(hp + 1) * 128], hcT[:, kc, :], start=(kc == 0), stop=(kc == KC - 1))
                            P.cp(kcT[:, hp, :], pa[0][:, 0:CTX])
                        wA = wAs[0]
                        load_w(st2, wst, wA, 2, 256, 2 * 512 + half * 256)
                        for i in range(NTH + 2):
                            a_ = pa[i % 2]
                            for kc in range(KC):
                                lhs = hcT[:, kc, (i - NTH) * 128:(i - NTH + 1) * 128] if i >= NTH else hT[:, kc, i * 128:(i + 1) * 128]
                                P.mm(a_[:, 0:256], lhs, wA[:, kc, 0:256], start=(kc == 0), stop=(kc == KC - 1))
                            dst = vck[:, i - NTH, :] if i >= NTH else vtk[:, i, :]
                            if i % 2 == 0:
                                P.act(dst, a_[:, 0:256], AF.Copy)
                            else:
                                P.cp(dst, a_[:, 0:256])
                    P.barrier()
                    if half == 0:
                        stop("c1")
                    with ExitStack() as st2:
                        mk = [sb(st2, "mk%d" % i, [128, 5, 1024]) for i in range(2)]
                        scb = [sb(st2, "scb%d" % i, [128, 1024]) for i in range(2)]
                        pb_ = [sb(st2, "pbf%d" % i, [128, 1024], BF16) for i in range(2)]
                        ptb = [sb(st2, "ptb%d" % i, [128, 8, 128], BF16) for i in range(2)]
                        sm = sb(st2, "smx", [128, 8])
                        psS = [ps(st2, "psS%d" % i, [128, 1024]) for i in range(2)]
                        psT = [ps(st2, "psT%d" % i, [128, 1024], BF16) for i in range(2)]
                        psO = [ps(st2, "psO%d" % i, [128, 64]) for i in range(2)]
                        smd = [sb(st2, "smd%d" % i, [128, 8]) for i in range(2)]

                        def geom(rp):
                            cls = 0 if rp == 0 else 1 if rp == 1 else 3 if rp == 14 else 4 if rp == 15 else 2
                            brow = 0 if rp <= 1 else 28 if rp >= 14 else 2 * rp
                            return cls, brow * 64

                        def stA1(hl, rp, j):
                            hp, hf = hl // 2, (hl % 2) * 64
                            mkh = mk[hl % 2]
                            cls, k0 = geom(rp)
                            S_ = psS[j]
                            sm_ = smd[j]
                            qsl = slice(rp * 128, (rp + 1) * 128)
                            P.mm(S_[:, 0:256], qT[hf:hf + 64, hp, qsl], kcT[hf:hf + 64, hp, :])
                            P.mm(S_[:, 256:512], qrT[hf:hf + 64, hp, qsl], krT[hf:hf + 64, hp, k0:k0 + 256])
                            P.mm(S_[:, 512:1024], qrT[hf:hf + 64, hp, qsl], krT[hf:hf + 64, hp, k0 + 256:k0 + 768])
                            P.stt(scb[j], S_, 0.125, mkh[:, cls, :], ALU.mult, ALU.add)
                            P.rmax(sm_[:, 0:1], scb[j])
                            P.ts(sm_[:, 1:2], sm_[:, 0:1], -1.0, None, ALU.mult)

                        def stA2(hl, rp, j):
                            sm_ = smd[j]
                            P.act(pb_[j], scb[j], AF.Exp, bias=sm_[:, 1:2], accum=sm_[:, 2:3])

                        def stB1(hl, rp, j):
                            for c in range(8):
                                P.tr(psT[j][:, c * 128:(c + 1) * 128], pb_[j][:, c * 128:(c + 1) * 128], idb)
                            P.act(ptb[j].re("p c n -> p (c n)"), psT[j], AF.Copy)

                        def stB2(hl, rp, j):
                            h = half * 4 + hl
                            cls, k0 = geom(rp)
                            sm_ = smd[j]
                            t0 = k0 // 128
                            for c in range(8):
                                rhs = vck[:, c, hl * 64:(hl + 1) * 64] if c < 2 else vtk[:, t0 + c - 2, hl * 64:(hl + 1) * 64]
                                P.mm(psO[j], ptb[j][:, c, :], rhs, start=(c == 0), stop=(c == 7))
                            P.recip(sm_[:, 3:4], sm_[:, 2:3])
                            P.ts(na_tok[:, rp, h * 64:(h + 1) * 64], psO[j], sm_[:, 3:4], None, ALU.mult)

                        seq = [(hl, rp) for hl in range(4) for rp in range(NTO)]
                        for n_ in range(len(seq) + 1):
                            nxt = seq[n_] if n_ < len(seq) else None
                            cur = seq[n_ - 1] if n_ >= 1 else None
                            if nxt is not None:
                                if nxt[1] == 0:
                                    P.ld(mk[nxt[0] % 2].re("p c n -> p (c n)"), masks[half * 4 + nxt[0]])
                                stA1(nxt[0], nxt[1], n_ % 2)
                            if cur is not None:
                                stB1(cur[0], cur[1], (n_ - 1) % 2)
                            if nxt is not None:
                                stA2(nxt[0], nxt[1], n_ % 2)
                            if cur is not None:
                                stB2(cur[0], cur[1], (n_ - 1) % 2)
                    P.barrier()
            for i_ in range(NTO):
                P.ld(na_d[i_ * 128:(i_ + 1) * 128, :], na_tok[:, i_, :])
        P.barrier()
        stop("c")
        hg = ExitStack()
        open_stacks.append(hg)
        o0 = sb(hg, "o0", [128, NTO, 512], BF16)
        qseg = sb(hg, "qseg", [128, 8, TOK], BF16)
        Send = sb(hg, "Send", [128, 8, 128])
        Sctx = sb(hg, "Sctx", [128, 8, 128])
        Dtot = sb(hg, "Dtot", [128, 8])
        lbt = sb(hg, "lbt", [128, 16])
        lbn = sb(hg, "lbn", [128, 4])
        ones64 = sb(hg, "ones64", [128, 64])
        rmk = sb(hg, "rmk", [128, 4])
        mAt = sb(hg, "mAt", [128, 256])
        mAb = sb(hg, "mAb", [128, 256], BF16)
        P.ld(lbt[:, 0:8], lbl)
        P.ld(rmk, rowmask)
        P.ld(mAt, maskA)
        P.cp(mAb, mAt)
        P.memset(ones64, 1.0)
        P.tt(lbt[:, 8:12], lbt[:, 0:4], lbt[:, 4:8], ALU.subtract)
        P.act(lbt[:, 12:16], lbt[:, 8:12], AF.Sigmoid, scale=-1.0)
        P.act(lbt[:, 8:12], lbt[:, 8:12], AF.Sigmoid)
        P.ts(lbn, lbt[:, 12:16], -1.0, None, ALU.mult)
        with ExitStack() as st:
            ih = sb(st, "ih", [128, NTO, 512], BF16)
            ihc = sb(st, "ihc", [128, 2, 512], BF16)
            wst = [sb(st, "wst%d" % i, [128, 512]) for i in range(2)]
            rsm = sb(st, "rsm", [128, TOK])
            P.ld(rsm, resetm)
            hTo = hT[:, :, OWN0 * 128:OWN0 * 128 + TOK]
            with ExitStack() as st2:
                pa = [ps(st2, "pa%d" % i, [128, 512]) for i in range(2)]
                sgt = [sb(st2, "sgt%d" % i, [128, 512], BF16) for i in range(2)]
                wA = sb(st2, "wA", [128, KC, 512], BF16)
                for (blk, dstt, dstc, fn) in ((6, ih, ihc, AF.Copy), (7, None, None, AF.Sigmoid)):
                    load_w(st2, wst, wA, blk)
                    for i in range(NTO + (2 if dstc is not None else 0)):
                        a_ = pa[i % 2]
                        for kc in range(KC):
                            lhs = hcT[:, kc, (i - NTO) * 128:(i - NTO + 1) * 128] if i >= NTO else hTo[:, kc, i * 128:(i + 1) * 128]
                            P.mm(a_, lhs, wA[:, kc, :], start=(kc == 0), stop=(kc == KC - 1))
                        if dstt is None:
                            P.act(sgt[i % 2], a_, fn)
                            P.ld(sg_d[i * 128:(i + 1) * 128, :], sgt[i % 2])
                        else:
                            P.act(dstc[:, i - NTO, :] if i >= NTO else dstt[:, i, :], a_, fn)
            P.barrier()
            with ExitStack() as st2:
                wzs = [sb(st2, "wz%d" % i_, [128, KC, 128], BF16) for i_ in range(2)]
                sg_ = sb(st2, "s_", [128, TOK])
                binc = sb(st2, "binc", [128, TOK])
                qhh = sb(st2, "qhh", [128, TOK], BF16)
                wqs = [sb(st2, "wq%d" % i_, [128, KC, 128], BF16) for i_ in range(2)]
                kk = sb(st2, "kk", [128, TOK], BF16)
                kdec = sb(st2, "kdec", [128, TOK], BF16)
                kend = sb(st2, "kend", [128, TOK], BF16)
                qdec = sb(st2, "qdec", [128, TOK], BF16)
                sm = sb(st2, "hsm", [128, 4, 64])
                S = sb(st2, "S", [128, 128])
                tot = sb(st2, "tot", [128, 64])
                Sb = [sb(st2, "Sb%d" % i, [128, 4, 128], BF16) for i in range(2)]
                QM = [sb(st2, "QM%d" % i, [128, 640], BF16) for i in range(2)]
                kendT = [sb(st2, "kendT%d" % i, [128, 128], BF16) for i in range(2)]
                vm = [sb(st2, "vm%d" % i, [128, 4, 128], BF16) for i in range(2)]
                Am = [sb(st2, "Am%d" % i, [128, 128], BF16) for i in range(2)]
                pz = [ps(st2, "pz%d" % i, [128, 512]) for i in range(2)]
                pT = ps(st2, "pT", [128, 1024], BF16)
                pKV = [ps(st2, "pKV%d" % i, [128, 4, 128]) for i in range(2)]
                pA = ps(st2, "pA", [128, 128])
                pO = [ps(st2, "pO%d" % i, [128, 128]) for i in range(2)]
                P.memset(QM[0], 0.0)
                P.memset(QM[1], 0.0)
                from functools import partial as F_
                from itertools import zip_longest
                kdecs = [kdec, sb(st2, "kdec2", [128, TOK], BF16)]
                kends = [kend, sb(st2, "kend2", [128, TOK], BF16)]
                qdecs = [qdec, sb(st2, "qdec2", [128, TOK], BF16)]
                sms = [sm, sb(st2, "hsm2", [128, 4, 64])]
                jobs = [(ic, d, h) for ic in (True, False) for d in range(2) for h in range(4)]

                def Xops(job, par):
                    isctx, d, h = job
                    ops = []
                    A = ops.append
                    ntok = CTX if isctx else TOK
                    nch = ntok // 32
                    hsrc = hcT if isctx else hTo
                    k = d * 4 + h
                    kdec_, kend_, qdec_, sm_ = kdecs[par], kends[par], qdecs[par], sms[par]
                    wz, wq = wzs[k % 2], wqs[k % 2]
                    A(F_(load_w, st2, wst, wz, None, 128, (4 + d) * 512 + h * 128))
                    nbs = [(0, 256)] if isctx else [(i * 512, 512) for i in range(4)]
                    for bi, (c0, cn) in enumerate(nbs):
                        a_ = pz[bi % 2]
                        for kc in range(KC):
                            A(F_(P.mm, a_[:, 0:cn], wz[:, kc, :], hsrc[:, kc, c0:c0 + cn], start=(kc == 0), stop=(kc == KC - 1)))
                        A(F_(P.act, sg_[:, c0:c0 + cn], a_[:, 0:cn], AF.Sigmoid))
                    sv = sg_[:, 0:ntok]
                    A(F_(P.ts, kk[:, 0:ntok], sv, lbn[:, h:h + 1], lbt[:, 12 + h:13 + h], ALU.mult, ALU.add))
                    A(F_(P.act, sv, sv, AF.Ln, bias=lbt[:, 8 + h:9 + h], scale=lbt[:, 12 + h:13 + h]))
                    A(F_(P.scan, binc[:, 0:ntok], rsm[:, 0:ntok], sv, 0.0, ALU.mult, ALU.add))
                    b3 = binc[:, 0:ntok].re("p (c t) -> p c t", t=32)
                    A(F_(P.cp, tot[:, 0:nch], b3[:, :, 31]))
                    bend = tot[:, 0:nch]
                    B = binc[:, 0:ntok]
                    if d == 1:
                        A(F_(P.tt, B, sv, B, ALU.subtract))
                        A(F_(P.tt, b3, b3, bend.re("p (c o) -> p c o", o=1).bc([128, nch, 32]), ALU.add))
                    Ee = sg_
                    A(F_(P.act, sm_[:, 2, 0:nch], bend, AF.Exp))
                    A(F_(P.scan, sm_[:, 0, 0:nch], ones64[:, 0:nch], bend, 0.0, ALU.mult, ALU.add))
                    if d == 0:
                        A(F_(P.tt, sm_[:, 1, 0:nch], sm_[:, 0, 0:nch], bend, ALU.subtract))
                    else:
                        A(F_(P.ts, sm_[:, 1, 0:nch], sm_[:, 0, 0:nch], -1.0, sm_[:, 0, nch - 1:nch], ALU.mult, ALU.add))
                    A(F_(P.act, sm_[:, 3, 0:nch], sm_[:, 1, 0:nch], AF.Exp))
                    if not isctx:
                        A(F_(P.act, Dtot[:, k:k + 1], sm_[:, 0, nch - 1:nch], AF.Exp))
                        A(F_(P.act, Ee[:, 0:ntok], B, AF.Exp))
                        A(F_(load_w, st2, wst, wq, None, 128, 3 * 512 + h * 128))
                        for nb in range(4):
                            a_ = pz[nb % 2]
                            for kc in range(KC):
                                A(F_(P.mm, a_, wq[:, kc, :], hTo[:, kc, nb * 512:(nb + 1) * 512], start=(kc == 0), stop=(kc == KC - 1)))
                            A(F_(P.cp, qhh[:, nb * 512:(nb + 1) * 512], a_))
                        A(F_(P.tt, qdec_[:, 0:ntok], qhh, Ee[:, 0:ntok], ALU.mult))
                        A(F_(P.tt, qseg[:, k, :].re("p (c t) -> p c t", t=32), qdec_.re("p (c t) -> p c t", t=32),
                             sm_[:, 3, 0:nch].re("p (c o) -> p c o", o=1).bc([128, nch, 32]), ALU.mult))
                    A(F_(P.act, Ee[:, 0:ntok], B, AF.Exp, scale=-1.0))
                    A(F_(P.tt, kdec_[:, 0:ntok], kk[:, 0:ntok], Ee[:, 0:ntok], ALU.mult))
                    A(F_(P.tt, kend_[:, 0:ntok].re("p (c t) -> p c t", t=32), kdec_[:, 0:ntok].re("p (c t) -> p c t", t=32),
                         sm_[:, 2, 0:nch].re("p (c o) -> p c o", o=1).bc([128, nch, 32]), ALU.mult))
                    return ops

                def Tops(job, par):
                    isctx, d, h = job
                    ops = []
                    A = ops.append
                    ntok = CTX if isctx else TOK
                    nt = ntok // 128
                    vsrc = ihc if isctx else ih
                    k = d * 4 + h
                    kdec_, kend_, qdec_, sm_ = kdecs[par], kends[par], qdecs[par], sms[par]
                    A(F_(P.memset, S, 0.0, eng="dve"))
                    tiles = list(range(nt)) if d == 0 else list(range(nt - 1, -1, -1))
                    chs = [0, 1, 2, 3] if d == 0 else [3, 2, 1, 0]
                    for ti, i in enumerate(tiles):
                        j2 = ti % 2
                        tsl = slice(i * 128, (i + 1) * 128)
                        hc = slice(h * 128, (h + 1) * 128)
                        A(F_(P.tr, pT[:, 0:128], kend_[:, tsl], idb))
                        A(F_(P.act, kendT[j2], pT[:, 0:128], AF.Copy))
                        A(F_(P.tt, vm[j2], vsrc[:, i, hc].re("p (o v) -> p o v", o=1).bc([128, 4, 128]),
                             rmk.re("p (j o) -> p j o", o=1).bc([128, 4, 128]), ALU.mult))
                        for j in range(4):
                            A(F_(P.mm, pKV[j2][:, j, :], kendT[j2], vm[j2][:, j, :]))
                        if not isctx:
                            A(F_(P.mm, pA, kdec_[:, tsl], qdec_[:, tsl]))
                            A(F_(P.tt, Am[j2], pA, mAb[:, d * 128:(d + 1) * 128], ALU.mult))
                            A(F_(P.act, QM[j2].re("p (j x) -> p j x", x=160)[:, :, 0:32],
                                 qdec_[:, tsl].re("p (j t) -> p j t", t=32), AF.Copy))
                        for j in chs:
                            if not isctx:
                                A(F_(P.act, Sb[j2][:, j, :], S, AF.Copy))
                            A(F_(P.stt, S, S, sm_[:, 2, i * 4 + j:i * 4 + j + 1], pKV[j2][:, j, :], ALU.mult, ALU.add))
                        if not isctx:
                            A(F_(P.mm, pO[j2], Am[j2], vsrc[:, i, hc], start=True, stop=False))
                            for j in range(4):
                                A(F_(P.mm, pO[j2], QM[j2][:, j * 128:(j + 1) * 128], Sb[j2][:, j, :], start=False, stop=(j == 3)))
                            if d == 0:
                                A(F_(P.cp, o0[:, i, hc], pO[j2]))
                            else:
                                A(F_(P.tt, o0[:, i, hc], o0[:, i, hc], pO[j2], ALU.add))
                    A(F_(P.cp, (Sctx if isctx else Send)[:, k, :], S))
                    return ops

                for o_ in Xops(jobs[0], 0):
                    o_()
                for n_ in range(len(jobs)):
                    ta = Tops(jobs[n_], n_ % 2)
                    xb = Xops(jobs[n_ + 1], (n_ + 1) % 2) if n_ + 1 < len(jobs) else []
                    for x_, y_ in zip_longest(ta, xb):
                        if x_ is not None:
                            x_()
                        if y_ is not None:
                            y_()
        P.barrier()
        stop("d")
        s1.close()
        open_stacks.remove(s1)
        Ssb = sb(hg, "Ssb", [128, 8, 128], BF16)
        with ExitStack() as st:
            pD = ps(st, "pD", [8, 128])
            dT = sb(st, "dT", [8, 128])
            mf = sb(st, "mf", [128, 8])
            U = [sb(st, "U%d" % i, [128, 128]) for i in range(2)]
            Dj = [sb(st, "Dj%d" % i, [128, 4]) for i in range(2)]
            P.ld(mf, mfold)
            P.tr(pD, Dtot, idf)
            P.cp(dT, pD)
            for k in range(8):
                P.ld(st_in[k * 128:(k + 1) * 128, :], Send[:, k, :])
            P.ld(st_in[1024:1032, :], dT)
            P.cc("AllGather", ALU.bypass, st_in, st_all)
            it = 0
            for k in range(8):
                d = k // 4
                Sk = Sctx[:, k, :]
                for j in ([0, 1, 2, 3] if d == 0 else [3, 2, 1, 0]):
                    u_, d_ = U[it % 2], Dj[it % 2]
                    it += 1
                    P.ld(u_, st_all[j * 1032 + k * 128:j * 1032 + (k + 1) * 128, :])
                    P.ld(d_[:, 0:1], st_all[j * 1032 + 1024 + k:j * 1032 + 1025 + k, :].re("o d -> d o"))
                    m_ = mf[:, d * 4 + j:d * 4 + j + 1]
                    P.ts(d_[:, 1:2], d_[:, 0:1], -1.0, m_, ALU.add, ALU.mult)
                    P.ts(d_[:, 1:2], d_[:, 1:2], 1.0, None, ALU.add)
                    P.ts(u_, u_, m_, None, ALU.mult)
                    P.stt(Sk, Sk, d_[:, 1:2], u_, ALU.mult, ALU.add)
                P.cp(Ssb[:, k, :], Sk)
        P.barrier()
        stop("e")
        with ExitStack() as st:
            woutb = sb(st, "woutb", [128, KC, D], BF16)
            gt1g = sb(st, "gt1g", [128, D])
            a2 = sb(st, "a2", [128, D])
            sh2 = sb(st, "sh2", [128, D])
            for q_, t_ in enumerate((gt1g, a2, sh2)):
                P.ld(t_, modsc[q_ * 128:(q_ + 1) * 128, :])
            wst2 = [sb(st, "wst2%d" % i, [128, D]) for i in range(2)]
            wrs = sb(st, "wrs", [128, KC, 16])
            hgn = sb(st, "hgn", [128, 512])
            affT = sb(st, "affT", [16, TOK])
            ot = sb(st, "ot", [128, 512])
            hgt = sb(st, "hgt", [128, 512], BF16)
            nat = [sb(st, "nat%d" % i, [128, 512], BF16) for i in range(2)]
            sgl = [sb(st, "sgl%d" % i, [128, 512], BF16) for i in range(2)]
            xt = [sb(st, "xt%d" % i, [128, D]) for i in range(2)]
            x1t = [sb(st, "x1t%d" % i, [128, D]) for i in range(2)]
            h2t = [sb(st, "h2t%d" % i, [128, D]) for i in range(2)]
            h2b = [sb(st, "h2b%d" % i, [128, D], BF16) for i in range(2)]
            mixT = sb(st, "mixT", [128, KC, 128], BF16)
            h2T = sb(st, "h2T", [128, KC, 128])
            junk = sb(st, "junk2", [128, D])
            sm = sb(st, "rsm2", [128, 16])
            lg = sb(st, "lg", [128, 16])
            psC = ps(st, "psC", [128, 512])
            psT = ps(st, "psT", [128, 1024], BF16)
            psM = ps(st, "psM", [128, 1024])
            psT32 = ps(st, "psT32", [128, 1024])
            psL = ps(st, "psL", [128, 128])
            woutv = wout.re("(kc p) n -> p kc n", p=128)
            for kc in range(KC):
                P.ld(wst2[kc % 2], woutv[:, kc, :])
                P.cp(woutb[:, kc, :], wst2[kc % 2], eng="pool")
            P.ld(wrs, wr.re("(kc p) n -> p kc n", p=128))
            P.ld(hgn, hgnb)
            xhv = xh.re("(n p) d -> n p d", p=128)
            from functools import partial as F_
            from itertools import zip_longest
            smA = [sb(st, "smA%d" % i, [128, 8]) for i in range(2)]
            smB = [sb(st, "smB%d" % i, [128, 8]) for i in range(2)]
            smC = [sb(st, "smC%d" % i, [128, 8]) for i in range(2)]
            junkA = sb(st, "junkA", [128, 128])

            def S1(i):
                ops = []
                A = ops.append
                j2 = i % 2
                tsl = slice(i * 128, (i + 1) * 128)
                sm_ = smA[j2]
                A(F_(P.ld, nat[j2], na_d[tsl, :]))
                A(F_(P.ld, sgl[j2], sg_d[tsl, :]))
                A(F_(P.ld, xt[j2], xhv[i + OWN0]))
                for h in range(4):
                    A(F_(P.mm, psC[:, h * 128:(h + 1) * 128], qseg[:, h, tsl], Ssb[:, h, :], start=True, stop=False))
                    A(F_(P.mm, psC[:, h * 128:(h + 1) * 128], qseg[:, 4 + h, tsl], Ssb[:, 4 + h, :], start=False, stop=True))
                A(F_(P.tt, ot, o0[:, i, :], psC, ALU.add))
                for h in range(4):
                    A(F_(P.act, junkA, ot[:, h * 128:(h + 1) * 128], AF.Square, accum=sm_[:, h:h + 1]))
                A(F_(P.act, sm_[:, 4:8], sm_[:, 0:4], AF.Sqrt, bias=epsc[:, 0:1], scale=1.0 / 128))
                A(F_(P.recip, sm_[:, 4:8], sm_[:, 4:8]))
                o3 = ot.re("p (h v) -> p h v", v=128)
                A(F_(P.tt, o3, o3, sm_[:, 4:8].re("p (h o) -> p h o", o=1).bc([128, 4, 128]), ALU.mult))
                A(F_(P.tt, ot, ot, hgn, ALU.mult))
                A(F_(P.tt, hgt, ot, sgl[j2], ALU.mult))
                for c in range(4):
                    A(F_(P.tr, psT[:, c * 128:(c + 1) * 128], nat[j2][:, c * 128:(c + 1) * 128], idb))
                    A(F_(P.tr, psT[:, (4 + c) * 128:(5 + c) * 128], hgt[:, c * 128:(c + 1) * 128], idb))
                A(F_(P.act, mixT.re("p c n -> p (c n)"), psT, AF.Copy))
                for nb in range(2):
                    for mc in range(KC):
                        A(F_(P.mm, psM[:, nb * 512:(nb + 1) * 512], mixT[:, mc, :], woutb[:, mc, nb * 512:(nb + 1) * 512], start=(mc == 0), stop=(mc == KC - 1)))
                return ops

            def S2(i):
                ops = []
                A = ops.append
                j2 = i % 2
                tsl = slice(i * 128, (i + 1) * 128)
                sm_ = smB[j2]
                A(F_(P.act, junk, psM, AF.Square, accum=sm_[:, 0:1]))
                A(F_(P.act, sm_[:, 1:2], sm_[:, 0:1], AF.Sqrt, bias=epsc[:, 0:1], scale=1.0 / D))
                A(F_(P.recip, sm_[:, 1:2], sm_[:, 1:2]))
                A(F_(P.stt, x1t[j2], psM, sm_[:, 1:2], gt1g, ALU.mult, ALU.mult))
                A(F_(P.tt, x1t[j2], x1t[j2], xt[j2], ALU.add))
                A(F_(P.ld, x1_d[tsl, :], x1t[j2]))
                if debug:
                    A(F_(P.ld, dbg_x1[tsl, :], x1t[j2]))
                A(F_(P.act, junk, x1t[j2], AF.Square, accum=sm_[:, 2:3]))
                A(F_(P.act, sm_[:, 3:4], sm_[:, 2:3], AF.Sqrt, bias=epsc[:, 0:1], scale=1.0 / D))
                A(F_(P.recip, sm_[:, 3:4], sm_[:, 3:4]))
                A(F_(P.stt, h2t[j2], x1t[j2], sm_[:, 3:4], a2, ALU.mult, ALU.mult))
                A(F_(P.tt, h2t[j2], h2t[j2], sh2, ALU.add))
                A(F_(P.act, h2b[j2], h2t[j2], AF.Copy))
                A(F_(P.ld, h2_in[tsl, :], h2b[j2]))
                sm_ = smC[j2]
                for kc in range(KC):
                    A(F_(P.tr, psT32[:, kc * 128:(kc + 1) * 128], h2t[j2][:, kc * 128:(kc + 1) * 128], idf))
                A(F_(P.cp, h2T.re("p c n -> p (c n)"), psT32))
                for kc in range(KC):
                    A(F_(P.mm, psL[:, 0:16], h2T[:, kc, :], wrs[:, kc, :], start=(kc == 0), stop=(kc == KC - 1)))
                A(F_(P.rmax, sm_[:, 0:1], psL[:, 0:16]))
                A(F_(P.ts, sm_[:, 1:2], sm_[:, 0:1], -1.0, None, ALU.mult))
                A(F_(P.act, lg, psL[:, 0:16], AF.Exp, bias=sm_[:, 1:2], accum=sm_[:, 2:3]))
                A(F_(P.recip, sm_[:, 3:4], sm_[:, 2:3]))
                A(F_(P.ts, lg, lg, sm_[:, 3:4], None, ALU.mult))
                A(F_(P.tr, psL[0:16, :], lg, idf))
                A(F_(P.cp, affT[:, tsl], psL[0:16, :]))
                return ops

            for i in range(NTO + 1):
                oa = S1(i) if i < NTO else []
                ob = S2(i - 1) if i >= 1 else []
                for x_, y_ in zip_longest(oa, ob):
                    if x_ is not None:
                        x_()
                    if y_ is not None:
                        y_()
            P.ld(af_in, affT)
            if debug:
                P.ld(dbg_aff, affT)
        hg.close()
        open_stacks.remove(hg)
        stop("f")
        P.barrier()
        P.cc("AllGather", ALU.bypass, af_in, af_all)
        for ch in range(4):
            P.cc("AllGather", ALU.bypass, h2_in[ch * 512:(ch + 1) * 512, :], h2_all[ch * 2048:(ch + 1) * 2048, :])
        stop("1")
        rt = ExitStack()
        idx_i = sb(rt, "idx_i", [128, 4, 8], I32)
        idx_t = sb(rt, "idx_t", [128, 4, 8], I32)
        gate = sb(rt, "gate", [128, 4, 8])
        with ExitStack() as st:
            A = sb(st, "A", [64, TOK])
            selb = sb(st, "selb", [64, TOK])
            incl = sb(st, "incl", [64, TOK])
            ones = sb(st, "ones", [64, TOK])
            Gs = sb(st, "Gs", [64, 64])
            Gp = sb(st, "Gp", [64, 64])
            sM = sb(st, "sM", [64, 16])
            bs = sb(st, "bs", [64, 8])
            rkM = sb(st, "rkM", [128, 16, 16])
            afM = sb(st, "afM", [128, 16, 16])
            res = sb(st, "res", [128, 256])
            gb = sb(st, "gb", [128, 256], BF16)
            vals0 = sb(st, "vals0", [128, 768])
            VALS = sb(st, "VALS", [128, 256, 8], BF16)
            iot = sb(st, "iot", [128, 1024])
            oh = [sb(st, "oh%d" % i, [128, 1024], BF16) for i in range(2)]
            idxf = sb(st, "idxf", [128, 8, 8])
            tokf = sb(st, "tokf", [128, 8])
            P.ld(A, af_all)
            P.ld(Gs, Gm)
            P.ld(Gp, Gpre)
            P.ld(sM, selM)
            P.ld(iot, iota1k)
            P.ld(vals0, tokc)
            P.memset(ones, 1.0)
            P.memset(bs, 0.0)
            P.memset(bs[:, 1:2], 1.0)
            with ExitStack() as st2:
                psb = ps(st2, "psb", [64, 8])
                psr = [ps(st2, "psr%d" % i, [128, 16]) for i in range(2)]
                lo, hi, mid, cpart, cond, tmp = (bs[:, i:i + 1] for i in range(6))
                for it in range(32):
                    P.tt(mid, lo, hi, ALU.add)
                    P.ts(mid, mid, 0.5, None, ALU.mult)
                    P.ts(selb, A, mid, 0.0, ALU.is_gt, ALU.add, accum=cpart)
                    P.mm(psb[:, 0:1], Gs, cpart)
                    P.ts(cond, psb[:, 0:1], 1024.0, None, ALU.is_ge)
                    P.tt(tmp, mid, lo, ALU.subtract)
                    P.stt(lo, tmp, cond, lo, ALU.mult, ALU.add)
                    P.tt(tmp, hi, mid, ALU.subtract)
                    P.stt(hi, tmp, cond, mid, ALU.mult, ALU.add)
                P.ts(selb, A, lo, None, ALU.is_gt)
                P.scan(incl, ones, selb, 0.0, ALU.mult, ALU.add)
                P.mm(psb[:, 1:2], Gp, incl[:, TOK - 1:TOK])
                P.cp(tmp, psb[:, 1:2])
                P.stt(incl, incl, tmp, selb, ALU.add, ALU.mult)
                P.ts(incl, incl, -1.0, None, ALU.add)
                for j in range(16):
                    P.mm(psr[0], incl[:, j * 128:(j + 1) * 128], sM)
                    P.cp(rkM[:, j, :], psr[0])
                    P.mm(psr[1], A[:, j * 128:(j + 1) * 128], sM)
                    P.cp(afM[:, j, :], psr[1])
            af2 = afM.re("p j c -> p (j c)")
            V3 = VALS
            P.cp(V3[:, :, 0], vals0[:, 0:256])
            P.cp(V3[:, :, 1], vals0[:, 256:512])
            P.cp(gb, af2)
            P.cp(V3[:, :, 2], gb)
            P.tt(res, af2, gb, ALU.subtract)
            P.cp(gb, res)
            P.cp(V3[:, :, 3], gb)
            P.tt(res, res, gb, ALU.subtract)
            P.cp(V3[:, :, 4], res)
            P.cp(V3[:, :, 5], vals0[:, 512:768])
            P.memset(V3[:, :, 6:8], 0.0)
            with ExitStack() as st2:
                pI = [ps(st2, "pI%d" % g, [128, 512]) for g in range(8)]
                n = 0
                for i in range(4):
                    cnt = 0
                    for r in range(4):
                        for j in range(16):
                            col = r * 4 + i
                            o_ = oh[n % 2]
                            P.ts(o_, iot, rkM[:, j, col:col + 1], None, ALU.is_equal)
                            n += 1
                            for g in range(8):
                                P.mm(pI[g][:, 0:8], o_[:, g * 128:(g + 1) * 128], V3[:, j * 16 + col, :], start=(cnt == 0), stop=(cnt == 63))
                            cnt += 1
                    for g in range(8):
                        P.cp(idxf[:, g, :], pI[g][:, 0:8])
                    P.stt(tokf, idxf[:, :, 0], 128.0, idxf[:, :, 1], ALU.mult, ALU.add)
                    P.cp(idx_i[:, i, :], tokf)
                    P.stt(tokf, idxf[:, :, 5], 128.0, idxf[:, :, 1], ALU.mult, ALU.add)
                    P.cp(idx_t[:, i, :], tokf)
                    P.tt(gate[:, i, :], idxf[:, :, 2], idxf[:, :, 3], ALU.add)
                    P.tt(gate[:, i, :], gate[:, i, :], idxf[:, :, 4], ALU.add)
        if debug:
            dbt = sb(rt, "dbt", [128, 64])
            P.cp(dbt[:, 0:32], idx_t.re("p a b -> p (a b)"))
            P.cp(dbt[:, 32:64], gate.re("p a b -> p (a b)"))
            P.ld(dbg_idx, dbt)
        P.barrier()
        stop("r")
        with ExitStack() as st:
            xsT = sb(st, "xsT", [128, KC, 1024], BF16)
            hidT = sb(st, "hidT", [128, NFC, 1024], BF16)
            wdb = sb(st, "wdb", [128, NFC, D], BF16)
            wgb = [sb(st, "wgb%d" % i, [128, KC, 256], BF16) for i in range(2)]
            wub = [sb(st, "wub%d" % i, [128, KC, 256], BF16) for i in range(2)]
            xg = [sb(st, "xg%d" % i, [128, D], BF16) for i in range(2)]
            yt = [sb(st, "yt%d" % i, [128, D]) for i in range(2)]
            sil = [sb(st, "sil%d" % i, [128, 512]) for i in range(2)]
            pT = ps(st, "pTx", [128, 1024], BF16)
            pg = [ps(st, "pg%d" % i, [128, 512]) for i in range(2)]
            pu = [ps(st, "pu%d" % i, [128, 512]) for i in range(2)]
            py = [ps(st, "py%d" % i, [128, 512]) for i in range(2)]
            nq = 0
            for i in range(4):
                for g in range(8):
                    P.gather(xg[g % 2], h2_all, idx_i[:, i, g:g + 1])
                    for kc in range(KC):
                        P.tr(pT[:, kc * 128:(kc + 1) * 128], xg[g % 2][:, kc * 128:(kc + 1) * 128], idb)
                    P.act(xsT[:, :, g * 128:(g + 1) * 128], pT.re("p (c n) -> p c n", n=128), AF.Copy)
                wgv = wg4[i].re("(kc p) f -> p kc f", p=128)
                wuv = wu4[i].re("(kc p) f -> p kc f", p=128)
                for fb in range(11):
                    f0 = fb * 256
                    fn = min(256, DE - f0)
                    gb_, ub_ = wgb[fb % 2], wub[fb % 2]
                    P.ld(gb_[:, :, 0:fn], wgv[:, :, f0:f0 + fn], q="pool")
                    P.ld(ub_[:, :, 0:fn], wuv[:, :, f0:f0 + fn], q="pool")
                    for fc_ in (2 * fb, 2 * fb + 1):
                        m_ = 128 if fc_ < 21 else 64
                        P.ld(wdb[0:m_, fc_, :], wd4[i][fc_ * 128:fc_ * 128 + m_, :], q="pool")
                    for c in range((fn + 127) // 128):
                        fc = fb * 2 + c
                        m = min(128, fn - c * 128)
                        for half in range(2):
                            a_, b_ = pg[nq % 2], pu[nq % 2]
                            s_ = sil[nq % 2]
                            nq += 1
                            for kc in range(KC):
                                P.mm(a_[0:m, :], gb_[:, kc, c * 128:c * 128 + m], xsT[:, kc, half * 512:(half + 1) * 512], start=(kc == 0), stop=(kc == KC - 1))
                            for kc in range(KC):
                                P.mm(b_[0:m, :], ub_[:, kc, c * 128:c * 128 + m], xsT[:, kc, half * 512:(half + 1) * 512], start=(kc == 0), stop=(kc == KC - 1))
                            P.act(s_[0:m, :], a_[0:m, :], AF.Silu)
                            P.tt(hidT[0:m, fc, half * 512:(half + 1) * 512], s_[0:m, :], b_[0:m, :], ALU.mult)
                for ct in range(8):
                    y_ = yt[ct % 2]
                    for nb in range(2):
                        p_ = py[nb]
                        for fc in range(NFC):
                            m = 128 if fc < 21 else 64
                            P.mm(p_, hidT[0:m, fc, ct * 128:(ct + 1) * 128], wdb[0:m, fc, nb * 512:(nb + 1) * 512], start=(fc == 0), stop=(fc == NFC - 1))
                        P.ts(y_[:, nb * 512:(nb + 1) * 512], p_, gate[:, i, ct:ct + 1], None, ALU.mult)
                    P.scatter_add(acc, y_, idx_t[:, i, ct:ct + 1])
        rt.close()
        P.barrier()
        P.cc("ReduceScatter", ALU.add, acc, rs_out)
        with ExitStack() as st:
            mt = [sb(st, "mt%d" % i, [128, D]) for i in range(2)]
            x1l = [sb(st, "x1l%d" % i, [128, D]) for i in range(2)]
            junk = sb(st, "junk3", [128, D])
            gt2g = sb(st, "gt2g", [128, D])
            P.ld(gt2g, modsc[3 * 128:4 * 128, :])
            sm = sb(st, "fsm", [128, 4])
            for i in range(NTO):
                j2 = i % 2
                tsl = slice(i * 128, (i + 1) * 128)
                P.ld(mt[j2], rs_out[tsl, :])
                P.ld(x1l[j2], x1_d[tsl, :])
                P.act(junk, mt[j2], AF.Square, accum=sm[:, 0:1])
                P.act(sm[:, 1:2], sm[:, 0:1], AF.Sqrt, bias=epsc[:, 0:1], scale=1.0 / D)
                P.recip(sm[:, 1:2], sm[:, 1:2])
                P.stt(mt[j2], mt[j2], sm[:, 1:2], gt2g, ALU.mult, ALU.mult)
                P.tt(mt[j2], mt[j2], x1l[j2], ALU.add)
                P.ld(out[tsl, :], mt[j2])
    except _Stop:
        pass
    for stx in reversed(open_stacks):
        stx.close()
    P.barrier()
    top.close()
    return nc


def _consts():
    f = np.float32
    p = np.arange(128)
    cst = {}
    cst["identf"] = np.eye(128, dtype=f)
    sidx, cidx = p[:, None], p[None, :]
    same = (sidx // 32) == (cidx // 32)
    cst["maskA"] = np.concatenate([(same & (cidx >= sidx)), (same & (cidx <= sidx))], axis=1).astype(f)
    cst["rowmask"] = ((p[:, None] // 32) == np.arange(4)[None, :]).astype(f)
    rm = np.ones((128, TOK), f)
    rm[:, 0::32] = 0.0
    cst["resetm"] = rm
    cst["iota1k"] = np.broadcast_to(np.arange(1024, dtype=f)[None, :], (128, 1024)).copy()
    tok = np.zeros((128, 768), f)
    for j in range(16):
        for col in range(16):
            r = col // 4
            tok[:, j * 16 + col] = (j // 4) * 16 + r * 4 + (j % 4)
            tok[:, 512 + j * 16 + col] = r * 16 + j
    tok[:, 256:512] = p[:, None].astype(f)
    cst["tokc"] = tok
    re_ = np.arange(64)
    r_, e_ = re_ // 16, re_ % 16
    cst["Gm"] = (e_[:, None] == e_[None, :]).astype(f)
    cst["Gpre"] = ((e_[:, None] == e_[None, :]) & (r_[:, None] < r_[None, :])).astype(f)
    return cst


def prep(x, c, ctx, c_ctx, w_mod, b_mod, g_pre1, g_post1, g_pre2, g_post2, w_in, w_out,
         na_rpb, hg_lb_logits, hg_norm, w_router, w_gate, w_up, w_down):
    f = np.float32
    A_ = lambda a: np.ascontiguousarray(np.asarray(a, dtype=f))
    x, c, ctx, c_ctx = A_(x), A_(c), A_(ctx), A_(c_ctx)
    w_mod, b_mod, w_in, w_out = A_(w_mod)[0], A_(b_mod)[0], A_(w_in)[0], A_(w_out)[0]
    rpb = A_(na_rpb)[0]
    lbl_ = A_(hg_lb_logits)
    hgn_ = A_(hg_norm)[0]
    wr_ = A_(w_router)[0]
    wg_, wu_, wd_ = np.asarray(w_gate)[0], np.asarray(w_up)[0], np.asarray(w_down)[0]
    bc = lambda v: np.ascontiguousarray(np.broadcast_to(v[None, :], (128, v.shape[0])))
    pp = np.arange(512)
    perm = np.where(pp % 32 < 16, pp + 16, pp - 16)
    winx = np.ascontiguousarray(np.concatenate([w_in, w_in[:, 0:512][:, perm], w_in[:, 512:1024][:, perm]], axis=1))
    cst = _consts()
    shared = dict(cst)
    shared.update(wmod=w_mod, bmodb=bc(b_mod), g1b=bc(A_(g_pre1)[0]), gp1b=bc(A_(g_post1)[0]),
                  g2b=bc(A_(g_pre2)[0]), gp2b=bc(A_(g_post2)[0]), winx=winx, wout=w_out,
                  hgnb=bc(np.tile(hgn_, 4)), wr=wr_,
                  lbl=np.ascontiguousarray(lbl_.reshape(2, 4, 128).transpose(2, 0, 1).reshape(128, 8)))
    ccrep = np.ascontiguousarray(np.broadcast_to(c_ctx.reshape(KC, 128).T[:, :, None], (128, KC, 128)).reshape(128, KC * 128))
    d64 = np.arange(128) % 64
    seg = d64 // 32
    first = (d64 % 32) < 16
    inv = (10000.0 ** (-(2.0 * (d64 % 16)) / 32.0)).astype(f)
    in_maps = []
    for core in range(8):
        b, s = core // 4, core % 4
        m = dict(shared)
        xh = np.zeros((HTOK, D), f)
        g0 = TOK * s - 256
        lo, hi = max(g0, 0), min(g0 + HTOK, 8192)
        xh[lo - g0:hi - g0] = x[b, lo:hi]
        m["xh"] = xh
        m["ctxb"] = ctx[b]
        m["crep"] = np.ascontiguousarray(np.broadcast_to(c[b].reshape(KC, 128).T[:, :, None], (128, KC, 128)).reshape(128, KC * 128))
        m["ccrep"] = ccrep
        tl = np.arange(HTOK)
        row = (32 * s - 4 + tl // 64).astype(f)
        colp = (tl % 64).astype(f)
        pos = np.where(seg[:, None] == 0, row[None, :], colp[None, :]).astype(f)
        ang = (pos * inv[:, None]).astype(f)
        m["cosT"] = np.cos(ang).astype(f)
        m["sinT"] = np.where(first[:, None], -np.sin(ang), np.sin(ang)).astype(f)
        mk = np.zeros((8, 128, 5, 1024), f)
        qp = np.arange(128)
        a_, qc = qp // 64, qp % 64
        nn = np.arange(768)
        for cls, rp in enumerate((0, 1, 4, 14, 15)):
            brow = 0 if rp <= 1 else 28 if rp >= 14 else 2 * rp
            r = 32 * s + 2 * rp + a_
            grow = 32 * s - 4 + brow + nn // 64
            kc_ = nn % 64
            r0 = np.clip(r - 4, 0, 120)
            vrow = (grow[None, :] >= r0[:, None]) & (grow[None, :] < r0[:, None] + 8)
            wc0 = np.clip(qc - 8, 0, 48)
            vcol = (kc_[None, :] >= wc0[:, None]) & (kc_[None, :] < wc0[:, None] + 16)
            dr = np.clip(grow[None, :] - r[:, None] + 7, 0, 14)
            dc = np.clip(kc_[None, :] - qc[:, None] + 15, 0, 30)
            bias = rpb[:, dr, dc]
            mk[:, :, cls, 256:1024] = np.where((vrow & vcol)[None], bias, f(NEG))
        m["masks"] = mk.reshape(8, 128, 5 * 1024)
        mf = np.zeros((128, 8), f)
        for j in range(4):
            mf[:, j] = 1.0 if j < s else 0.0
            mf[:, 4 + j] = 1.0 if j > s else 0.0
        m["mfold"] = mf
        sel = np.zeros((64, 16), f)
        for r in range(4):
            for i in range(4):
                sel[r * 16 + 4 * s + i, r * 4 + i] = 1.0
        m["selM"] = sel
        m["selO"] = np.zeros((64, 16), f)
        m["wg4"] = np.ascontiguousarray(wg_[4 * s:4 * s + 4], dtype=f)
        m["wu4"] = np.ascontiguousarray(wu_[4 * s:4 * s + 4], dtype=f)
        m["wd4"] = np.ascontiguousarray(wd_[4 * s:4 * s + 4], dtype=f)
        in_maps.append(m)
    return in_maps


def kernel(**inputs):
    f = np.float32
    in_maps = prep(**inputs)
    import os
    dbg = os.environ.get("KDEBUG", "") == "1"
    nc = build(debug=dbg)
    res = run_bass_kernel_spmd(nc, in_maps, core_ids=list(range(8)))
    if dbg:
        global LAST
        LAST = res.results
    out = np.zeros((2, 8192, D), f)
    for core in range(8):
        b, s = core // 4, core % 4
        out[b, TOK * s:TOK * (s + 1)] = res.results[core]["out"]
    return out
```

```python
import numpy as np
from contextlib import ExitStack
import concourse.bass as bass
import concourse.mybir as mybir
from concourse.bass_utils import run_bass_kernel_spmd

F32 = mybir.dt.float32
BF16 = mybir.dt.bfloat16
I32 = mybir.dt.int32
ALU = mybir.AluOpType
AF = mybir.ActivationFunctionType
AX = mybir.AxisListType

D = 1024
KC = 8
NTO = 16
NTH = 20
OWN0 = 2
TOK = 2048
HTOK = 2560
CTX = 256
DE = 2752
NFC = 22
EPS = 1e-6
GROUPS = [[0, 1, 2, 3], [4, 5, 6, 7]]
NDS = 24
NEG = -30000.0


class _Stop(Exception):
    pass


class V:
    def __init__(s, ap, key):
        s.ap = ap
        s.key = key

    def __getitem__(s, idx):
        return V(s.ap[idx], s.key)

    def sub(s, k):
        return V(s.ap, s.key + "/" + str(k))

    def re(s, pat, **kw):
        return V(s.ap.rearrange(pat, **kw), s.key)

    def bc(s, shape):
        return V(s.ap.to_broadcast(shape), s.key)


def _ap(x):
    return x.ap if isinstance(x, V) else x


def _keys(*xs):
    return [x.key for x in xs if isinstance(x, V)]


def _ovl(a, b):
    return a == b or a.startswith(b + "/") or b.startswith(a + "/")


class Prog:
    def __init__(s, nc, st):
        s.nc = nc
        s.E = {"pe": nc.tensor, "act": nc.scalar, "dve": nc.vector, "pool": nc.gpsimd, "sp": nc.sync}
        s.sems = []
        s.esem = {}
        s.ecnt = {}
        for e in ["pe", "act", "dve", "pool"]:
            s.esem[e] = s._new(st, "s_" + e)
            s.ecnt[e] = 0
        s.dq = {}
        for q in ["sp", "pool"]:
            s.dq[q] = {"sems": [s._new(st, "d_%s%d" % (q, i)) for i in range(NDS)], "use": [0] * NDS, "nxt": 0}
        s.ccsem = s._new(st, "ccs")
        s.cccnt = 0
        s.waited = {e: {} for e in s.E}
        s.bufs = {}
        s.alltok = {}
        s.halt = False

    def _new(s, st, name):
        s.sems.append(st.enter_context(s.nc.semaphore(name)))
        return len(s.sems) - 1

    def _collect(s, reads, writes):
        t = {}

        def add(d):
            for k, v in d.items():
                if t.get(k, 0) < v:
                    t[k] = v
        for k in reads:
            for k2, stt in s.bufs.get(k.split("/")[0], {}).items():
                if _ovl(k, k2):
                    add(stt["w"])
        for k in writes:
            for k2, stt in s.bufs.get(k.split("/")[0], {}).items():
                if _ovl(k, k2):
                    add(stt["w"])
                    add(stt["r"])
        return t

    def _update(s, reads, writes, tok):
        for k in reads:
            stt = s.bufs.setdefault(k.split("/")[0], {}).setdefault(k, {"w": {}, "r": {}})
            if stt["r"].get(tok[0], 0) < tok[1]:
                stt["r"][tok[0]] = tok[1]
        for k in writes:
            d = s.bufs.setdefault(k.split("/")[0], {})
            for k2 in list(d):
                if k2 != k and k2.startswith(k + "/"):
                    del d[k2]
            d[k] = {"w": {tok[0]: tok[1]}, "r": {}}
        if s.alltok.get(tok[0], 0) < tok[1]:
            s.alltok[tok[0]] = tok[1]

    def _wait(s, eng, toks, skip=None):
        e = s.E[eng]
        for sm, v in toks.items():
            if sm == skip:
                continue
            if s.waited[eng].get(sm, 0) < v:
                e.wait_ge(s.sems[sm], v)
                s.waited[eng][sm] = v

    def op(s, eng, fn, reads=(), writes=()):
        if s.halt:
            return
        if eng != "pe":
            pr = [k for k in reads if k.startswith("PS")]
            if pr:
                writes = list(writes) + pr
        toks = s._collect(reads, writes)
        s._wait(eng, toks, skip=s.esem[eng] if eng == "pe" else None)
        ins = fn()
        s.ecnt[eng] += 1
        ins.then_inc(s.sems[s.esem[eng]], 1)
        s._update(reads, writes, (s.esem[eng], s.ecnt[eng]))

    def dma(s, q, fn, reads=(), writes=()):
        if s.halt:
            return
        dq = s.dq[q]
        i = dq["nxt"]
        dq["nxt"] = (i + 1) % NDS
        sm = dq["sems"][i]
        toks = s._collect(reads, writes)
        if dq["use"][i] > 0 and toks.get(sm, 0) < 16 * dq["use"][i]:
            toks[sm] = 16 * dq["use"][i]
        s._wait(q, toks)
        ins = fn()
        dq["use"][i] += 1
        ins.then_inc(s.sems[sm], 16)
        s._update(reads, writes, (sm, 16 * dq["use"][i]))

    def cc(s, kind, op, src, dst):
        if s.halt:
            return
        import os
        if os.environ.get("KNOCC", "") == "1" or (os.environ.get("KNOCC", "") == kind):
            n = min(src.ap.shape[0], dst.ap.shape[0])
            s.ld(dst[0:n, :], src[0:n, :])
            return
        toks = s._collect([src.key], [dst.key])
        s._wait("pool", toks)
        ins = s.nc.gpsimd.collective_compute(kind, op, replica_groups=GROUPS, ins=[src.ap.opt()], outs=[dst.ap.opt()])
        s.cccnt += 1
        ins.then_inc(s.sems[s.ccsem])
        s._update([src.key], [dst.key], (s.ccsem, s.cccnt))

    def barrier(s):
        if s.halt:
            return
        for eng in s.E:
            s._wait(eng, dict(s.alltok), skip=None)

    def mm(s, out, lhsT, rhs, start=True, stop=True):
        s.op("pe", lambda: s.nc.tensor.matmul(_ap(out), _ap(lhsT), _ap(rhs), start=start, stop=stop),
             _keys(lhsT, rhs), _keys(out))

    def tr(s, out, in_, ident):
        s.op("pe", lambda: s.nc.tensor.transpose(_ap(out), _ap(in_), _ap(ident)), _keys(in_, ident), _keys(out))

    def act(s, out, in_, func, bias=None, scale=1.0, accum=None):
        kw = {}
        if bias is not None:
            kw["bias"] = _ap(bias)
        if accum is not None:
            kw["accum_out"] = _ap(accum)
        s.op("act", lambda: s.nc.scalar.activation(out=_ap(out), in_=_ap(in_), func=func, scale=_ap(scale), **kw),
             _keys(in_, bias, scale), _keys(out, accum))

    def tt(s, out, a, b, op, eng="dve"):
        e = s.E[eng]
        s.op(eng, lambda: e.tensor_tensor(out=_ap(out), in0=_ap(a), in1=_ap(b), op=op), _keys(a, b), _keys(out))

    def ts(s, out, a, s1, s2, op0, op1=None, accum=None, eng="dve"):
        e = s.E[eng]
        kw = {}
        if accum is not None:
            kw["accum_out"] = _ap(accum)
        if op1 is None:
            s.op(eng, lambda: e.tensor_scalar(out=_ap(out), in0=_ap(a), scalar1=_ap(s1), scalar2=None, op0=op0, **kw),
                 _keys(a, s1), _keys(out, accum))
        else:
            s.op(eng, lambda: e.tensor_scalar(out=_ap(out), in0=_ap(a), scalar1=_ap(s1), scalar2=_ap(s2), op0=op0, op1=op1, **kw),
                 _keys(a, s1, s2), _keys(out, accum))

    def stt(s, out, a, sc, b, op0, op1, eng="dve"):
        e = s.E[eng]
        s.op(eng, lambda: e.scalar_tensor_tensor(out=_ap(out), in0=_ap(a), scalar=_ap(sc), in1=_ap(b), op0=op0, op1=op1),
             _keys(a, sc, b), _keys(out))

    def cp(s, out, in_, eng="dve"):
        e = s.E[eng]
        s.op(eng, lambda: e.tensor_copy(out=_ap(out), in_=_ap(in_)), _keys(in_), _keys(out))

    def memset(s, out, val, eng="pool"):
        e = s.E[eng]
        s.op(eng, lambda: e.memset(_ap(out), val), [], _keys(out))

    def rmax(s, out, in_):
        s.op("dve", lambda: s.nc.vector.reduce_max(out=_ap(out), in_=_ap(in_), axis=AX.X), _keys(in_), _keys(out))

    def recip(s, out, in_):
        s.op("dve", lambda: s.nc.vector.reciprocal(out=_ap(out), in_=_ap(in_)), _keys(in_), _keys(out))

    def scan(s, out, d0, d1, init, op0, op1):
        s.op("dve", lambda: s.nc.vector.tensor_tensor_scan(out=_ap(out), data0=_ap(d0), data1=_ap(d1), initial=init, op0=op0, op1=op1),
             _keys(d0, d1), _keys(out))

    def ld(s, out, in_, q="sp"):
        e = s.E[q]
        s.dma(q, lambda: e.dma_start(out=_ap(out), in_=_ap(in_)), _keys(in_), _keys(out))

    def gather(s, out, src, idx):
        s.dma("pool", lambda: s.nc.gpsimd.indirect_dma_start(
            out=_ap(out), out_offset=None, in_=_ap(src),
            in_offset=bass.IndirectOffsetOnAxis(ap=_ap(idx), axis=0)), _keys(src, idx), _keys(out))

    def scatter_add(s, dst, src, idx):
        s.dma("pool", lambda: s.nc.gpsimd.indirect_dma_start(
            out=_ap(dst), out_offset=bass.IndirectOffsetOnAxis(ap=_ap(idx), axis=0),
            in_=_ap(src), in_offset=None, compute_op=ALU.add), _keys(src, idx, dst), _keys(dst))


def build(debug=False):
    nc = bass.Bass("TRN2", target_bir_lowering=False)
    top = ExitStack()
    P = Prog(nc, top)

    import os
    KSTOP = os.environ.get("KSTOP", "")
    open_stacks = []

    def stop(tag):
        if KSTOP == tag:
            P.barrier()
            P.halt = True

    def din(name, shape, dt=F32):
        return V(nc.dram_tensor(name, list(shape), dt, kind="ExternalInput").ap(), name)

    def dscr(name, shape, dt=F32):
        return V(nc.dram_tensor(name, list(shape), dt).ap(), name)

    uq = [0]

    def sb(st, name, shape, dt=F32, side=None):
        uq[0] += 1
        name = "%s_%d" % (name, uq[0])
        return V(st.enter_context(nc.sbuf_tensor(name, list(shape), dt, side=side))[:], name)

    def ps(st, name, shape, dt=F32):
        uq[0] += 1
        name = "PS%s_%d" % (name, uq[0])
        return V(st.enter_context(nc.psum_tensor(name, list(shape), dt))[:], name)

    xh = din("xh", [HTOK, D])
    ctxb = din("ctxb", [CTX, D])
    crep = din("crep", [128, KC * 128])
    ccrep = din("ccrep", [128, KC * 128])
    wmod = din("wmod", [D, 6 * D])
    bmodb = din("bmodb", [128, 6 * D])
    g1b = din("g1b", [128, D])
    gp1b = din("gp1b", [128, D])
    g2b = din("g2b", [128, D])
    gp2b = din("gp2b", [128, D])
    winx = din("winx", [D, 5120])
    wout = din("wout", [D, D])
    cosT = din("cosT", [128, HTOK])
    sinT = din("sinT", [128, HTOK])
    masks = din("masks", [8, 128, 5 * 1024])
    lbl = din("lbl", [128, 8])
    hgnb = din("hgnb", [128, 512])
    wr = din("wr", [D, 16])
    mfold = din("mfold", [128, 8])
    wg4 = din("wg4", [4, D, DE])
    wu4 = din("wu4", [4, D, DE])
    wd4 = din("wd4", [4, DE, D])
    selM = din("selM", [64, 16])
    selO = din("selO", [64, 16])
    Gm = din("Gm", [64, 64])
    Gpre = din("Gpre", [64, 64])
    identf = din("identf", [128, 128])
    maskA = din("maskA", [128, 256])
    rowmask = din("rowmask", [128, 4])
    resetm = din("resetm", [128, TOK])
    iota1k = din("iota1k", [128, 1024])
    tokc = din("tokc", [128, 768])
    out = V(nc.dram_tensor("out", [TOK, D], F32, kind="ExternalOutput").ap(), "out")
    if debug:
        dbg_x1 = V(nc.dram_tensor("dbg_x1", [TOK, D], F32, kind="ExternalOutput").ap(), "dbg_x1")
        dbg_mix = V(nc.dram_tensor("dbg_mix", [TOK, D], F32, kind="ExternalOutput").ap(), "dbg_mix")
        dbg_aff = V(nc.dram_tensor("dbg_aff", [16, TOK], F32, kind="ExternalOutput").ap(), "dbg_aff")
        dbg_idx = V(nc.dram_tensor("dbg_idx", [128, 64], F32, kind="ExternalOutput").ap(), "dbg_idx")

    na_d = dscr("na_d", [TOK, 512], BF16)
    sg_d = dscr("sg_d", [TOK, 512], BF16)
    x1_d = dscr("x1_d", [TOK, D])
    h2_in = dscr("h2_in", [TOK, D], BF16)
    h2_all = dscr("h2_all", [4 * TOK, D], BF16)
    st_in = dscr("st_in", [1032, 128])
    st_all = dscr("st_all", [4 * 1032, 128])
    af_in = dscr("af_in", [16, TOK])
    af_all = dscr("af_all", [64, TOK])
    acc = dscr("acc", [4 * TOK, D])
    rs_out = dscr("rs_out", [TOK, D])
    modsc = dscr("modsc", [4 * 128, D])

    try:
        idf = sb(top, "idf", [128, 128])
        idb = sb(top, "idb", [128, 128], BF16)
        mst = ExitStack()
        cols = sb(top, "cols", [128, 32])
        zero4k = sb(top, "zero4k", [128, D])
        epsc = sb(top, "epsc", [128, 1])
        gt1g = sb(mst, "gt1g", [128, D])
        a2 = sb(mst, "a2", [128, D])
        sh2 = sb(mst, "sh2", [128, D])
        gt2g = sb(mst, "gt2g", [128, D])
        modB = sb(mst, "modB", [128, 6 * D])
        P.memset(epsc, EPS)
        P.ld(idf, identf)
        P.cp(idb, idf)
        P.memset(zero4k, 0.0)
        accv = acc.re("(n p) d -> n p d", p=128)
        for n in range(64):
            P.ld(accv[n], zero4k)

        with ExitStack() as st:
            sc_ = sb(st, "siluc", [128, KC * 128])
            scc = sb(st, "silucc", [128, KC * 128])
            modC = sb(st, "modC", [128, 2 * D])
            wmb = [sb(st, "wmb%d" % i, [128, KC, 512]) for i in range(2)]
            bmb = [sb(st, "bmb%d" % i, [128, 512]) for i in range(2)]
            pm = [ps(st, "pm%d" % i, [128, 512]) for i in range(2)]
            tmpb = sb(st, "tmpb", [128, D])
            ptr = ps(st, "ptr", [128, 128])
            P.ld(sc_, crep)
            P.ld(scc, ccrep)
            P.act(sc_, sc_, AF.Silu)
            P.act(scc, scc, AF.Silu)
            wmv = wmod.re("(kc p) n -> p kc n", p=128)
            for nb in range(12):
                wb = wmb[nb % 2]
                P.ld(wb, wmv[:, :, nb * 512:(nb + 1) * 512])
                P.ld(bmb[nb % 2], bmodb[:, nb * 512:(nb + 1) * 512])
                for kc in range(KC):
                    P.mm(pm[0], sc_[:, kc * 128:(kc + 1) * 128], wb[:, kc, :], start=(kc == 0), stop=(kc == KC - 1))
                P.tt(modB[:, nb * 512:(nb + 1) * 512], pm[0], bmb[nb % 2], ALU.add)
                if nb < 4:
                    for kc in range(KC):
                        P.mm(pm[1], scc[:, kc * 128:(kc + 1) * 128], wb[:, kc, :], start=(kc == 0), stop=(kc == KC - 1))
                    P.tt(modC[:, nb * 512:(nb + 1) * 512], pm[1], bmb[nb % 2], ALU.add)
            g1t = sb(st, "g1t", [128, D])
            P.ld(g1t, g1b)
            for (src_sh, src_sc, c0) in ((modB[:, 0:D], modB[:, D:2 * D], 0), (modC[:, 0:D], modC[:, D:2 * D], 16)):
                P.stt(tmpb, src_sc, 1.0, g1t, ALU.add, ALU.mult)
                for kc in range(KC):
                    P.tr(ptr, tmpb[:, kc * 128:(kc + 1) * 128], idf)
                    P.cp(cols[:, c0 + kc:c0 + kc + 1], ptr[:, 0:1])
                    P.tr(ptr, src_sh[:, kc * 128:(kc + 1) * 128], idf)
                    P.cp(cols[:, c0 + 8 + kc:c0 + 8 + kc + 1], ptr[:, 0:1])
            P.ld(tmpb, gp1b)
            P.tt(gt1g, modB[:, 2 * D:3 * D], tmpb, ALU.mult)
            P.ld(tmpb, g2b)
            P.stt(a2, modB[:, 4 * D:5 * D], 1.0, tmpb, ALU.add, ALU.mult)
            P.cp(sh2, modB[:, 3 * D:4 * D])
            P.ld(tmpb, gp2b)
            P.tt(gt2g, modB[:, 5 * D:6 * D], tmpb, ALU.mult)
            for q_, t_ in enumerate((gt1g, a2, sh2, gt2g)):
                P.ld(modsc[q_ * 128:(q_ + 1) * 128, :], t_)
        P.barrier()
        mst.close()
        stop("a")

        s1 = ExitStack()
        open_stacks.append(s1)
        hT = sb(s1, "hT", [128, KC, HTOK], BF16, side="right")
        hcT = sb(s1, "hcT", [128, KC, CTX], BF16, side="right")
        with ExitStack() as st:
            xt = [sb(st, "xt%d" % i, [128, D]) for i in range(2)]
            xs = [sb(st, "xs%d" % i, [128, D], BF16) for i in range(2)]
            junk = sb(st, "junk", [128, D])
            ss = sb(st, "ss", [128, 2])
            pt = [ps(st, "pt%d" % i, [128, D], BF16) for i in range(2)]
            xhv = xh.re("(n p) d -> n p d", p=128)
            cxv = ctxb.re("(n p) d -> n p d", p=128)
            for i in range(NTH + 2):
                isctx = i >= NTH
                src = cxv[i - NTH] if isctx else xhv[i]
                x_ = xt[i % 2]
                P.ld(x_, src)
                P.act(junk, x_, AF.Square, accum=ss[:, 0:1])
                P.act(ss[:, 1:2], ss[:, 0:1], AF.Sqrt, bias=epsc[:, 0:1], scale=1.0 / D)
                P.recip(ss[:, 1:2], ss[:, 1:2])
                P.ts(xs[i % 2], x_, ss[:, 1:2], None, ALU.mult)
                for kc in range(KC):
                    P.tr(pt[i % 2][:, kc * 128:(kc + 1) * 128], xs[i % 2][:, kc * 128:(kc + 1) * 128], idb)
                c0 = 16 if isctx else 0
                for kc in range(KC):
                    dst = hcT[:, kc, (i - NTH) * 128:(i - NTH + 1) * 128] if isctx else hT[:, kc, i * 128:(i + 1) * 128]
                    if kc % 2 == 0:
                        P.act(dst, pt[i % 2][:, kc * 128:(kc + 1) * 128], AF.Identity,
                              bias=cols[:, c0 + 8 + kc:c0 + 9 + kc], scale=cols[:, c0 + kc:c0 + kc + 1])
                    else:
                        P.ts(dst, pt[i % 2][:, kc * 128:(kc + 1) * 128], cols[:, c0 + kc:c0 + kc + 1],
                             cols[:, c0 + 8 + kc:c0 + 9 + kc], ALU.mult, ALU.add)
        P.barrier()
        stop("b")

        winv = winx.re("(kc p) n -> p kc n", p=128)

        def load_w(st_w, wst, wbf, blk, ncols=512, col0=None):
            c0 = blk * 512 if col0 is None else col0
            for kc in range(KC):
                P.ld(wst[kc % 2][:, 0:ncols], winv[:, kc, c0:c0 + ncols])
                P.cp(wbf[:, kc, 0:ncols], wst[kc % 2][:, 0:ncols], eng="pool")

        with ExitStack() as st:
            na_tok = sb(st, "na_tok", [128, NTO, 512], BF16)
            for half in range(2):
                with ExitStack() as sth:
                    qT = sb(sth, "qT", [128, 2, TOK], BF16)
                    qrT = sb(sth, "qrT", [128, 2, TOK], BF16)
                    krT = sb(sth, "krT", [128, 2, HTOK], BF16)
                    vtk = sb(sth, "vtk", [128, NTH, 256], BF16)
                    kcT = sb(sth, "kcT", [128, 2, CTX], BF16)
                    vck = sb(sth, "vck", [128, 2, 256], BF16)
                    with ExitStack() as st2:
                        wst = [sb(st2, "wst%d" % i, [128, 512]) for i in range(2)]
                        wAs = [sb(st2, "wA%d" % i_, [128, KC, 256], BF16) for i_ in range(2)]
                        wBs = [sb(st2, "wB%d" % i_, [128, KC, 256], BF16) for i_ in range(2)]
                        cs = sb(st2, "cs", [128, HTOK])
                        sn = sb(st2, "sn", [128, HTOK])
                        t1 = sb(st2, "t1", [128, 512])
                        t2 = sb(st2, "t2", [128, 512])
                        pa = [ps(st2, "pa%d" % i, [128, 512]) for i in range(2)]
                        pb = [ps(st2, "pb%d" % i, [128, 512]) for i in range(2)]
                        P.ld(cs, cosT)
                        P.ld(sn, sinT)
                        for (blk, pblk, ntok, tok0, dstp, dstr) in ((0, 8, TOK, OWN0 * 128, qT, qrT), (1, 9, HTOK, 0, None, krT)):
                            wA, wB = wAs[blk], wBs[blk]
                            load_w(st2, wst, wA, blk, 256, blk * 512 + half * 256)
                            load_w(st2, wst, wB, pblk, 256, pblk * 512 + half * 256)
                            if half == 0 and blk == 0:
                                stop("c1a")
                            it = 0
                            for hp in range(2):
                                for nb in range(ntok // 512):
                                    a_, b_ = pa[it % 2], pb[it % 2]
                                    it += 1
                                    tk = tok0 + nb * 512
                                    for kc in range(KC):
                                        P.mm(a_, wA[:, kc, hp * 128:(hp + 1) * 128], hT[:, kc, tk:tk + 512], start=(kc == 0), stop=(kc == KC - 1))
                                    for kc in range(KC):
                                        P.mm(b_, wB[:, kc, hp * 128:(hp + 1) * 128], hT[:, kc, tk:tk + 512], start=(kc == 0), stop=(kc == KC - 1))
                                    KV = os.environ.get("KVAR", "")
                                    if dstp is not None and KV not in ("1", "3"):
                                        P.act(dstp[:, hp, nb * 512:(nb + 1) * 512], a_, AF.Copy)
                                    if KV not in ("2", "3"):
                                        P.tt(t1, a_, cs[:, tk:tk + 512], ALU.mult)
                                        P.tt(t2, b_, sn[:, tk:tk + 512], ALU.mult)
                                        P.tt(dstr[:, hp, nb * 512:(nb + 1) * 512], t1, t2, ALU.add)
                            if half == 0 and blk == 0:
                                stop("c1b")
                            if half == 0 and blk == 1:
                                stop("c1c")
                        for hp in range(2):
                            for kc in range(KC):
                                P.mm(pa[0][:, 0:CTX], wA[:, kc, hp * 128:(hp + 1) * 128], hcT[:, kc, :], start=(kc == 0), stop=(kc == KC - 1))
                            P.cp(kcT[:, hp, :], pa[0][:, 0:CTX])
                        wA = wAs[0]
                        load_w(st2, wst, wA, 2, 256, 2 * 512 + half * 256)
                        for i in range(NTH + 2):
                            a_ = pa[i % 2]
                            for kc in range(KC):
                                lhs = hcT[:, kc, (i - NTH) * 128:(i - NTH + 1) * 128] if i >= NTH else hT[:, kc, i * 128:(i + 1) * 128]
                                P.mm(a_[:, 0:256], lhs, wA[:, kc, 0:256], start=(kc == 0), stop=(kc == KC - 1))
                            dst = vck[:, i - NTH, :] if i >= NTH else vtk[:, i, :]
                            if i % 2 == 0:
                                P.act(dst, a_[:, 0:256], AF.Copy)
                            else:
                                P.cp(dst, a_[:, 0:256])
                    P.barrier()
                    if half == 0:
                        stop("c1")
                    with ExitStack() as st2:
                        mk = [sb(st2, "mk%d" % i, [128, 5, 1024]) for i in range(2)]
                        scb = [sb(st2, "scb%d" % i, [128, 1024]) for i in range(2)]
                        pb_ = [sb(st2, "pbf%d" % i, [128, 1024], BF16) for i in range(2)]
                        ptb = [sb(st2, "ptb%d" % i, [128, 8, 128], BF16) for i in range(2)]
                        sm = sb(st2, "smx", [128, 8])
                        psS = [ps(st2, "psS%d" % i, [128, 1024]) for i in range(2)]
                        psT = [ps(st2, "psT%d" % i, [128, 1024], BF16) for i in range(2)]
                        psO = [ps(st2, "psO%d" % i, [128, 64]) for i in range(2)]
                        smd = [sb(st2, "smd%d" % i, [128, 8]) for i in range(2)]

                        def geom(rp):
                            cls = 0 if rp == 0 else 1 if rp == 1 else 3 if rp == 14 else 4 if rp == 15 else 2
                            brow = 0 if rp <= 1 else 28 if rp >= 14 else 2 * rp
                            return cls, brow * 64

                        def stA1(hl, rp, j):
                            hp, hf = hl // 2, (hl % 2) * 64
                            mkh = mk[hl % 2]
                            cls, k0 = geom(rp)
                            S_ = psS[j]
                            sm_ = smd[j]
                            qsl = slice(rp * 128, (rp + 1) * 128)
                            P.mm(S_[:, 0:256], qT[hf:hf + 64, hp, qsl], kcT[hf:hf + 64, hp, :])
                            P.mm(S_[:, 256:512], qrT[hf:hf + 64, hp, qsl], krT[hf:hf + 64, hp, k0:k0 + 256])
                            P.mm(S_[:, 512:1024], qrT[hf:hf + 64, hp, qsl], krT[hf:hf + 64, hp, k0 + 256:k0 + 768])
                            P.stt(scb[j], S_, 0.125, mkh[:, cls, :], ALU.mult, ALU.add)
                            P.rmax(sm_[:, 0:1], scb[j])
                            P.ts(sm_[:, 1:2], sm_[:, 0:1], -1.0, None, ALU.mult)

                        def stA2(hl, rp, j):
                            sm_ = smd[j]
                            P.act(pb_[j], scb[j], AF.Exp, bias=sm_[:, 1:2], accum=sm_[:, 2:3])

                        def stB1(hl, rp, j):
                            for c in range(8):
                                P.tr(psT[j][:, c * 128:(c + 1) * 128], pb_[j][:, c * 128:(c + 1) * 128], idb)
                            P.act(ptb[j].re("p c n -> p (c n)"), psT[j], AF.Copy)

                        def stB2(hl, rp, j):
                            h = half * 4 + hl
                            cls, k0 = geom(rp)
                            sm_ = smd[j]
                            t0 = k0 // 128
                            for c in range(8):
                                rhs = vck[:, c, hl * 64:(hl + 1) * 64] if c < 2 else vtk[:, t0 + c - 2, hl * 64:(hl + 1) * 64]
                                P.mm(psO[j], ptb[j][:, c, :], rhs, start=(c == 0), stop=(c == 7))
                            P.recip(sm_[:, 3:4], sm_[:, 2:3])
                            P.ts(na_tok[:, rp, h * 64:(h + 1) * 64], psO[j], sm_[:, 3:4], None, ALU.mult)

                        seq = [(hl, rp) for hl in range(4) for rp in range(NTO)]
                        for n_ in range(len(seq) + 1):
                            nxt = seq[n_] if n_ < len(seq) else None
                            cur = seq[n_ - 1] if n_ >= 1 else None
                            if nxt is not None:
                                if nxt[1] == 0:
                                    P.ld(mk[nxt[0] % 2].re("p c n -> p (c n)"), masks[half * 4 + nxt[0]])
                                stA1(nxt[0], nxt[1], n_ % 2)
                            if cur is not None:
                                stB1(cur[0], cur[1], (n_ - 1) % 2)
                            if nxt is not None:
                                stA2(nxt[0], nxt[1], n_ % 2)
                            if cur is not None:
                                stB2(cur[0], cur[1], (n_ - 1) % 2)
                    P.barrier()
            for i_ in range(NTO):
                P.ld(na_d[i_ * 128:(i_ + 1) * 128, :], na_tok[:, i_, :])
        P.barrier()
        stop("c")
        hg = ExitStack()
        open_stacks.append(hg)
        o0 = sb(hg, "o0", [128, NTO, 512], BF16)
        qseg = sb(hg, "qseg", [128, 8, TOK], BF16)
        Send = sb(hg, "Send", [128, 8, 128])
        Sctx = sb(hg, "Sctx", [128, 8, 128])
        Dtot = sb(hg, "Dtot", [128, 8])
        lbt = sb(hg, "lbt", [128, 16])
        lbn = sb(hg, "lbn", [128, 4])
        ones64 = sb(hg, "ones64", [128, 64])
        rmk = sb(hg, "rmk", [128, 4])
        mAt = sb(hg, "mAt", [128, 256])
        mAb = sb(hg, "mAb", [128, 256], BF16)
        P.ld(lbt[:, 0:8], lbl)
        P.ld(rmk, rowmask)
        P.ld(mAt, maskA)
        P.cp(mAb, mAt)
        P.memset(ones64, 1.0)
        P.tt(lbt[:, 8:12], lbt[:, 0:4], lbt[:, 4:8], ALU.subtract)
        P.act(lbt[:, 12:16], lbt[:, 8:12], AF.Sigmoid, scale=-1.0)
        P.act(lbt[:, 8:12], lbt[:, 8:12], AF.Sigmoid)
        P.ts(lbn, lbt[:, 12:16], -1.0, None, ALU.mult)
        with ExitStack() as st:
            ih = sb(st, "ih", [128, NTO, 512], BF16)
            ihc = sb(st, "ihc", [128, 2, 512], BF16)
            wst = [sb(st, "wst%d" % i, [128, 512]) for i in range(2)]
            rsm = sb(st, "rsm", [128, TOK])
            P.ld(rsm, resetm)
            hTo = hT[:, :, OWN0 * 128:OWN0 * 128 + TOK]
            with ExitStack() as st2:
                pa = [ps(st2, "pa%d" % i, [128, 512]) for i in range(2)]
                sgt = [sb(st2, "sgt%d" % i, [128, 512], BF16) for i in range(2)]
                wA = sb(st2, "wA", [128, KC, 512], BF16)
                for (blk, dstt, dstc, fn) in ((6, ih, ihc, AF.Copy), (7, None, None, AF.Sigmoid)):
                    load_w(st2, wst, wA, blk)
                    for i in range(NTO + (2 if dstc is not None else 0)):
                        a_ = pa[i % 2]
                        for kc in range(KC):
                            lhs = hcT[:, kc, (i - NTO) * 128:(i - NTO + 1) * 128] if i >= NTO else hTo[:, kc, i * 128:(i + 1) * 128]
                            P.mm(a_, lhs, wA[:, kc, :], start=(kc == 0), stop=(kc == KC - 1))
                        if dstt is None:
                            P.act(sgt[i % 2], a_, fn)
                            P.ld(sg_d[i * 128:(i + 1) * 128, :], sgt[i % 2])
                        else:
                            P.act(dstc[:, i - NTO, :] if i >= NTO else dstt[:, i, :], a_, fn)
            P.barrier()
            with ExitStack() as st2:
                wzs = [sb(st2, "wz%d" % i_, [128, KC, 128], BF16) for i_ in range(2)]
                sg_ = sb(st2, "s_", [128, TOK])
                binc = sb(st2, "binc", [128, TOK])
                qhh = sb(st2, "qhh", [128, TOK], BF16)
                wqs = [sb(st2, "wq%d" % i_, [128, KC, 128], BF16) for i_ in range(2)]
                kk = sb(st2, "kk", [128, TOK], BF16)
                kdec = sb(st2, "kdec", [128, TOK], BF16)
                kend = sb(st2, "kend", [128, TOK], BF16)
                qdec = sb(st2, "qdec", [128, TOK], BF16)
                sm = sb(st2, "hsm", [128, 4, 64])
                S = sb(st2, "S", [128, 128])
                tot = sb(st2, "tot", [128, 64])
                Sx = sb(st2, "Sx", [128, 128])
                Sb = [sb(st2, "Sb%d" % i, [128, 4, 128], BF16) for i in range(2)]
                QM = [sb(st2, "QM%d" % i, [128, 640], BF16) for i in range(2)]
                kendT = [sb(st2, "kendT%d" % i, [128, 128], BF16) for i in range(2)]
                vm = [sb(st2, "vm%d" % i, [128, 4, 128], BF16) for i in range(2)]
                Am = [sb(st2, "Am%d" % i, [128, 128], BF16) for i in range(2)]
                pz = [ps(st2, "pz%d" % i, [128, 512]) for i in range(2)]
                pT = ps(st2, "pT", [128, 1024], BF16)
                pKV = [ps(st2, "pKV%d" % i, [128, 4, 128]) for i in range(2)]
                pA = ps(st2, "pA", [128, 128])
                pO = [ps(st2, "pO%d" % i, [128, 128]) for i in range(2)]
                P.memset(QM[0], 0.0)
                P.memset(QM[1], 0.0)
                from functools import partial as F_
                from itertools import zip_longest
                kdecs = [kdec, sb(st2, "kdec2", [128, TOK], BF16)]
                kends = [kend, sb(st2, "kend2", [128, TOK], BF16)]
                qdecs = [qdec, sb(st2, "qdec2", [128, TOK], BF16)]
                sms = [sm, sb(st2, "hsm2", [128, 4, 64])]
                jobs = [(ic, d, h) for ic in (True, False) for d in range(2) for h in range(4)]

                def Xops(job, par):
                    isctx, d, h = job
                    ops = []
                    A = ops.append
                    ntok = CTX if isctx else TOK
                    nch = ntok // 32
                    hsrc = hcT if isctx else hTo
                    k = d * 4 + h
                    kdec_, kend_, qdec_, sm_ = kdecs[par], kends[par], qdecs[par], sms[par]
                    wz, wq = wzs[k % 2], wqs[k % 2]
                    A(F_(load_w, st2, wst, wz, None, 128, (4 + d) * 512 + h * 128))
                    nbs = [(0, 256)] if isctx else [(i * 512, 512) for i in range(4)]
                    for bi, (c0, cn) in enumerate(nbs):
                        a_ = pz[bi % 2]
                        for kc in range(KC):
                            A(F_(P.mm, a_[:, 0:cn], wz[:, kc, :], hsrc[:, kc, c0:c0 + cn], start=(kc == 0), stop=(kc == KC - 1)))
                        A(F_(P.act, sg_[:, c0:c0 + cn], a_[:, 0:cn], AF.Sigmoid))
                    sv = sg_[:, 0:ntok]
                    A(F_(P.ts, kk[:, 0:ntok], sv, lbn[:, h:h + 1], lbt[:, 12 + h:13 + h], ALU.mult, ALU.add))
                    A(F_(P.act, sv, sv, AF.Ln, bias=lbt[:, 8 + h:9 + h], scale=lbt[:, 12 + h:13 + h]))
                    A(F_(P.scan, binc[:, 0:ntok], rsm[:, 0:ntok], sv, 0.0, ALU.mult, ALU.add))
                    b3 = binc[:, 0:ntok].re("p (c t) -> p c t", t=32)
                    A(F_(P.cp, tot[:, 0:nch], b3[:, :, 31]))
                    bend = tot[:, 0:nch]
                    B = binc[:, 0:ntok]
                    if d == 1:
                        A(F_(P.tt, B, sv, B, ALU.subtract))
                        A(F_(P.tt, b3, b3, bend.re("p (c o) -> p c o", o=1).bc([128, nch, 32]), ALU.add))
                    Ee = sg_
                    A(F_(P.act, sm_[:, 2, 0:nch], bend, AF.Exp))
                    A(F_(P.scan, sm_[:, 0, 0:nch], ones64[:, 0:nch], bend, 0.0, ALU.mult, ALU.add))
                    if d == 0:
                        A(F_(P.tt, sm_[:, 1, 0:nch], sm_[:, 0, 0:nch], bend, ALU.subtract))
                    else:
                        A(F_(P.ts, sm_[:, 1, 0:nch], sm_[:, 0, 0:nch], -1.0, sm_[:, 0, nch - 1:nch], ALU.mult, ALU.add))
                    A(F_(P.act, sm_[:, 3, 0:nch], sm_[:, 1, 0:nch], AF.Exp))
                    if not isctx:
                        A(F_(P.act, Dtot[:, k:k + 1], sm_[:, 0, nch - 1:nch], AF.Exp))
                        A(F_(P.act, Ee[:, 0:ntok], B, AF.Exp))
                        A(F_(load_w, st2, wst, wq, None, 128, 3 * 512 + h * 128))
                        for nb in range(4):
                            a_ = pz[nb % 2]
                            for kc in range(KC):
                                A(F_(P.mm, a_, wq[:, kc, :], hTo[:, kc, nb * 512:(nb + 1) * 512], start=(kc == 0), stop=(kc == KC - 1)))
                            A(F_(P.cp, qhh[:, nb * 512:(nb + 1) * 512], a_))
                        A(F_(P.tt, qdec_[:, 0:ntok], qhh, Ee[:, 0:ntok], ALU.mult))
                        A(F_(P.tt, qseg[:, k, :].re("p (c t) -> p c t", t=32), qdec_.re("p (c t) -> p c t", t=32),
                             sm_[:, 3, 0:nch].re("p (c o) -> p c o", o=1).bc([128, nch, 32]), ALU.mult))
                    A(F_(P.act, Ee[:, 0:ntok], B, AF.Exp, scale=-1.0))
                    A(F_(P.tt, kdec_[:, 0:ntok], kk[:, 0:ntok], Ee[:, 0:ntok], ALU.mult))
                    A(F_(P.tt, kend_[:, 0:ntok].re("p (c t) -> p c t", t=32), kdec_[:, 0:ntok].re("p (c t) -> p c t", t=32),
                         sm_[:, 2, 0:nch].re("p (c o) -> p c o", o=1).bc([128, nch, 32]), ALU.mult))
                    return ops

                def Tops(job, par):
                    isctx, d, h = job
                    ops = []
                    A = ops.append
                    ntok = CTX if isctx else TOK
                    nt = ntok // 128
                    vsrc = ihc if isctx else ih
                    k = d * 4 + h
                    kdec_, kend_, qdec_, sm_ = kdecs[par], kends[par], qdecs[par], sms[par]
                    S2 = [S, Sx]
                    cix = [0]
                    A(F_(P.memset, S, 0.0, eng="dve"))
                    tiles = list(range(nt)) if d == 0 else list(range(nt - 1, -1, -1))
                    chs = [0, 1, 2, 3] if d == 0 else [3, 2, 1, 0]
                    for ti, i in enumerate(tiles):
                        j2 = ti % 2
                        tsl = slice(i * 128, (i + 1) * 128)
                        hc = slice(h * 128, (h + 1) * 128)
                        A(F_(P.tr, pT[:, 0:128], kend_[:, tsl], idb))
                        A(F_(P.act, kendT[j2], pT[:, 0:128], AF.Copy))
                        A(F_(P.tt, vm[j2], vsrc[:, i, hc].re("p (o v) -> p o v", o=1).bc([128, 4, 128]),
                             rmk.re("p (j o) -> p j o", o=1).bc([128, 4, 128]), ALU.mult))
                        for j in range(4):
                            A(F_(P.mm, pKV[j2][:, j, :], kendT[j2], vm[j2][:, j, :]))
                        if not isctx:
                            A(F_(P.mm, pA, kdec_[:, tsl], qdec_[:, tsl]))
                            A(F_(P.tt, Am[j2], pA, mAb[:, d * 128:(d + 1) * 128], ALU.mult))
                            A(F_(P.act, QM[j2].re("p (j x) -> p j x", x=160)[:, :, 0:32],
                                 qdec_[:, tsl].re("p (j t) -> p j t", t=32), AF.Copy))
                        for j in chs:
                            cur_, nxt_ = S2[cix[0] % 2], S2[(cix[0] + 1) % 2]
                            cix[0] += 1
                            if not isctx:
                                A(F_(P.act, Sb[j2][:, j, :], cur_, AF.Copy))
                            A(F_(P.stt, nxt_, cur_, sm_[:, 2, i * 4 + j:i * 4 + j + 1], pKV[j2][:, j, :], ALU.mult, ALU.add))
                        if not isctx:
                            A(F_(P.mm, pO[j2], Am[j2], vsrc[:, i, hc], start=True, stop=False))
                            for j in range(4):
                                A(F_(P.mm, pO[j2], QM[j2][:, j * 128:(j + 1) * 128], Sb[j2][:, j, :], start=False, stop=(j == 3)))
                            if d == 0:
                                A(F_(P.cp, o0[:, i, hc], pO[j2]))
                            else:
                                A(F_(P.tt, o0[:, i, hc], o0[:, i, hc], pO[j2], ALU.add))
                    A(F_(P.cp, (Sctx if isctx else Send)[:, k, :], S2[cix[0] % 2]))
                    return ops

                for o_ in Xops(jobs[0], 0):
                    o_()
                for n_ in range(len(jobs)):
                    ta = Tops(jobs[n_], n_ % 2)
                    xb = Xops(jobs[n_ + 1], (n_ + 1) % 2) if n_ + 1 < len(jobs) else []
                    for x_, y_ in zip_longest(ta, xb):
                        if x_ is not None:
                            x_()
                        if y_ is not None:
                            y_()
        P.barrier()
        stop("d")
        s1.close()
        open_stacks.remove(s1)
        Ssb = sb(hg, "Ssb", [128, 8, 128], BF16)
        with ExitStack() as st:
            pD = ps(st, "pD", [8, 128])
            dT = sb(st, "dT", [8, 128])
            mf = sb(st, "mf", [128, 8])
            U = [sb(st, "U%d" % i, [128, 128]) for i in range(2)]
            Dj = [sb(st, "Dj%d" % i, [128, 4]) for i in range(2)]
            P.ld(mf, mfold)
            P.tr(pD, Dtot, idf)
            P.cp(dT, pD)
            for k in range(8):
                P.ld(st_in[k * 128:(k + 1) * 128, :], Send[:, k, :])
            P.ld(st_in[1024:1032, :], dT)
            P.cc("AllGather", ALU.bypass, st_in, st_all)
            it = 0
            for k in range(8):
                d = k // 4
                Sk = Sctx[:, k, :]
                for j in ([0, 1, 2, 3] if d == 0 else [3, 2, 1, 0]):
                    u_, d_ = U[it % 2], Dj[it % 2]
                    it += 1
                    P.ld(u_, st_all[j * 1032 + k * 128:j * 1032 + (k + 1) * 128, :])
                    P.ld(d_[:, 0:1], st_all[j * 1032 + 1024 + k:j * 1032 + 1025 + k, :].re("o d -> d o"))
                    m_ = mf[:, d * 4 + j:d * 4 + j + 1]
                    P.ts(d_[:, 1:2], d_[:, 0:1], -1.0, m_, ALU.add, ALU.mult)
                    P.ts(d_[:, 1:2], d_[:, 1:2], 1.0, None, ALU.add)
                    P.ts(u_, u_, m_, None, ALU.mult)
                    P.stt(Sk, Sk, d_[:, 1:2], u_, ALU.mult, ALU.add)
                P.cp(Ssb[:, k, :], Sk)
        P.barrier()
        stop("e")
        with ExitStack() as st:
            woutb = sb(st, "woutb", [128, KC, D], BF16)
            gt1g = sb(st, "gt1g", [128, D])
            a2 = sb(st, "a2", [128, D])
            sh2 = sb(st, "sh2", [128, D])
            for q_, t_ in enumerate((gt1g, a2, sh2)):
                P.ld(t_, modsc[q_ * 128:(q_ + 1) * 128, :])
            wst2 = [sb(st, "wst2%d" % i, [128, D]) for i in range(2)]
            wrs = sb(st, "wrs", [128, KC, 16])
            hgn = sb(st, "hgn", [128, 512])
            affT = sb(st, "affT", [16, TOK])
            ot = sb(st, "ot", [128, 512])
            hgt = sb(st, "hgt", [128, 512], BF16)
            nat = [sb(st, "nat%d" % i, [128, 512], BF16) for i in range(2)]
            sgl = [sb(st, "sgl%d" % i, [128, 512], BF16) for i in range(2)]
            xt = [sb(st, "xt%d" % i, [128, D]) for i in range(2)]
            x1t = [sb(st, "x1t%d" % i, [128, D]) for i in range(2)]
            h2t = [sb(st, "h2t%d" % i, [128, D]) for i in range(2)]
            h2b = [sb(st, "h2b%d" % i, [128, D], BF16) for i in range(2)]
            mixT = sb(st, "mixT", [128, KC, 128], BF16)
            h2T = sb(st, "h2T", [128, KC, 128])
            junk = sb(st, "junk2", [128, D])
            sm = sb(st, "rsm2", [128, 16])
            lg = sb(st, "lg", [128, 16])
            psC = ps(st, "psC", [128, 512])
            psT = ps(st, "psT", [128, 1024], BF16)
            psM = ps(st, "psM", [128, 1024])
            psT32 = ps(st, "psT32", [128, 1024])
            psL = ps(st, "psL", [128, 128])
            woutv = wout.re("(kc p) n -> p kc n", p=128)
            for kc in range(KC):
                P.ld(wst2[kc % 2], woutv[:, kc, :])
                P.cp(woutb[:, kc, :], wst2[kc % 2], eng="pool")
            P.ld(wrs, wr.re("(kc p) n -> p kc n", p=128))
            P.ld(hgn, hgnb)
            xhv = xh.re("(n p) d -> n p d", p=128)
            from functools import partial as F_
            from itertools import zip_longest
            smA = [sb(st, "smA%d" % i, [128, 8]) for i in range(2)]
            smB = [sb(st, "smB%d" % i, [128, 8]) for i in range(2)]
            smC = [sb(st, "smC%d" % i, [128, 8]) for i in range(2)]
            junkA = sb(st, "junkA", [128, 128])

            def S1(i):
                ops = []
                A = ops.append
                j2 = i % 2
                tsl = slice(i * 128, (i + 1) * 128)
                sm_ = smA[j2]
                A(F_(P.ld, nat[j2], na_d[tsl, :]))
                A(F_(P.ld, sgl[j2], sg_d[tsl, :]))
                A(F_(P.ld, xt[j2], xhv[i + OWN0]))
                for h in range(4):
                    A(F_(P.mm, psC[:, h * 128:(h + 1) * 128], qseg[:, h, tsl], Ssb[:, h, :], start=True, stop=False))
                    A(F_(P.mm, psC[:, h * 128:(h + 1) * 128], qseg[:, 4 + h, tsl], Ssb[:, 4 + h, :], start=False, stop=True))
                A(F_(P.tt, ot, o0[:, i, :], psC, ALU.add))
                for h in range(4):
                    A(F_(P.act, junkA, ot[:, h * 128:(h + 1) * 128], AF.Square, accum=sm_[:, h:h + 1]))
                A(F_(P.act, sm_[:, 4:8], sm_[:, 0:4], AF.Sqrt, bias=epsc[:, 0:1], scale=1.0 / 128))
                A(F_(P.recip, sm_[:, 4:8], sm_[:, 4:8]))
                o3 = ot.re("p (h v) -> p h v", v=128)
                A(F_(P.tt, o3, o3, sm_[:, 4:8].re("p (h o) -> p h o", o=1).bc([128, 4, 128]), ALU.mult))
                A(F_(P.tt, ot, ot, hgn, ALU.mult))
                A(F_(P.tt, hgt, ot, sgl[j2], ALU.mult))
                for c in range(4):
                    A(F_(P.tr, psT[:, c * 128:(c + 1) * 128], nat[j2][:, c * 128:(c + 1) * 128], idb))
                    A(F_(P.tr, psT[:, (4 + c) * 128:(5 + c) * 128], hgt[:, c * 128:(c + 1) * 128], idb))
                A(F_(P.act, mixT.re("p c n -> p (c n)"), psT, AF.Copy))
                for nb in range(2):
                    for mc in range(KC):
                        A(F_(P.mm, psM[:, nb * 512:(nb + 1) * 512], mixT[:, mc, :], woutb[:, mc, nb * 512:(nb + 1) * 512], start=(mc == 0), stop=(mc == KC - 1)))
                return ops

            def S2(i):
                ops = []
                A = ops.append
                j2 = i % 2
                tsl = slice(i * 128, (i + 1) * 128)
                sm_ = smB[j2]
                A(F_(P.act, junk, psM, AF.Square, accum=sm_[:, 0:1]))
                A(F_(P.act, sm_[:, 1:2], sm_[:, 0:1], AF.Sqrt, bias=epsc[:, 0:1], scale=1.0 / D))
                A(F_(P.recip, sm_[:, 1:2], sm_[:, 1:2]))
                A(F_(P.stt, x1t[j2], psM, sm_[:, 1:2], gt1g, ALU.mult, ALU.mult))
                A(F_(P.tt, x1t[j2], x1t[j2], xt[j2], ALU.add))
                A(F_(P.ld, x1_d[tsl, :], x1t[j2]))
                if debug:
                    A(F_(P.ld, dbg_x1[tsl, :], x1t[j2]))
                A(F_(P.act, junk, x1t[j2], AF.Square, accum=sm_[:, 2:3]))
                A(F_(P.act, sm_[:, 3:4], sm_[:, 2:3], AF.Sqrt, bias=epsc[:, 0:1], scale=1.0 / D))
                A(F_(P.recip, sm_[:, 3:4], sm_[:, 3:4]))
                A(F_(P.stt, h2t[j2], x1t[j2], sm_[:, 3:4], a2, ALU.mult, ALU.mult))
                A(F_(P.tt, h2t[j2], h2t[j2], sh2, ALU.add))
                A(F_(P.act, h2b[j2], h2t[j2], AF.Copy))
                A(F_(P.ld, h2_in[tsl, :], h2b[j2]))
                sm_ = smC[j2]
                for kc in range(KC):
                    A(F_(P.tr, psT32[:, kc * 128:(kc + 1) * 128], h2t[j2][:, kc * 128:(kc + 1) * 128], idf))
                A(F_(P.cp, h2T.re("p c n -> p (c n)"), psT32))
                for kc in range(KC):
                    A(F_(P.mm, psL[:, 0:16], h2T[:, kc, :], wrs[:, kc, :], start=(kc == 0), stop=(kc == KC - 1)))
                A(F_(P.rmax, sm_[:, 0:1], psL[:, 0:16]))
                A(F_(P.ts, sm_[:, 1:2], sm_[:, 0:1], -1.0, None, ALU.mult))
                A(F_(P.act, lg, psL[:, 0:16], AF.Exp, bias=sm_[:, 1:2], accum=sm_[:, 2:3]))
                A(F_(P.recip, sm_[:, 3:4], sm_[:, 2:3]))
                A(F_(P.ts, lg, lg, sm_[:, 3:4], None, ALU.mult))
                A(F_(P.tr, psL[0:16, :], lg, idf))
                A(F_(P.cp, affT[:, tsl], psL[0:16, :]))
                return ops

            for i in range(NTO + 1):
                oa = S1(i) if i < NTO else []
                ob = S2(i - 1) if i >= 1 else []
                for x_, y_ in zip_longest(oa, ob):
                    if x_ is not None:
                        x_()
                    if y_ is not None:
                        y_()
            P.ld(af_in, affT)
            if debug:
                P.ld(dbg_aff, affT)
        hg.close()
        open_stacks.remove(hg)
        stop("f")
        P.barrier()
        P.cc("AllGather", ALU.bypass, af_in, af_all)
        for ch in range(4):
            P.cc("AllGather", ALU.bypass, h2_in[ch * 512:(ch + 1) * 512, :], h2_all[ch * 2048:(ch + 1) * 2048, :])
        stop("1")
        rt = ExitStack()
        idx_i = sb(rt, "idx_i", [128, 4, 8], I32)
        idx_t = sb(rt, "idx_t", [128, 4, 8], I32)
        gate = sb(rt, "gate", [128, 4, 8])
        with ExitStack() as st:
            A = sb(st, "A", [64, TOK])
            selb = sb(st, "selb", [64, TOK])
            incl = sb(st, "incl", [64, TOK])
            ones = sb(st, "ones", [64, TOK])
            Gs = sb(st, "Gs", [64, 64])
            Gp = sb(st, "Gp", [64, 64])
            sM = sb(st, "sM", [64, 16])
            bs = sb(st, "bs", [64, 8])
            rkM = sb(st, "rkM", [128, 16, 16])
            afM = sb(st, "afM", [128, 16, 16])
            res = sb(st, "res", [128, 256])
            gb = sb(st, "gb", [128, 256], BF16)
            vals0 = sb(st, "vals0", [128, 768])
            VALS = sb(st, "VALS", [128, 256, 8], BF16)
            iot = sb(st, "iot", [128, 1024])
            oh = [sb(st, "oh%d" % i, [128, 1024], BF16) for i in range(2)]
            idxf = sb(st, "idxf", [128, 8, 8])
            tokf = sb(st, "tokf", [128, 8])
            P.ld(A, af_all)
            P.ld(Gs, Gm)
            P.ld(Gp, Gpre)
            P.ld(sM, selM)
            P.ld(iot, iota1k)
            P.ld(vals0, tokc)
            P.memset(ones, 1.0)
            P.memset(bs, 0.0)
            P.memset(bs[:, 1:2], 1.0)
            with ExitStack() as st2:
                psb = ps(st2, "psb", [64, 8])
                psr = [ps(st2, "psr%d" % i, [128, 16]) for i in range(2)]
                lo, hi, mid, cpart, cond, tmp = (bs[:, i:i + 1] for i in range(6))
                for it in range(32):
                    P.tt(mid, lo, hi, ALU.add)
                    P.ts(mid, mid, 0.5, None, ALU.mult)
                    P.ts(selb, A, mid, 0.0, ALU.is_gt, ALU.add, accum=cpart)
                    P.mm(psb[:, 0:1], Gs, cpart)
                    P.ts(cond, psb[:, 0:1], 1024.0, None, ALU.is_ge)
                    P.tt(tmp, mid, lo, ALU.subtract)
                    P.stt(lo, tmp, cond, lo, ALU.mult, ALU.add)
                    P.tt(tmp, hi, mid, ALU.subtract)
                    P.stt(hi, tmp, cond, mid, ALU.mult, ALU.add)
                P.ts(selb, A, lo, None, ALU.is_gt)
                P.scan(incl, ones, selb, 0.0, ALU.mult, ALU.add)
                P.mm(psb[:, 1:2], Gp, incl[:, TOK - 1:TOK])
                P.cp(tmp, psb[:, 1:2])
                P.stt(incl, incl, tmp, selb, ALU.add, ALU.mult)
                P.ts(incl, incl, -1.0, None, ALU.add)
                for j in range(16):
                    P.mm(psr[0], incl[:, j * 128:(j + 1) * 128], sM)
                    P.cp(rkM[:, j, :], psr[0])
                    P.mm(psr[1], A[:, j * 128:(j + 1) * 128], sM)
                    P.cp(afM[:, j, :], psr[1])
            af2 = afM.re("p j c -> p (j c)")
            V3 = VALS
            P.cp(V3[:, :, 0], vals0[:, 0:256])
            P.cp(V3[:, :, 1], vals0[:, 256:512])
            P.cp(gb, af2)
            P.cp(V3[:, :, 2], gb)
            P.tt(res, af2, gb, ALU.subtract)
            P.cp(gb, res)
            P.cp(V3[:, :, 3], gb)
            P.tt(res, res, gb, ALU.subtract)
            P.cp(V3[:, :, 4], res)
            P.cp(V3[:, :, 5], vals0[:, 512:768])
            P.memset(V3[:, :, 6:8], 0.0)
            with ExitStack() as st2:
                pI = [ps(st2, "pI%d" % g, [128, 512]) for g in range(8)]
                n = 0
                for i in range(4):
                    cnt = 0
                    for r in range(4):
                        for j in range(16):
                            col = r * 4 + i
                            o_ = oh[n % 2]
                            P.ts(o_, iot, rkM[:, j, col:col + 1], None, ALU.is_equal)
                            n += 1
                            for g in range(8):
                                P.mm(pI[g][:, 0:8], o_[:, g * 128:(g + 1) * 128], V3[:, j * 16 + col, :], start=(cnt == 0), stop=(cnt == 63))
                            cnt += 1
                    for g in range(8):
                        P.cp(idxf[:, g, :], pI[g][:, 0:8])
                    P.stt(tokf, idxf[:, :, 0], 128.0, idxf[:, :, 1], ALU.mult, ALU.add)
                    P.cp(idx_i[:, i, :], tokf)
                    P.stt(tokf, idxf[:, :, 5], 128.0, idxf[:, :, 1], ALU.mult, ALU.add)
                    P.cp(idx_t[:, i, :], tokf)
                    P.tt(gate[:, i, :], idxf[:, :, 2], idxf[:, :, 3], ALU.add)
                    P.tt(gate[:, i, :], gate[:, i, :], idxf[:, :, 4], ALU.add)
        if debug:
            dbt = sb(rt, "dbt", [128, 64])
            P.cp(dbt[:, 0:32], idx_t.re("p a b -> p (a b)"))
            P.cp(dbt[:, 32:64], gate.re("p a b -> p (a b)"))
            P.ld(dbg_idx, dbt)
        P.barrier()
        stop("r")
        with ExitStack() as st:
            xsT = sb(st, "xsT", [128, KC, 1024], BF16)
            hidT = sb(st, "hidT", [128, NFC, 1024], BF16)
            wdb = sb(st, "wdb", [128, NFC, D], BF16)
            wgb = [sb(st, "wgb%d" % i, [128, KC, 256], BF16) for i in range(2)]
            wub = [sb(st, "wub%d" % i, [128, KC, 256], BF16) for i in range(2)]
            xg = [sb(st, "xg%d" % i, [128, D], BF16) for i in range(2)]
            yt = [sb(st, "yt%d" % i, [128, D]) for i in range(2)]
            sil = [sb(st, "sil%d" % i, [128, 512]) for i in range(2)]
            pT = ps(st, "pTx", [128, 1024], BF16)
            pg = [ps(st, "pg%d" % i, [128, 512]) for i in range(2)]
            pu = [ps(st, "pu%d" % i, [128, 512]) for i in range(2)]
            py = [ps(st, "py%d" % i, [128, 512]) for i in range(2)]
            nq = 0
            for i in range(4):
                for g in range(8):
                    P.gather(xg[g % 2], h2_all, idx_i[:, i, g:g + 1])
                    for kc in range(KC):
                        P.tr(pT[:, kc * 128:(kc + 1) * 128], xg[g % 2][:, kc * 128:(kc + 1) * 128], idb)
                    P.act(xsT[:, :, g * 128:(g + 1) * 128], pT.re("p (c n) -> p c n", n=128), AF.Copy)
                wgv = wg4[i].re("(kc p) f -> p kc f", p=128)
                wuv = wu4[i].re("(kc p) f -> p kc f", p=128)
                for fb in range(11):
                    f0 = fb * 256
                    fn = min(256, DE - f0)
                    gb_, ub_ = wgb[fb % 2], wub[fb % 2]
                    P.ld(gb_[:, :, 0:fn], wgv[:, :, f0:f0 + fn], q="pool")
                    P.ld(ub_[:, :, 0:fn], wuv[:, :, f0:f0 + fn], q="pool")
                    for fc_ in (2 * fb, 2 * fb + 1):
                        m_ = 128 if fc_ < 21 else 64
                        P.ld(wdb[0:m_, fc_, :], wd4[i][fc_ * 128:fc_ * 128 + m_, :], q="pool")
                    for c in range((fn + 127) // 128):
                        fc = fb * 2 + c
                        m = min(128, fn - c * 128)
                        for half in range(2):
                            a_, b_ = pg[nq % 2], pu[nq % 2]
                            s_ = sil[nq % 2]
                            nq += 1
                            for kc in range(KC):
                                P.mm(a_[0:m, :], gb_[:, kc, c * 128:c * 128 + m], xsT[:, kc, half * 512:(half + 1) * 512], start=(kc == 0), stop=(kc == KC - 1))
                            for kc in range(KC):
                                P.mm(b_[0:m, :], ub_[:, kc, c * 128:c * 128 + m], xsT[:, kc, half * 512:(half + 1) * 512], start=(kc == 0), stop=(kc == KC - 1))
                            P.act(s_[0:m, :], a_[0:m, :], AF.Silu)
                            P.tt(hidT[0:m, fc, half * 512:(half + 1) * 512], s_[0:m, :], b_[0:m, :], ALU.mult)
                for ct in range(8):
                    y_ = yt[ct % 2]
                    for nb in range(2):
                        p_ = py[nb]
                        for fc in range(NFC):
                            m = 128 if fc < 21 else 64
                            P.mm(p_, hidT[0:m, fc, ct * 128:(ct + 1) * 128], wdb[0:m, fc, nb * 512:(nb + 1) * 512], start=(fc == 0), stop=(fc == NFC - 1))
                        P.ts(y_[:, nb * 512:(nb + 1) * 512], p_, gate[:, i, ct:ct + 1], None, ALU.mult)
                    P.scatter_add(acc, y_, idx_t[:, i, ct:ct + 1])
        rt.close()
        P.barrier()
        P.cc("ReduceScatter", ALU.add, acc, rs_out)
        with ExitStack() as st:
            mt = [sb(st, "mt%d" % i, [128, D]) for i in range(2)]
            x1l = [sb(st, "x1l%d" % i, [128, D]) for i in range(2)]
            junk = sb(st, "junk3", [128, D])
            gt2g = sb(st, "gt2g", [128, D])
            P.ld(gt2g, modsc[3 * 128:4 * 128, :])
            sm = sb(st, "fsm", [128, 4])
            for i in range(NTO):
                j2 = i % 2
                tsl = slice(i * 128, (i + 1) * 128)
                P.ld(mt[j2], rs_out[tsl, :])
                P.ld(x1l[j2], x1_d[tsl, :])
                P.act(junk, mt[j2], AF.Square, accum=sm[:, 0:1])
                P.act(sm[:, 1:2], sm[:, 0:1], AF.Sqrt, bias=epsc[:, 0:1], scale=1.0 / D)
                P.recip(sm[:, 1:2], sm[:, 1:2])
                P.stt(mt[j2], mt[j2], sm[:, 1:2], gt2g, ALU.mult, ALU.mult)
                P.tt(mt[j2], mt[j2], x1l[j2], ALU.add)
                P.ld(out[tsl, :], mt[j2])
    except _Stop:
        pass
    for stx in reversed(open_stacks):
        stx.close()
    P.barrier()
    top.close()
    return nc


def _consts():
    f = np.float32
    p = np.arange(128)
    cst = {}
    cst["identf"] = np.eye(128, dtype=f)
    sidx, cidx = p[:, None], p[None, :]
    same = (sidx // 32) == (cidx // 32)
    cst["maskA"] = np.concatenate([(same & (cidx >= sidx)), (same & (cidx <= sidx))], axis=1).astype(f)
    cst["rowmask"] = ((p[:, None] // 32) == np.arange(4)[None, :]).astype(f)
    rm = np.ones((128, TOK), f)
    rm[:, 0::32] = 0.0
    cst["resetm"] = rm
    cst["iota1k"] = np.broadcast_to(np.arange(1024, dtype=f)[None, :], (128, 1024)).copy()
    tok = np.zeros((128, 768), f)
    for j in range(16):
        for col in range(16):
            r = col // 4
            tok[:, j * 16 + col] = (j // 4) * 16 + r * 4 + (j % 4)
            tok[:, 512 + j * 16 + col] = r * 16 + j
    tok[:, 256:512] = p[:, None].astype(f)
    cst["tokc"] = tok
    re_ = np.arange(64)
    r_, e_ = re_ // 16, re_ % 16
    cst["Gm"] = (e_[:, None] == e_[None, :]).astype(f)
    cst["Gpre"] = ((e_[:, None] == e_[None, :]) & (r_[:, None] < r_[None, :])).astype(f)
    return cst


def prep(x, c, ctx, c_ctx, w_mod, b_mod, g_pre1, g_post1, g_pre2, g_post2, w_in, w_out,
         na_rpb, hg_lb_logits, hg_norm, w_router, w_gate, w_up, w_down):
    f = np.float32
    A_ = lambda a: np.ascontiguousarray(np.asarray(a, dtype=f))
    x, c, ctx, c_ctx = A_(x), A_(c), A_(ctx), A_(c_ctx)
    w_mod, b_mod, w_in, w_out = A_(w_mod)[0], A_(b_mod)[0], A_(w_in)[0], A_(w_out)[0]
    rpb = A_(na_rpb)[0]
    lbl_ = A_(hg_lb_logits)
    hgn_ = A_(hg_norm)[0]
    wr_ = A_(w_router)[0]
    wg_, wu_, wd_ = np.asarray(w_gate)[0], np.asarray(w_up)[0], np.asarray(w_down)[0]
    bc = lambda v: np.ascontiguousarray(np.broadcast_to(v[None, :], (128, v.shape[0])))
    pp = np.arange(512)
    perm = np.where(pp % 32 < 16, pp + 16, pp - 16)
    winx = np.ascontiguousarray(np.concatenate([w_in, w_in[:, 0:512][:, perm], w_in[:, 512:1024][:, perm]], axis=1))
    cst = _consts()
    shared = dict(cst)
    shared.update(wmod=w_mod, bmodb=bc(b_mod), g1b=bc(A_(g_pre1)[0]), gp1b=bc(A_(g_post1)[0]),
                  g2b=bc(A_(g_pre2)[0]), gp2b=bc(A_(g_post2)[0]), winx=winx, wout=w_out,
                  hgnb=bc(np.tile(hgn_, 4)), wr=wr_,
                  lbl=np.ascontiguousarray(lbl_.reshape(2, 4, 128).transpose(2, 0, 1).reshape(128, 8)))
    ccrep = np.ascontiguousarray(np.broadcast_to(c_ctx.reshape(KC, 128).T[:, :, None], (128, KC, 128)).reshape(128, KC * 128))
    d64 = np.arange(128) % 64
    seg = d64 // 32
    first = (d64 % 32) < 16
    inv = (10000.0 ** (-(2.0 * (d64 % 16)) / 32.0)).astype(f)
    in_maps = []
    for core in range(8):
        b, s = core // 4, core % 4
        m = dict(shared)
        xh = np.zeros((HTOK, D), f)
        g0 = TOK * s - 256
        lo, hi = max(g0, 0), min(g0 + HTOK, 8192)
        xh[lo - g0:hi - g0] = x[b, lo:hi]
        m["xh"] = xh
        m["ctxb"] = ctx[b]
        m["crep"] = np.ascontiguousarray(np.broadcast_to(c[b].reshape(KC, 128).T[:, :, None], (128, KC, 128)).reshape(128, KC * 128))
        m["ccrep"] = ccrep
        tl = np.arange(HTOK)
        row = (32 * s - 4 + tl // 64).astype(f)
        colp = (tl % 64).astype(f)
        pos = np.where(seg[:, None] == 0, row[None, :], colp[None, :]).astype(f)
        ang = (pos * inv[:, None]).astype(f)
        m["cosT"] = np.cos(ang).astype(f)
        m["sinT"] = np.where(first[:, None], -np.sin(ang), np.sin(ang)).astype(f)
        mk = np.zeros((8, 128, 5, 1024), f)
        qp = np.arange(128)
        a_, qc = qp // 64, qp % 64
        nn = np.arange(768)
        for cls, rp in enumerate((0, 1, 4, 14, 15)):
            brow = 0 if rp <= 1 else 28 if rp >= 14 else 2 * rp
            r = 32 * s + 2 * rp + a_
            grow = 32 * s - 4 + brow + nn // 64
            kc_ = nn % 64
            r0 = np.clip(r - 4, 0, 120)
            vrow = (grow[None, :] >= r0[:, None]) & (grow[None, :] < r0[:, None] + 8)
            wc0 = np.clip(qc - 8, 0, 48)
            vcol = (kc_[None, :] >= wc0[:, None]) & (kc_[None, :] < wc0[:, None] + 16)
            dr = np.clip(grow[None, :] - r[:, None] + 7, 0, 14)
            dc = np.clip(kc_[None, :] - qc[:, None] + 15, 0, 30)
            bias = rpb[:, dr, dc]
            mk[:, :, cls, 256:1024] = np.where((vrow & vcol)[None], bias, f(NEG))
        m["masks"] = mk.reshape(8, 128, 5 * 1024)
        mf = np.zeros((128, 8), f)
        for j in range(4):
            mf[:, j] = 1.0 if j < s else 0.0
            mf[:, 4 + j] = 1.0 if j > s else 0.0
        m["mfold"] = mf
        sel = np.zeros((64, 16), f)
        for r in range(4):
            for i in range(4):
                sel[r * 16 + 4 * s + i, r * 4 + i] = 1.0
        m["selM"] = sel
        m["selO"] = np.zeros((64, 16), f)
        m["wg4"] = np.ascontiguousarray(wg_[4 * s:4 * s + 4], dtype=f)
        m["wu4"] = np.ascontiguousarray(wu_[4 * s:4 * s + 4], dtype=f)
        m["wd4"] = np.ascontiguousarray(wd_[4 * s:4 * s + 4], dtype=f)
        in_maps.append(m)
    return in_maps


def kernel(**inputs):
    f = np.float32
    in_maps = prep(**inputs)
    import os
    dbg = os.environ.get("KDEBUG", "") == "1"
    nc = build(debug=dbg)
    res = run_bass_kernel_spmd(nc, in_maps, core_ids=list(range(8)))
    if dbg:
        global LAST
        LAST = res.results
    out = np.zeros((2, 8192, D), f)
    for core in range(8):
        b, s = core // 4, core % 4
        out[b, TOK * s:TOK * (s + 1)] = res.results[core]["out"]
    return out
```
